# Optimizing a Trainium2 kernel written in Bass

```python
import math
import jax, jax.numpy as jnp
from jax import lax
import numpy as np

D_MODEL = 4096
BATCH = 1
SEQ = 16384
DEPTH = 2

N_MIXERS = 2

GDN_QK_HEADS = 32
GDN_V_HEADS = 64
GDN_HEAD_DIM = 128
GDN_KEY_DIM = GDN_QK_HEADS * GDN_HEAD_DIM
GDN_VALUE_DIM = GDN_V_HEADS * GDN_HEAD_DIM
GDN_CONV_DIM = 2 * GDN_KEY_DIM + GDN_VALUE_DIM
GDN_IN_DIM = GDN_CONV_DIM + GDN_VALUE_DIM + 2 * GDN_V_HEADS
CONV_WIDTH = 4
GDN_CHUNK = 64

HGRN_EXPAND = 128
HGRN_HEADS = D_MODEL // HGRN_EXPAND
HGRN_HEAD_DIM = D_MODEL // HGRN_HEADS
HGRN_FORGET_DIM = HGRN_HEADS * HGRN_EXPAND
HGRN_VALUE_DIM = HGRN_HEADS * HGRN_HEAD_DIM
HGRN_IN_DIM = 2 * HGRN_FORGET_DIM + 2 * HGRN_VALUE_DIM
HGRN_CHUNK = 64

MOE_GROUPS = 8
MOE_EXPERTS_PER_GROUP = 8
MOE_N_EXPERTS = MOE_GROUPS * MOE_EXPERTS_PER_GROUP
MOE_TOP_K = 2
MOE_D_FF = 256
MOE_BLOCK = 128

NORM_EPS = 1e-6
DEEPNORM_ALPHA = (2 * DEPTH) ** 0.25
DEEPNORM_BETA = (8 * DEPTH) ** -0.25
N_GDN_LAYERS = (DEPTH + 1) // 2
N_HGRN_LAYERS = DEPTH // 2

kernel_name = 'hybrid_gdn_hgrn2_hmoe_deepnorm'


def _rmsnorm(x, w):
    x = x.astype(jnp.float32)
    return x * lax.rsqrt(jnp.mean(x * x, axis=-1, keepdims=True) + NORM_EPS) * w.astype(jnp.float32)


def _layernorm(x, w, b):
    xf = x.astype(jnp.float32)
    mu = jnp.mean(xf, axis=-1, keepdims=True)
    var = jnp.mean(jnp.square(xf - mu), axis=-1, keepdims=True)
    y = (xf - mu) * lax.rsqrt(var + NORM_EPS) * w.astype(jnp.float32) + b.astype(jnp.float32)
    return y.astype(x.dtype)


def _l2norm(x):
    return x * lax.rsqrt(jnp.sum(x * x, axis=-1, keepdims=True) + NORM_EPS)


def _chunk_major(t, chunk):
    b, s, h = t.shape[:3]
    rest = t.shape[3:]
    t = t.reshape(b, s // chunk, chunk, h, *rest)
    return jnp.transpose(t, (1, 0, 3, 2) + tuple(range(4, t.ndim)))


def _seq_major(t):
    n, b, h, c = t.shape[:4]
    rest = t.shape[4:]
    t = jnp.transpose(t, (1, 0, 3, 2) + tuple(range(4, t.ndim)))
    return t.reshape(b, n * c, h, *rest)


def _causal_depthwise_conv(x, w):
    ch = x.shape[-1]
    return lax.conv_general_dilated(
        x, w[:, None, :].astype(x.dtype), window_strides=(1,),
        padding=[(CONV_WIDTH - 1, 0)], dimension_numbers=('NWC', 'WIO', 'NWC'),
        feature_group_count=ch)


def _chunk_gated_delta_rule(q, k, v, g, beta):
    C = GDN_CHUNK
    qc, kc, vc = _chunk_major(q, C), _chunk_major(k, C), _chunk_major(v, C)
    gc, bc = _chunk_major(g, C), _chunk_major(beta, C)
    dv = v.shape[-1]
    gcum = jnp.cumsum(gc, axis=-1)
    causal = jnp.tril(jnp.ones((C, C), dtype=bool))
    strict = jnp.tril(jnp.ones((C, C), dtype=bool), k=-1)
    decay = jnp.exp(jnp.where(causal, gcum[..., :, None] - gcum[..., None, :], -jnp.inf))
    kb = kc * bc[..., None]
    a_strict = jnp.where(strict, jnp.einsum('nbhid,nbhjd->nbhij', kb, kc) * decay, 0.0)
    t_mat = a_strict + jnp.eye(C, dtype=jnp.float32)
    rhs = jnp.concatenate([vc * bc[..., None], kb * jnp.exp(gcum)[..., None]], axis=-1)
    sol = lax.linalg.triangular_solve(t_mat, rhs, left_side=True, lower=True, unit_diagonal=True)
    u_c, w_c = sol[..., :dv], sol[..., dv:]
    qk = jnp.where(causal, jnp.einsum('nbhid,nbhjd->nbhij', qc, kc) * decay, 0.0)
    q_dec = qc * jnp.exp(gcum)[..., None]
    k_dec = kc * jnp.exp(gcum[..., -1:] - gcum)[..., None]
    g_tot = jnp.exp(gcum[..., -1])

    def step(state, xs):
        qk_n, qd_n, kd_n, u_n, w_n, gt_n = xs
        v_new = u_n - jnp.einsum('bhcd,bhde->bhce', w_n, state)
        o_n = jnp.einsum('bhcd,bhde->bhce', qd_n, state) + jnp.einsum('bhij,bhje->bhie', qk_n, v_new)
        state = state * gt_n[..., None, None] + jnp.einsum('bhcd,bhce->bhde', kd_n, v_new)
        return state, o_n

    b, h, dk = q.shape[0], q.shape[2], q.shape[3]
    state0 = jnp.zeros((b, h, dk, dv), jnp.float32)
    _, o = lax.scan(step, state0, (qk, q_dec, k_dec, u_c, w_c, g_tot))
    return _seq_major(o)


def _chunk_gla(q, k, v, log_f):
    C = HGRN_CHUNK
    qc, kc, vc, gc = (_chunk_major(t, C) for t in (q, k, v, log_f))
    bcum = jnp.cumsum(gc, axis=3)
    causal = jnp.tril(jnp.ones((C, C), dtype=bool))[:, :, None]

    def step(state, xs):
        q_n, k_n, v_n, b_n = xs
        dec = jnp.exp(jnp.where(causal, b_n[..., :, None, :] - b_n[..., None, :, :], -jnp.inf))
        att = jnp.einsum('bhid,bhijd,bhjd->bhij', q_n, dec, k_n)
        o_n = (jnp.einsum('bhij,bhje->bhie', att, v_n)
               + jnp.einsum('bhid,bhde->bhie', q_n * jnp.exp(b_n), state))
        b_last = b_n[..., -1:, :]
        state = (state * jnp.exp(b_last)[..., 0, :, None]
                 + jnp.einsum('bhcd,bhce->bhde', k_n * jnp.exp(b_last - b_n), v_n))
        return state, o_n

    b, h, dk, dv = q.shape[0], q.shape[2], q.shape[3], v.shape[3]
    state0 = jnp.zeros((b, h, dk, dv), jnp.float32)
    _, o = lax.scan(step, state0, (qc, kc, vc, bcum))
    return _seq_major(o)


def gated_deltanet_mixer(u, w_in, conv_w, a_log, dt_bias, norm_w, w_out):
    B, S, _ = u.shape
    proj = u @ w_in
    qkv, z, a, b = jnp.split(proj, [GDN_CONV_DIM, GDN_CONV_DIM + GDN_VALUE_DIM,
                                    GDN_CONV_DIM + GDN_VALUE_DIM + GDN_V_HEADS], axis=-1)
    qkv = jax.nn.silu(_causal_depthwise_conv(qkv, conv_w)).astype(jnp.float32)
    q, k, v = jnp.split(qkv, [GDN_KEY_DIM, 2 * GDN_KEY_DIM], axis=-1)
    rep = GDN_V_HEADS // GDN_QK_HEADS
    q = jnp.repeat(_l2norm(q.reshape(B, S, GDN_QK_HEADS, GDN_HEAD_DIM)), rep, axis=2) * (GDN_HEAD_DIM ** -0.5)
    k = jnp.repeat(_l2norm(k.reshape(B, S, GDN_QK_HEADS, GDN_HEAD_DIM)), rep, axis=2)
    v = v.reshape(B, S, GDN_V_HEADS, GDN_HEAD_DIM)
    beta = jax.nn.sigmoid(b.astype(jnp.float32))
    g = -jnp.exp(a_log.astype(jnp.float32)) * jax.nn.softplus(a.astype(jnp.float32) + dt_bias.astype(jnp.float32))
    o = _chunk_gated_delta_rule(q, k, v, g, beta)
    o = _rmsnorm(o, norm_w) * jax.nn.silu(z.astype(jnp.float32).reshape(B, S, GDN_V_HEADS, GDN_HEAD_DIM))
    return o.reshape(B, S, GDN_VALUE_DIM).astype(u.dtype) @ w_out


def hgrn2_mixer(u, w_in, lower_bound, norm_w, w_out):
    B, S, _ = u.shape
    proj = (u @ w_in).astype(jnp.float32)
    q, f, i, og = jnp.split(proj, [HGRN_FORGET_DIM, 2 * HGRN_FORGET_DIM,
                                   2 * HGRN_FORGET_DIM + HGRN_VALUE_DIM], axis=-1)
    lb = lower_bound.astype(jnp.float32)
    log_f = jnp.logaddexp(jnp.log(lb), jnp.log1p(-lb) + jax.nn.log_sigmoid(f))
    k = (1.0 - lb) * jax.nn.sigmoid(-f)
    hd = lambda t, d: t.reshape(B, S, HGRN_HEADS, d)
    o = _chunk_gla(hd(jax.nn.silu(q), HGRN_EXPAND), hd(k, HGRN_EXPAND),
                   hd(i, HGRN_HEAD_DIM), hd(log_f, HGRN_EXPAND))
    o = _rmsnorm(o, norm_w) * jax.nn.silu(hd(og, HGRN_HEAD_DIM))
    return o.reshape(B, S, HGRN_VALUE_DIM).astype(u.dtype) @ w_out


def hierarchical_moe(u, w_group, w_expert, w_gate, w_up, w_down):
    B, S, D = u.shape
    xt = u.reshape(-1, D)
    T = xt.shape[0]
    xf = xt.astype(jnp.float32)
    p_group = jax.nn.softmax(xf @ w_group.astype(jnp.float32), axis=-1)
    grp = jnp.argmax(p_group, axis=-1)
    p_grp = jnp.take_along_axis(p_group, grp[:, None], axis=1)
    logits_e = (xf @ w_expert.astype(jnp.float32)).reshape(T, MOE_GROUPS, MOE_EXPERTS_PER_GROUP)
    logits_sel = jnp.take_along_axis(logits_e, grp[:, None, None], axis=1)[:, 0]
    top_p, top_i = lax.top_k(jax.nn.softmax(logits_sel, axis=-1), MOE_TOP_K)
    gate = p_grp * top_p / jnp.sum(top_p, axis=-1, keepdims=True)
    eid = (grp[:, None] * MOE_EXPERTS_PER_GROUP + top_i).reshape(-1)
    tok = jnp.repeat(jnp.arange(T, dtype=jnp.int32), MOE_TOP_K)
    wts = gate.reshape(-1)
    A = T * MOE_TOP_K
    order = jnp.argsort(eid)
    eid_s, tok_s, w_s = eid[order], tok[order], wts[order]
    counts = jnp.bincount(eid, length=MOE_N_EXPERTS)
    padded = (counts + MOE_BLOCK - 1) // MOE_BLOCK * MOE_BLOCK
    pad_end = jnp.cumsum(padded)
    pad_start = pad_end - padded
    start = jnp.cumsum(counts) - counts
    dest = pad_start[eid_s] + (jnp.arange(A, dtype=jnp.int32) - start[eid_s])
    n_blk = -(-A // MOE_BLOCK) + MOE_N_EXPERTS
    P = n_blk * MOE_BLOCK
    buf_tok = jnp.zeros((P,), jnp.int32).at[dest].set(tok_s)
    buf_w = jnp.zeros((P,), jnp.float32).at[dest].set(w_s)
    blk_e = jnp.minimum(jnp.searchsorted(pad_end, jnp.arange(n_blk) * MOE_BLOCK, side='right'),
                        MOE_N_EXPERTS - 1)

    def step(out, blk):
        idx, wb, e = blk
        xb = xt[idx]
        h = jax.nn.silu(xb @ w_gate[e]) * (xb @ w_up[e])
        yb = (h @ w_down[e]) * wb[:, None].astype(h.dtype)
        return out.at[idx].add(yb), None

    out, _ = lax.scan(step, jnp.zeros_like(xt),
                      (buf_tok.reshape(n_blk, MOE_BLOCK), buf_w.reshape(n_blk, MOE_BLOCK), blk_e))
    return out.reshape(B, S, D)


def setup_inputs(seed: int = 0) -> dict:
    key = jax.random.key(seed)
    ks = jax.random.split(key, 24)
    nrm = jax.random.normal
    D = D_MODEL
    x = nrm(ks[0], (BATCH, SEQ, D), jnp.float32)
    c = nrm(ks[1], (BATCH, D), jnp.float32)
    ada_w = nrm(ks[2], (DEPTH, D, 6 * D), jnp.float32) * (0.5 * D ** -0.5)
    ada_b = 0.01 * nrm(ks[3], (DEPTH, 6 * D), jnp.float32)
    ln_w = 1.0 + 0.02 * nrm(ks[4], (DEPTH, 2, D), jnp.float32)
    ln_b = 0.02 * nrm(ks[5], (DEPTH, 2, D), jnp.float32)
    gdn_w_in = nrm(ks[6], (N_GDN_LAYERS, D, GDN_IN_DIM), jnp.float32) * (D ** -0.5)
    gdn_conv_w = nrm(ks[7], (N_GDN_LAYERS, CONV_WIDTH, GDN_CONV_DIM), jnp.float32) * (CONV_WIDTH ** -0.5)
    gdn_a_log = jnp.log(jax.random.uniform(ks[8], (N_GDN_LAYERS, GDN_V_HEADS), jnp.float32, 1.0, 16.0))
    dt = jnp.exp(jax.random.uniform(ks[9], (N_GDN_LAYERS, GDN_V_HEADS), jnp.float32,
                                    math.log(1e-3), math.log(1e-1)))
    gdn_dt_bias = dt + jnp.log(-jnp.expm1(-dt))
    gdn_norm_w = 1.0 + 0.02 * nrm(ks[10], (N_GDN_LAYERS, GDN_HEAD_DIM), jnp.float32)
    gdn_w_out = nrm(ks[11], (N_GDN_LAYERS, GDN_VALUE_DIM, D), jnp.float32) * (GDN_VALUE_DIM ** -0.5 * DEEPNORM_BETA)
    hgrn_w_in = nrm(ks[12], (N_HGRN_LAYERS, D, HGRN_IN_DIM), jnp.float32) * (D ** -0.5)
    hgrn_lb_logits = 0.5 * nrm(ks[13], (DEPTH, HGRN_FORGET_DIM), jnp.float32)
    hgrn_norm_w = 1.0 + 0.02 * nrm(ks[14], (N_HGRN_LAYERS, HGRN_HEAD_DIM), jnp.float32)
    hgrn_w_out = nrm(ks[15], (N_HGRN_LAYERS, HGRN_VALUE_DIM, D), jnp.float32) * (HGRN_VALUE_DIM ** -0.5 * DEEPNORM_BETA)
    moe_w_group = nrm(ks[16], (DEPTH, D, MOE_GROUPS), jnp.float32) * (D ** -0.5)
    moe_w_expert = nrm(ks[17], (DEPTH, D, MOE_N_EXPERTS), jnp.float32) * (D ** -0.5)
    moe_w_gate = nrm(ks[18], (DEPTH, MOE_N_EXPERTS, D, MOE_D_FF), jnp.float32) * (D ** -0.5)
    moe_w_up = nrm(ks[19], (DEPTH, MOE_N_EXPERTS, D, MOE_D_FF), jnp.float32) * (D ** -0.5)
    moe_w_down = nrm(ks[20], (DEPTH, MOE_N_EXPERTS, MOE_D_FF, D), jnp.float32) * (MOE_D_FF ** -0.5 * DEEPNORM_BETA)
    return {'x': x, 'c': c, 'ada_w': ada_w, 'ada_b': ada_b, 'ln_w': ln_w, 'ln_b': ln_b,
            'gdn_w_in': gdn_w_in, 'gdn_conv_w': gdn_conv_w, 'gdn_a_log': gdn_a_log,
            'gdn_dt_bias': gdn_dt_bias, 'gdn_norm_w': gdn_norm_w, 'gdn_w_out': gdn_w_out,
            'hgrn_w_in': hgrn_w_in, 'hgrn_lb_logits': hgrn_lb_logits, 'hgrn_norm_w': hgrn_norm_w,
            'hgrn_w_out': hgrn_w_out, 'moe_w_group': moe_w_group, 'moe_w_expert': moe_w_expert,
            'moe_w_gate': moe_w_gate, 'moe_w_up': moe_w_up, 'moe_w_down': moe_w_down}


def reference(x, c, ada_w, ada_b, ln_w, ln_b, gdn_w_in, gdn_conv_w, gdn_a_log, gdn_dt_bias,
              gdn_norm_w, gdn_w_out, hgrn_w_in, hgrn_lb_logits, hgrn_norm_w, hgrn_w_out,
              moe_w_group, moe_w_expert, moe_w_gate, moe_w_up, moe_w_down):
    p_lb = jax.nn.softmax(hgrn_lb_logits.astype(jnp.float32), axis=0)
    lower_bounds = jnp.cumsum(p_lb, axis=0) - p_lb[0]
    cond = jax.nn.silu(c)
    h = x
    for layer in range(DEPTH):
        mod = cond @ ada_w[layer] + ada_b[layer]
        shift1, scale1, gate1, shift2, scale2, gate2 = [m[:, None, :] for m in jnp.split(mod, 6, axis=-1)]
        u = h * (1.0 + scale1) + shift1
        j = layer // N_MIXERS
        if layer % N_MIXERS == 0:
            y = gated_deltanet_mixer(u, gdn_w_in[j], gdn_conv_w[j], gdn_a_log[j], gdn_dt_bias[j],
                                     gdn_norm_w[j], gdn_w_out[j])
        else:
            y = hgrn2_mixer(u, hgrn_w_in[j], lower_bounds[layer], hgrn_norm_w[j], hgrn_w_out[j])
        h = _layernorm(DEEPNORM_ALPHA * h + (1.0 + gate1) * y, ln_w[layer, 0], ln_b[layer, 0])
        u = h * (1.0 + scale2) + shift2
        y = hierarchical_moe(u, moe_w_group[layer], moe_w_expert[layer], moe_w_gate[layer],
                             moe_w_up[layer], moe_w_down[layer])
        h = _layernorm(DEEPNORM_ALPHA * h + (1.0 + gate2) * y, ln_w[layer, 1], ln_b[layer, 1])
    return h
```

```python
import numpy as np
import concourse.bass as bass
import concourse.mybir as mybir
from concourse.bass_utils import run_bass_kernel_spmd

F32 = mybir.dt.float32; F32R = mybir.dt.float32r
I32 = mybir.dt.int32
AF = mybir.ActivationFunctionType; ALU = mybir.AluOpType
AX = mybir.AxisListType

D = 4096; SEQ = 16384; NCORE = 8; KC = D // 128
ALPHA = 4.0 ** 0.25
EPS = 1e-6
SIM_MODE = False


class Sched:
    def __init__(self, nc, n_dma_sems=32):
        self.nc = nc
        self.eng = {'pe': nc.tensor, 'dve': nc.vector, 'act': nc.scalar, 'pool': nc.gpsimd, 'sp': nc.sync}
        self.esem = {k: nc.alloc_semaphore('es_' + k) for k in ('pe', 'dve', 'act', 'pool')}
        self.ecnt = {k: 0 for k in self.esem}
        self.dsem = [nc.alloc_semaphore('ds%d' % i) for i in range(n_dma_sems)]
        self.dcnt = [0] * n_dma_sems
        self.dnext = 0
        self.waited = {}
        self.lastw = {}
        self.readers = {}
        self.nins = 0

    def _wait(self, e, ev):
        semkey, h, val, src = ev
        if src == e and e == 'pe':
            return
        k = (e, semkey)
        if self.waited.get(k, 0) >= val:
            return
        self.waited[k] = val
        self.eng[e].wait_ge(h, val)

    def _deps(self, e, reads, writes):
        for r in reads:
            ev = self.lastw.get(r)
            if ev is not None:
                self._wait(e, ev)
        for w in writes:
            ev = self.lastw.get(w)
            if ev is not None:
                self._wait(e, ev)
            for ev in self.readers.get(w, ()):
                self._wait(e, ev)

    def _commit(self, ev, reads, writes):
        for r in reads:
            lst = self.readers.setdefault(r, [])
            lst.append(ev)
            if len(lst) > 48:
                best = {}
                for x in lst:
                    if x[0] not in best or best[x[0]][2] < x[2]:
                        best[x[0]] = x
                self.readers[r] = list(best.values())
        for w in writes:
            self.lastw[w] = ev
            self.readers[w] = []

    def op(self, e, fn, reads=(), writes=()):
        self._deps(e, reads, writes)
        ins = fn(self.eng[e])
        self.ecnt[e] += 1
        ins.then_inc(self.esem[e], 1)
        ev = (e, self.esem[e], self.ecnt[e], e)
        self._commit(ev, reads, writes)
        self.nins += 1
        return ev

    def dma(self, q, out, in_, reads=(), writes=(), **kw):
        if q == 'pool' and SIM_MODE:
            q = 'sp'
            out = out.bitcast(F32)
        self._deps(q, reads, writes)
        i = self.dnext
        self.dnext = (self.dnext + 1) % len(self.dsem)
        if self.dcnt[i] > 0:
            self._wait(q, (('d', i), self.dsem[i], self.dcnt[i], None))
        self.dcnt[i] += 16
        self.eng[q].dma_start(out=out, in_=in_, **kw).then_inc(self.dsem[i], 16)
        ev = (('d', i), self.dsem[i], self.dcnt[i], None)
        self._commit(ev, reads, writes)
        self.nins += 1
        return ev

    def _dma_pool(self, out, in_, reads, writes, **kw):
        self._deps('pool', reads, writes)
        key = ('pd', writes[0])
        if not hasattr(self, 'psem'):
            self.psem = {}
        if key not in self.psem:
            self.psem[key] = self.nc.alloc_semaphore('pd%d' % len(self.psem))
        else:
            self.eng['pool'].wait_ge(self.psem[key], 16)
            self.eng['pool'].sem_clear(self.psem[key])
            for k in [k for k in self.waited if k[1] == key]:
                del self.waited[k]
        h = self.psem[key]
        self.eng['pool'].dma_start(out=out, in_=in_, **kw).then_inc(h, 16)
        ev = (key, h, 16, None)
        self._commit(ev, reads, writes)
        self.nins += 1
        return ev

    def finish(self, evs, q='sp'):
        for ev in evs:
            self._wait(q, ev)


def _run(nc, in_maps):
    res = run_bass_kernel_spmd(nc, in_maps, core_ids=list(range(NCORE)))
    return res.results


def blk_cols(w, kc=None):
    K, N = w.shape
    kc = K // 128
    nb = N // 128
    return np.ascontiguousarray(w.reshape(kc, 128, nb, 128).transpose(2, 1, 0, 3).reshape(nb, 128, kc * 128))


def vec_pk(v):
    v = np.asarray(v, np.float32).reshape(-1, 128)
    return np.ascontiguousarray(v.T)


def build_mod(ncols):
    nc = bass.Bass("TRN2", target_bir_lowering=False)
    nb = ncols // 128
    c_in = nc.dram_tensor("c", [128, KC], F32, kind="ExternalInput").ap()
    w_in = nc.dram_tensor("w", [D, ncols], F32, kind="ExternalInput").ap()
    b_in = nc.dram_tensor("b", [128, nb], F32, kind="ExternalInput").ap()
    out = nc.dram_tensor("out", [128, nb], F32, kind="ExternalOutput").ap()
    S = Sched(nc)
    ct = nc.alloc_sbuf_tensor("ct", [128, KC], F32)
    cond = nc.alloc_sbuf_tensor("cond", [128, KC], F32)
    sg = nc.alloc_sbuf_tensor("sg", [128, KC], F32)
    bt = nc.alloc_sbuf_tensor("bt", [128, nb], F32)
    ot = nc.alloc_sbuf_tensor("ot", [128, nb], F32)
    CW = 512
    wt = [nc.alloc_sbuf_tensor("wt%d" % i, [128, KC, CW], F32) for i in range(2)]
    pm = nc.alloc_psum_tensor("pm", [128, 512], F32)
    S.dma('sp', ct[:], c_in, writes=['ct'])
    S.dma('sp', bt[:], b_in, writes=['bt'])
    S.op('act', lambda e: e.activation(out=sg[:], in_=ct[:], func=AF.Sigmoid), reads=['ct'], writes=['sg'])
    S.op('dve', lambda e: e.tensor_tensor(out=cond[:], in0=ct[:], in1=sg[:], op=ALU.mult), reads=['ct', 'sg'], writes=['cond'])
    wv = w_in.rearrange("(kc p) n -> p kc n", p=128)
    for g in range(ncols // CW):
        buf = wt[g % 2]
        S.dma('sp', buf[:], wv[:, :, g * CW:(g + 1) * CW], writes=[('wt', g % 2)])
        for jj in range(CW // 128):
            j = g * (CW // 128) + jj
            for kc in range(KC):
                S.op('pe', lambda e: e.matmul(pm[:, j:j + 1], buf[:, kc, jj * 128:(jj + 1) * 128], cond[:, kc:kc + 1],
                                              start=(kc == 0), stop=(kc == KC - 1)),
                     reads=[('wt', g % 2), 'cond'], writes=['pm'])
    S.op('dve', lambda e: e.tensor_tensor(out=ot[:], in0=pm[:, 0:nb], in1=bt[:], op=ALU.add), reads=['pm', 'bt'], writes=['ot'])
    ev = S.dma('sp', out, ot[:], reads=['ot'])
    S.finish([ev])
    return nc


def run_mod(c, ada_w, ada_b):
    depth = ada_w.shape[0]
    ncols_all = depth * 6 * D
    ncols = ncols_all // NCORE
    nc = build_mod(ncols)
    cpk = vec_pk(c.reshape(-1))
    in_maps = []
    for i in range(NCORE):
        lo = i * ncols
        l = lo // (6 * D)
        off = lo % (6 * D)
        w = np.ascontiguousarray(ada_w[l][:, off:off + ncols])
        b = vec_pk(ada_b[l][off:off + ncols])
        in_maps.append({"c": cpk, "w": w, "b": b})
    res = _run(nc, in_maps)
    mod = np.concatenate([np.ascontiguousarray(r["out"].T).reshape(-1) for r in res]).reshape(depth, 6 * D)
    return mod


def emit_ln_tile(S, nc, T, zt, zkey, NT, eps, lnw, lnb, out_fn, ones, ps1, ps2, tmp, k1='ps1', k2='ps2'):
    sq, mean, msq, var, rstd, nmr, t1 = tmp
    for blk in range(KC):
        S.op('act', lambda e: e.activation(out=sq[blk % 2][:], in_=zt[:, blk, :], func=AF.Square),
             reads=[(zkey, blk)], writes=[('sq', blk % 2)])
        S.op('pe', lambda e: e.matmul(ps1[:, 0:NT], ones[:], zt[:, blk, :], start=(blk == 0), stop=(blk == KC - 1)),
             reads=[(zkey, blk), 'ones'], writes=[k1])
        S.op('pe', lambda e: e.matmul(ps2[:, 0:NT], ones[:], sq[blk % 2][:], start=(blk == 0), stop=(blk == KC - 1)),
             reads=[('sq', blk % 2), 'ones'], writes=[k2])
    S.op('act', lambda e: e.activation(out=mean[:], in_=ps1[:, 0:NT], func=AF.Copy, scale=1.0 / D), reads=[k1], writes=['mean'])
    S.op('dve', lambda e: e.tensor_tensor(out=msq[:], in0=mean[:], in1=mean[:], op=ALU.mult), reads=['mean'], writes=['msq'])
    S.op('dve', lambda e: e.scalar_tensor_tensor(out=var[:], in0=ps2[:, 0:NT], scalar=1.0 / D, in1=msq[:], op0=ALU.mult, op1=ALU.subtract),
         reads=[k2, 'msq'], writes=['var'])
    S.op('dve', lambda e: e.tensor_scalar(out=var[:], in0=var[:], scalar1=eps, scalar2=None, op0=ALU.add), reads=['var'], writes=['var'])
    S.op('act', lambda e: e.activation(out=var[:], in_=var[:], func=AF.Ln), reads=['var'], writes=['var'])
    S.op('act', lambda e: e.activation(out=rstd[:], in_=var[:], func=AF.Exp, scale=-0.5), reads=['var'], writes=['rstd'])
    S.op('dve', lambda e: e.scalar_tensor_tensor(out=nmr[:], in0=mean[:], scalar=-1.0, in1=rstd[:], op0=ALU.mult, op1=ALU.mult),
         reads=['mean', 'rstd'], writes=['nmr'])
    for blk in range(KC):
        tt = t1[blk % 2]
        S.op('pool', lambda e: e.tensor_tensor(out=tt[:], in0=zt[:, blk, :], in1=rstd[:], op=ALU.mult),
             reads=[(zkey, blk), 'rstd'], writes=[('t1', blk % 2)])
        S.op('dve', lambda e: e.tensor_tensor(out=tt[:], in0=tt[:], in1=nmr[:], op=ALU.add),
             reads=[('t1', blk % 2), 'nmr'], writes=[('t1', blk % 2)])
        out_fn(blk, tt, ('t1', blk % 2))


def alloc_ln_tmp(nc, NT):
    sq = [nc.alloc_sbuf_tensor("ln_sq%d" % i, [128, NT], F32) for i in range(2)]
    t1 = [nc.alloc_sbuf_tensor("ln_t1%d" % i, [128, NT], F32) for i in range(2)]
    names = ["mean", "msq", "var", "rstd", "nmr"]
    ts = [nc.alloc_sbuf_tensor("ln_" + n, [128, NT], F32) for n in names]
    return (sq, ts[0], ts[1], ts[2], ts[3], ts[4], t1)


def build_outln(VD, TK, NT=256):
    nc = bass.Bass("TRN2", target_bir_lowering=False)
    VC = VD // 128
    ogT = nc.dram_tensor("ogT", [VD, TK], F32, kind="ExternalInput").ap()
    hT = nc.dram_tensor("hT", [D, TK], F32, kind="ExternalInput").ap()
    wout = nc.dram_tensor("wout", [KC, 128, VD], F32, kind="ExternalInput").ap()
    vecs = nc.dram_tensor("vecs", [128, 3, KC], F32, kind="ExternalInput").ap()
    out = nc.dram_tensor("out", [D, TK], F32, kind="ExternalOutput").ap()
    S = Sched(nc)
    vt = nc.alloc_sbuf_tensor("vt", [128, 3, KC], F32)
    gs = nc.alloc_sbuf_tensor("gs", [128, KC], F32)
    ones = nc.alloc_sbuf_tensor("ones", [128, 128], F32)
    ogt = nc.alloc_sbuf_tensor("ogt", [128, VC, NT], F32R)
    zt = nc.alloc_sbuf_tensor("zt", [128, KC, NT], F32)
    ot = nc.alloc_sbuf_tensor("ot", [128, KC, NT], F32)
    wb = [nc.alloc_sbuf_tensor("wb%d" % i, [128, VC, 128], F32R) for i in range(2)]
    tmp = alloc_ln_tmp(nc, NT)
    py = [nc.alloc_psum_tensor("py%d" % i, [128, 512], F32) for i in range(2)]
    ps1 = nc.alloc_psum_tensor("ps1", [128, 512], F32)
    ps2 = nc.alloc_psum_tensor("ps2", [128, 512], F32)
    S.dma('sp', vt[:], vecs, writes=['vt'])
    S.op('dve', lambda e: e.memset(ones[:], 1.0), writes=['ones'])
    S.op('dve', lambda e: e.tensor_scalar(out=gs[:], in0=vt[:, 0, :], scalar1=1.0, scalar2=1.0 / ALPHA, op0=ALU.add, op1=ALU.mult),
         reads=['vt'], writes=['gs'])
    ogv = ogT.rearrange("(kc p) t -> p kc t", p=128)
    hv = hT.rearrange("(kc p) t -> p kc t", p=128)
    ov = out.rearrange("(kc p) t -> p kc t", p=128)
    evs = []
    for t in range(TK // NT):
        ts = slice(t * NT, (t + 1) * NT)
        S.dma('pool', ogt[:], ogv[:, :, ts], writes=['ogt'])
        S.dma('sp', zt[:], hv[:, :, ts], writes=[('zt', b) for b in range(KC)])
        for blk in range(KC):
            w = wb[blk % 2]
            S.dma('pool', w[:].rearrange("p k c -> p (k c)"), wout[blk], writes=[('wb', blk % 2)], max_dma_last_dim=8192)
            p = py[blk % 2]
            for kc in range(VC):
                S.op('pe', lambda e: e.matmul(p[:, 0:NT], w[:, kc, :], ogt[:, kc, :], start=(kc == 0), stop=(kc == VC - 1)),
                     reads=[('wb', blk % 2), 'ogt'], writes=[('py', blk % 2)])
            S.op('dve', lambda e: e.scalar_tensor_tensor(out=zt[:, blk, :], in0=p[:, 0:NT], scalar=gs[:, blk:blk + 1], in1=zt[:, blk, :],
                                                          op0=ALU.mult, op1=ALU.add),
                 reads=[('py', blk % 2), 'gs', ('zt', blk)], writes=[('zt', blk)])

        def out_fn(blk, tt, tkey):
            S.op('act', lambda e: e.activation(out=ot[:, blk, :], in_=tt[:], func=AF.Identity, scale=vt[:, 1, blk:blk + 1], bias=vt[:, 2, blk:blk + 1]),
                 reads=[tkey, 'vt'], writes=[('ot', blk)])
        emit_ln_tile(S, nc, t, zt, 'zt', NT, EPS / (ALPHA * ALPHA), None, None, out_fn, ones, ps1, ps2, tmp)
        evs.append(S.dma('sp', ov[:, :, ts], ot[:], reads=[('ot', b) for b in range(KC)]))
    S.finish(evs)
    return nc


def run_outln(ogT_full, hT_full, w_out, gate, lnw, lnb, TK=None):
    VD, T = ogT_full.shape
    TK = T // NCORE
    nc = build_outln(VD, TK)
    vecs = np.ascontiguousarray(np.stack([vec_pk(gate), vec_pk(lnw), vec_pk(lnb)], axis=1))
    w_blk = blk_cols(w_out)
    in_maps = []
    for i in range(NCORE):
        in_maps.append({"ogT": np.ascontiguousarray(ogT_full[:, i * TK:(i + 1) * TK]),
                        "hT": np.ascontiguousarray(hT_full[:, i * TK:(i + 1) * TK]),
                        "wout": w_blk, "vecs": vecs})
    res = _run(nc, in_maps)
    return np.concatenate([r["out"] for r in res], axis=1)


def make_affine(S, out_tile_ap, key, ones_ap, pattern, base, cm, cmp, fill=0.0):
    S.op('pool', lambda e: e.affine_select(out=out_tile_ap, in_=ones_ap, pattern=pattern, compare_op=cmp, fill=fill,
                                           base=base, channel_multiplier=cm),
         reads=['ones'], writes=[key])


NE = 64; NG = 8; EPG = 8; DFF = 256


def build_moe(TK, NT=256, EPG=8, grouped=False):
    NE = EPG if grouped else NG * EPG
    n_exp = NE
    NT = min(NT, TK)
    nc = bass.Bass("TRN2", target_bir_lowering=False)
    h1T = nc.dram_tensor("h1T", [D, TK], F32, kind="ExternalInput").ap()
    vecs = nc.dram_tensor("vecs", [128, 5, KC], F32, kind="ExternalInput").ap()
    wr = nc.dram_tensor("wr", [D, NG + NE], F32, kind="ExternalInput").ap()
    wg = nc.dram_tensor("wg", [NE, 2, 128, KC * 128], F32, kind="ExternalInput").ap()
    wu = nc.dram_tensor("wu", [NE, 2, 128, KC * 128], F32, kind="ExternalInput").ap()
    wd = nc.dram_tensor("wd", [NE, DFF, D], F32, kind="ExternalInput").ap()
    out = nc.dram_tensor("out", [D, TK], F32, kind="ExternalOutput").ap()
    S = Sched(nc)
    NR = NG + NE
    vt = nc.alloc_sbuf_tensor("vt", [128, 5, KC], F32)
    s2 = nc.alloc_sbuf_tensor("s2", [128, KC], F32)
    gs = nc.alloc_sbuf_tensor("gs", [128, KC], F32)
    ones = nc.alloc_sbuf_tensor("ones", [128, 128], F32)
    ident = nc.alloc_sbuf_tensor("ident", [128, 128], F32)
    wrt = nc.alloc_sbuf_tensor("wrt", [128, KC, NR], F32)
    A = nc.alloc_sbuf_tensor("A", [128, KC, NT], F32R)
    Af = A[:].bitcast(F32)
    Y = nc.alloc_sbuf_tensor("Y", [128, KC, NT], F32)
    wgb = [nc.alloc_sbuf_tensor("wgb%d" % i, [128, KC, 128], F32R) for i in range(2)]
    wub = [nc.alloc_sbuf_tensor("wub%d" % i, [128, KC, 128], F32R) for i in range(2)]
    wdb = [nc.alloc_sbuf_tensor("wdb%d" % i, [128, D], F32R) for i in range(2)]
    tmp = alloc_ln_tmp(nc, NT)
    osm = [nc.alloc_sbuf_tensor("osm%d" % i, [128, NT], F32) for i in range(2)]
    lgt = nc.alloc_sbuf_tensor("lgt", [128, NR], F32)
    sm = nc.alloc_sbuf_tensor("sm", [128, 16], F32)
    ohg = nc.alloc_sbuf_tensor("ohg", [128, NG], F32)
    eg = nc.alloc_sbuf_tensor("eg", [128, NG], F32)
    lm = nc.alloc_sbuf_tensor("lm", [128, NG, EPG], F32)
    lm2 = nc.alloc_sbuf_tensor("lm2", [128, NE], F32)
    oh1 = nc.alloc_sbuf_tensor("oh1", [128, NE], F32)
    oh2 = nc.alloc_sbuf_tensor("oh2", [128, NE], F32)
    wts = nc.alloc_sbuf_tensor("wts", [128, NE], F32)
    wtsT = nc.alloc_sbuf_tensor("wtsT", [NE, NT], F32)
    rw = [nc.alloc_sbuf_tensor("rw%d" % i, [NE, NT], F32) for i in range(2)]
    sgt = [nc.alloc_sbuf_tensor("sgt%d" % i, [128, NT], F32) for i in range(2)]
    tgt = [nc.alloc_sbuf_tensor("tgt%d" % i, [128, NT], F32) for i in range(2)]
    actT = [nc.alloc_sbuf_tensor("actT%d" % i, [128, NT], F32R) for i in range(4)]
    PS = [nc.alloc_psum_tensor("ps%d" % i, [128, 512], F32) for i in range(8)]

    def pk(i):
        return ('ps', i)

    S.dma('sp', vt[:], vecs, writes=['vt'])
    S.dma('sp', wrt[:], wr.rearrange("(kc p) n -> p kc n", p=128), writes=['wrt'])
    S.op('dve', lambda e: e.memset(ones[:], 1.0), writes=['ones'])
    make_affine(S, ident[:], 'ident', ones[:], [[-1, 128]], 0, 1, ALU.is_equal)
    S.op('dve', lambda e: e.tensor_scalar(out=s2[:], in0=vt[:, 0, :], scalar1=1.0, scalar2=None, op0=ALU.add), reads=['vt'], writes=['s2'])
    S.op('dve', lambda e: e.tensor_scalar(out=gs[:], in0=vt[:, 2, :], scalar1=1.0, scalar2=1.0 / ALPHA, op0=ALU.add, op1=ALU.mult),
         reads=['vt'], writes=['gs'])
    hv = h1T.rearrange("(kc p) t -> p kc t", p=128)
    ov = out.rearrange("(kc p) t -> p kc t", p=128)
    evs = []
    Akeys = [('A', b) for b in range(KC)]
    wcount = [0]
    for t in range(TK // NT):
        ts = slice(t * NT, (t + 1) * NT)
        S.dma('sp', Y[:], hv[:, :, ts], writes=[('Y', b) for b in range(KC)])
        for blk in range(KC):
            S.op('act', lambda e: e.activation(out=A[:, blk, :], in_=Y[:, blk, :], func=AF.Identity, scale=s2[:, blk:blk + 1], bias=vt[:, 1, blk:blk + 1]),
                 reads=[('Y', blk), 's2', 'vt'], writes=[('A', blk)])
        for sub in range(NT // 128):
            ss = slice(sub * 128, (sub + 1) * 128)
            for kc in range(KC):
                S.op('pe', lambda e: e.matmul(PS[4][:, 0:NR], Af[:, kc, ss], wrt[:, kc, :], start=(kc == 0), stop=(kc == KC - 1)),
                     reads=[('A', kc), 'wrt'], writes=[pk(4)])
            S.op('act', lambda e: e.activation(out=lgt[:], in_=PS[4][:, 0:NR], func=AF.Copy), reads=[pk(4)], writes=['lgt'])
            R = ['lgt', 'sm', 'ohg', 'eg', 'lm', 'lm2', 'oh1', 'oh2', 'wts']
            def dv(fn):
                S.op('dve', fn, reads=R, writes=R)
            def ac(fn):
                S.op('act', fn, reads=R, writes=R)
            gm, ngm, sge, pgrp, m1, m2, dl, e2, wA, wB = [sm[:, i:i + 1] for i in range(10)]
            dv(lambda e: e.tensor_reduce(out=gm, in_=lgt[:, 0:NG], axis=AX.X, op=ALU.max))
            dv(lambda e: e.tensor_scalar(out=ngm, in0=gm, scalar1=-1.0, scalar2=None, op0=ALU.mult))
            ac(lambda e: e.activation(out=eg[:], in_=lgt[:, 0:NG], func=AF.Exp, bias=ngm, accum_out=sge))
            dv(lambda e: e.reciprocal(out=pgrp, in_=sge))
            if grouped:
                lmf = lm[:, 0, :]
                dv(lambda e: e.tensor_copy(out=lmf, in_=lgt[:, NG:NR]))
            else:
                dv(lambda e: e.tensor_scalar(out=ohg[:], in0=lgt[:, 0:NG], scalar1=gm, scalar2=None, op0=ALU.is_equal))
                dv(lambda e: e.tensor_scalar(out=ohg[:], in0=ohg[:], scalar1=-1.0, scalar2=30000.0, op0=ALU.add, op1=ALU.mult))
                dv(lambda e: e.tensor_tensor(out=lm[:], in0=lgt[:, NG:NR].rearrange("p (g x) -> p g x", g=NG),
                                             in1=ohg[:].unsqueeze(2).to_broadcast([128, NG, EPG]), op=ALU.add))
                lmf = lm[:].rearrange("p g x -> p (g x)")
            dv(lambda e: e.tensor_reduce(out=m1, in_=lmf, axis=AX.X, op=ALU.max))
            dv(lambda e: e.tensor_scalar(out=oh1[:], in0=lmf, scalar1=m1, scalar2=None, op0=ALU.is_equal))
            dv(lambda e: e.scalar_tensor_tensor(out=lm2[:], in0=oh1[:], scalar=-30000.0, in1=lmf, op0=ALU.mult, op1=ALU.add))
            dv(lambda e: e.tensor_reduce(out=m2, in_=lm2[:], axis=AX.X, op=ALU.max))
            dv(lambda e: e.tensor_scalar(out=oh2[:], in0=lm2[:], scalar1=m2, scalar2=None, op0=ALU.is_equal))
            dv(lambda e: e.tensor_tensor(out=dl, in0=m2, in1=m1, op=ALU.subtract))
            ac(lambda e: e.activation(out=e2, in_=dl, func=AF.Exp))
            dv(lambda e: e.tensor_scalar(out=wA, in0=e2, scalar1=1.0, scalar2=None, op0=ALU.add))
            dv(lambda e: e.reciprocal(out=wA, in_=wA))
            dv(lambda e: e.tensor_tensor(out=wA, in0=wA, in1=pgrp, op=ALU.mult))
            dv(lambda e: e.tensor_tensor(out=wB, in0=wA, in1=e2, op=ALU.mult))
            dv(lambda e: e.tensor_scalar(out=wts[:], in0=oh1[:], scalar1=wA, scalar2=None, op0=ALU.mult))
            dv(lambda e: e.scalar_tensor_tensor(out=wts[:], in0=oh2[:], scalar=wB, in1=wts[:], op0=ALU.mult, op1=ALU.add))
            S.op('pe', lambda e: e.transpose(PS[4][0:NE, 0:128], wts[:], ident[:]), reads=R + ['ident'], writes=[pk(4)])
            S.op('act', lambda e: e.activation(out=wtsT[:, ss], in_=PS[4][0:NE, 0:128], func=AF.Copy), reads=[pk(4)], writes=['wtsT'])
        for ex in range(n_exp):
            r = rw[ex % 2]
            S.op('dve', lambda e: e.tensor_scalar(out=r[:], in0=wtsT[:], scalar1=ident[0:NE, ex:ex + 1], scalar2=None, op0=ALU.mult),
                 reads=['wtsT', 'ident'], writes=[('rw', ex % 2)])
            S.op('pe', lambda e: e.matmul(PS[4][:, 0:NT], ones[0:NE, :], r[:], start=True, stop=True),
                 reads=[('rw', ex % 2), 'ones'], writes=[pk(4)])
            for fb in range(2):
                wi = wcount[0] % 2
                wcount[0] += 1
                S.dma('pool', wgb[wi][:].rearrange("p k c -> p (k c)"), wg[ex, fb], writes=[('wgb', wi)], max_dma_last_dim=8192)
                S.dma('pool', wub[wi][:].rearrange("p k c -> p (k c)"), wu[ex, fb], writes=[('wub', wi)], max_dma_last_dim=8192)
                S.dma('pool', wdb[wi][:], wd[ex, fb * 128:(fb + 1) * 128, :], writes=[('wdb', wi)], max_dma_last_dim=8192)
                pg = PS[0 + wi]; pu = PS[2 + wi]
                for kc in range(KC):
                    S.op('pe', lambda e: e.matmul(pg[:, 0:NT], wgb[wi][:, kc, :], A[:, kc, :], start=(kc == 0), stop=(kc == KC - 1)),
                         reads=[('wgb', wi), ('A', kc)], writes=[pk(0 + wi)])
                for kc in range(KC):
                    S.op('pe', lambda e: e.matmul(pu[:, 0:NT], wub[wi][:, kc, :], A[:, kc, :], start=(kc == 0), stop=(kc == KC - 1)),
                         reads=[('wub', wi), ('A', kc)], writes=[pk(2 + wi)])
                S.op('act', lambda e: e.activation(out=sgt[wi][:], in_=pg[:, 0:NT], func=AF.Silu), reads=[pk(0 + wi)], writes=[('sgt', wi)])
                S.op('dve', lambda e: e.tensor_tensor(out=tgt[wi][:], in0=pu[:, 0:NT], in1=sgt[wi][:], op=ALU.mult),
                     reads=[pk(2 + wi), ('sgt', wi)], writes=[('tgt', wi)])
                ai = (ex % 2) * 2 + fb
                S.op('dve', lambda e: e.tensor_tensor(out=actT[ai][:], in0=PS[4][:, 0:NT], in1=tgt[wi][:], op=ALU.mult),
                     reads=[pk(4), ('tgt', wi)], writes=[('actT', ai)])
            for blk in range(KC):
                pyi = 5 + (blk % 2)
                for fb in range(2):
                    ai = (ex % 2) * 2 + fb
                    wi = (wcount[0] - 2 + fb) % 2
                    S.op('pe', lambda e: e.matmul(PS[pyi][:, 0:NT], wdb[wi][:, blk * 128:(blk + 1) * 128], actT[ai][:], start=(fb == 0), stop=(fb == 1)),
                         reads=[('wdb', wi), ('actT', ai)], writes=[pk(pyi)])
                if ex == 0:
                    S.op('dve', lambda e: e.tensor_copy(out=Y[:, blk, :], in_=PS[pyi][:, 0:NT]), reads=[pk(pyi)], writes=[('Y', blk)])
                else:
                    S.op('dve', lambda e: e.tensor_tensor(out=Y[:, blk, :], in0=PS[pyi][:, 0:NT], in1=Y[:, blk, :], op=ALU.add),
                         reads=[pk(pyi), ('Y', blk)], writes=[('Y', blk)])
        for blk in range(KC):
            hs = sgt[blk % 2]
            S.dma('sp', hs[:], hv[:, blk, ts], writes=[('sgt', blk % 2)])
            S.op('dve', lambda e: e.scalar_tensor_tensor(out=Y[:, blk, :], in0=Y[:, blk, :], scalar=gs[:, blk:blk + 1], in1=hs[:],
                                                          op0=ALU.mult, op1=ALU.add),
                 reads=[('Y', blk), ('sgt', blk % 2), 'gs'], writes=[('Y', blk)])

        def out_fn(blk, tt, tkey):
            o = osm[blk % 2]
            S.op('act', lambda e: e.activation(out=o[:], in_=tt[:], func=AF.Identity, scale=vt[:, 3, blk:blk + 1], bias=vt[:, 4, blk:blk + 1]),
                 reads=[tkey, 'vt'], writes=[('osm', blk % 2)])
            evs.append(S.dma('sp', ov[:, blk, ts], o[:], reads=[('osm', blk % 2)]))
        emit_ln_tile(S, nc, t, Y, 'Y', NT, EPS / (ALPHA * ALPHA), None, None, out_fn, ones, PS[0], PS[2], tmp, k1=('ps', 0), k2=('ps', 2))
    S.finish(evs)
    return nc


def run_moe(h1T_full, scale2, shift2, gate2, lnw, lnb, w_group, w_expert, w_gate, w_up, w_down):
    T = h1T_full.shape[1]
    TK = T // NCORE
    nc = build_moe(TK, EPG=w_expert.shape[1] // NG)
    vecs = np.ascontiguousarray(np.stack([vec_pk(scale2), vec_pk(shift2), vec_pk(gate2), vec_pk(lnw), vec_pk(lnb)], axis=1))
    wr = np.ascontiguousarray(np.concatenate([w_group, w_expert], axis=1))
    in_maps = []
    for i in range(NCORE):
        in_maps.append({"h1T": np.ascontiguousarray(h1T_full[:, i * TK:(i + 1) * TK]), "vecs": vecs, "wr": wr,
                        "wg": np.stack([blk_cols(w_gate[e]) for e in range(w_gate.shape[0])]),
                        "wu": np.stack([blk_cols(w_up[e]) for e in range(w_up.shape[0])]), "wd": w_down})
    res = _run(nc, in_maps)
    return np.concatenate([r["out"] for r in res], axis=1)


def build_hgrn(T, HPC=4, NT=256):
    nc = bass.Bass("TRN2", target_bir_lowering=False)
    NCB = 4 * HPC
    hT = nc.dram_tensor("hT", [D, T], F32, kind="ExternalInput").ap()
    vecs = nc.dram_tensor("vecs", [128, 2, KC], F32, kind="ExternalInput").ap()
    w = nc.dram_tensor("w", [D, NCB * 128], F32, kind="ExternalInput").ap()
    lbl = nc.dram_tensor("lbl", [128, 2, HPC], F32, kind="ExternalInput").ap()
    nw_in = nc.dram_tensor("nw", [128, 1], F32, kind="ExternalInput").ap()
    out = nc.dram_tensor("out", [HPC * 128, T], F32, kind="ExternalOutput").ap()
    S = Sched(nc)
    vt = nc.alloc_sbuf_tensor("vt", [128, 2, KC], F32)
    s1 = nc.alloc_sbuf_tensor("s1", [128, KC], F32)
    lbt = nc.alloc_sbuf_tensor("lbt", [128, 2, HPC], F32)
    lb = nc.alloc_sbuf_tensor("lb", [128, HPC], F32)
    oml = nc.alloc_sbuf_tensor("oml", [128, HPC], F32)
    nw = nc.alloc_sbuf_tensor("nwt", [128, 1], F32)
    ones = nc.alloc_sbuf_tensor("ones", [128, 128], F32)
    ident = nc.alloc_sbuf_tensor("ident", [128, 128], F32)
    maskU = nc.alloc_sbuf_tensor("maskU", [128, 128], F32)
    Hs = nc.alloc_sbuf_tensor("Hs", [128, KC, NT], F32)
    U = nc.alloc_sbuf_tensor("U", [128, KC, NT], F32R)
    wb = [nc.alloc_sbuf_tensor("wb%d" % i, [128, KC, 128], F32R) for i in range(2)]
    QT = nc.alloc_sbuf_tensor("QT", [128, HPC, NT], F32)
    KT = nc.alloc_sbuf_tensor("KT", [128, HPC, NT], F32)
    LF = nc.alloc_sbuf_tensor("LF", [128, HPC, NT], F32)
    VT = nc.alloc_sbuf_tensor("VT", [128, HPC, NT], F32)
    OG = nc.alloc_sbuf_tensor("OG", [128, HPC, NT], F32)
    OT = nc.alloc_sbuf_tensor("OT", [128, HPC, NT], F32)
    St = nc.alloc_sbuf_tensor("St", [128, HPC, 128], F32)
    sgm = nc.alloc_sbuf_tensor("sgm", [128, NT], F32)
    NB = 2
    def mk(name):
        return [nc.alloc_sbuf_tensor("%s%d" % (name, i), [128, 128], F32) for i in range(NB)]
    B_, E1, Qt, Kt, Qs, KdT, At, V_, Kd, On, Jk = mk("B"), mk("E1"), mk("Qt"), mk("Kt"), mk("Qs"), mk("KdT"), mk("At"), mk("V"), mk("Kd"), mk("On"), mk("Jk")
    sc = [nc.alloc_sbuf_tensor("sc%d" % i, [128, 8], F32) for i in range(NB)]
    PS = [nc.alloc_psum_tensor("ps%d" % i, [128, 512], F32) for i in range(8)]
    pk = lambda i: ('ps', i)

    S.dma('sp', vt[:], vecs, writes=['vt'])
    S.dma('sp', lbt[:], lbl, writes=['lbt'])
    S.dma('sp', nw[:], nw_in, writes=['nw'])
    S.op('dve', lambda e: e.memset(ones[:], 1.0), writes=['ones'])
    S.op('dve', lambda e: e.memset(St[:], 0.0), writes=[('S', h) for h in range(HPC)])
    make_affine(S, ident[:], 'ident', ones[:], [[-1, 128]], 0, 1, ALU.is_equal)
    make_affine(S, maskU[:], 'maskU', ones[:], [[1, 128]], 0, -1, ALU.is_ge)
    S.op('dve', lambda e: e.tensor_scalar(out=s1[:], in0=vt[:, 0, :], scalar1=1.0, scalar2=None, op0=ALU.add), reads=['vt'], writes=['s1'])
    S.op('dve', lambda e: e.tensor_tensor(out=lb[:], in0=lbt[:, 0, :], in1=lbt[:, 1, :], op=ALU.subtract), reads=['lbt'], writes=['lb'])
    S.op('act', lambda e: e.activation(out=lb[:], in_=lb[:], func=AF.Exp), reads=['lb'], writes=['lb'])
    S.op('dve', lambda e: e.tensor_scalar(out=lb[:], in0=lb[:], scalar1=1.0, scalar2=None, op0=ALU.add), reads=['lb'], writes=['lb'])
    S.op('dve', lambda e: e.reciprocal(out=lb[:], in_=lb[:]), reads=['lb'], writes=['lb'])
    S.op('dve', lambda e: e.tensor_scalar(out=oml[:], in0=lb[:], scalar1=-1.0, scalar2=1.0, op0=ALU.mult, op1=ALU.add), reads=['lb'], writes=['oml'])

    hv = hT.rearrange("(kc p) t -> p kc t", p=128)
    wv = w.rearrange("(kc p) n -> p kc n", p=128)
    ov = out.rearrange("(h p) t -> p h t", p=128)
    evs = []
    cnt = [0]
    for t in range(T // NT):
        ts = slice(t * NT, (t + 1) * NT)
        S.dma('sp', Hs[:], hv[:, :, ts], writes=[('Hs', b) for b in range(KC)])
        for blk in range(KC):
            S.op('act', lambda e: e.activation(out=U[:, blk, :], in_=Hs[:, blk, :], func=AF.Identity, scale=s1[:, blk:blk + 1], bias=vt[:, 1, blk:blk + 1]),
                 reads=[('Hs', blk), 's1', 'vt'], writes=[('U', blk)])
        for cb in range(NCB):
            hh, typ = cb // 4, cb % 4
            wi = cb % 2
            S.dma('pool', wb[wi][:], wv[:, :, cb * 128:(cb + 1) * 128], writes=[('wb', wi)])
            pp = PS[wi]
            for kc in range(KC):
                S.op('pe', lambda e: e.matmul(pp[:, 0:NT], wb[wi][:, kc, :], U[:, kc, :], start=(kc == 0), stop=(kc == KC - 1)),
                     reads=[('wb', wi), ('U', kc)], writes=[pk(wi)])
            if typ == 0:
                S.op('act', lambda e: e.activation(out=QT[:, hh, :], in_=pp[:, 0:NT], func=AF.Silu), reads=[pk(wi)], writes=[('QT', hh)])
            elif typ == 1:
                S.op('act', lambda e: e.activation(out=sgm[:], in_=pp[:, 0:NT], func=AF.Sigmoid), reads=[pk(wi)], writes=['sgm'])
                S.op('dve', lambda e: e.tensor_scalar(out=sgm[:], in0=sgm[:], scalar1=oml[:, hh:hh + 1], scalar2=lb[:, hh:hh + 1], op0=ALU.mult, op1=ALU.add),
                     reads=['sgm', 'oml', 'lb'], writes=['sgm'])
                S.op('act', lambda e: e.activation(out=LF[:, hh, :], in_=sgm[:], func=AF.Ln), reads=['sgm'], writes=[('LF', hh)])
                S.op('dve', lambda e: e.tensor_scalar(out=KT[:, hh, :], in0=sgm[:], scalar1=-1.0, scalar2=1.0, op0=ALU.mult, op1=ALU.add),
                     reads=['sgm'], writes=[('KT', hh)])
            elif typ == 2:
                S.op('act', lambda e: e.activation(out=VT[:, hh, :], in_=pp[:, 0:NT], func=AF.Copy), reads=[pk(wi)], writes=[('VT', hh)])
            else:
                S.op('act', lambda e: e.activation(out=OG[:, hh, :], in_=pp[:, 0:NT], func=AF.Silu), reads=[pk(wi)], writes=[('OG', hh)])
        for c in range(NT // 128):
            cs = slice(c * 128, (c + 1) * 128)
            for hh in range(HPC):
                i = cnt[0] % NB
                cnt[0] += 1
                k = lambda n: (n, i)
                bref, nbref, blast, eblast, ssq, rstd = [sc[i][:, j:j + 1] for j in range(6)]
                S.op('dve', lambda e: e.tensor_tensor_scan(out=B_[i][:], data0=ones[:], data1=LF[:, hh, cs], initial=0.0, op0=ALU.mult, op1=ALU.add),
                     reads=[('LF', hh), 'ones'], writes=[k('B')])
                S.op('dve', lambda e: e.tensor_copy(out=bref, in_=B_[i][:, 63:64]), reads=[k('B')], writes=[k('sc')])
                S.op('dve', lambda e: e.tensor_scalar(out=nbref, in0=B_[i][:, 63:64], scalar1=-1.0, scalar2=None, op0=ALU.mult), reads=[k('B'), k('sc')], writes=[k('sc')])
                S.op('dve', lambda e: e.tensor_copy(out=blast, in_=B_[i][:, 127:128]), reads=[k('B'), k('sc')], writes=[k('sc')])
                S.op('act', lambda e: e.activation(out=eblast, in_=blast, func=AF.Exp), reads=[k('sc')], writes=[k('sc')])
                S.op('act', lambda e: e.activation(out=E1[i][:], in_=B_[i][:], func=AF.Exp, bias=nbref), reads=[k('B'), k('sc')], writes=[k('E1')])
                S.op('dve', lambda e: e.tensor_tensor(out=Qt[i][:], in0=QT[:, hh, cs], in1=E1[i][:], op=ALU.mult), reads=[('QT', hh), k('E1')], writes=[k('Qt')])
                S.op('act', lambda e: e.activation(out=E1[i][:], in_=B_[i][:], func=AF.Exp, scale=-1.0, bias=bref), reads=[k('B'), k('sc'), k('Qt')], writes=[k('E1')])
                S.op('dve', lambda e: e.tensor_tensor(out=Kt[i][:], in0=KT[:, hh, cs], in1=E1[i][:], op=ALU.mult), reads=[('KT', hh), k('E1')], writes=[k('Kt')])
                S.op('act', lambda e: e.activation(out=E1[i][:], in_=B_[i][:], func=AF.Exp), reads=[k('B'), k('Kt')], writes=[k('E1')])
                S.op('dve', lambda e: e.tensor_tensor(out=Qs[i][:], in0=QT[:, hh, cs], in1=E1[i][:], op=ALU.mult), reads=[('QT', hh), k('E1')], writes=[k('Qs')])
                S.op('act', lambda e: e.activation(out=E1[i][:], in_=B_[i][:], func=AF.Exp, scale=-1.0, bias=blast), reads=[k('B'), k('sc'), k('Qs')], writes=[k('E1')])
                S.op('dve', lambda e: e.tensor_tensor(out=KdT[i][:], in0=KT[:, hh, cs], in1=E1[i][:], op=ALU.mult), reads=[('KT', hh), k('E1')], writes=[k('KdT')])
                S.op('pe', lambda e: e.matmul(PS[2][:, 0:128], Kt[i][:], Qt[i][:], start=True, stop=True), reads=[k('Kt'), k('Qt')], writes=[pk(2)])
                S.op('dve', lambda e: e.tensor_scalar(out=At[i][:], in0=PS[2][:, 0:128], scalar1=-1e30, scalar2=1e30, op0=ALU.max, op1=ALU.min),
                     reads=[pk(2)], writes=[k('At')])
                S.op('dve', lambda e: e.tensor_tensor(out=At[i][:], in0=At[i][:], in1=maskU[:], op=ALU.mult), reads=[k('At'), 'maskU'], writes=[k('At')])
                S.op('pe', lambda e: e.transpose(PS[3][:, 0:128], VT[:, hh, cs], ident[:]), reads=[('VT', hh), 'ident'], writes=[pk(3)])
                S.op('act', lambda e: e.activation(out=V_[i][:], in_=PS[3][:, 0:128], func=AF.Copy), reads=[pk(3)], writes=[k('V')])
                S.op('pe', lambda e: e.transpose(PS[4][:, 0:128], KdT[i][:], ident[:]), reads=[k('KdT'), 'ident'], writes=[pk(4)])
                S.op('dve', lambda e: e.tensor_copy(out=Kd[i][:], in_=PS[4][:, 0:128]), reads=[pk(4)], writes=[k('Kd')])
                S.op('pe', lambda e: e.matmul(PS[5][:, 0:128], At[i][:], V_[i][:], start=True, stop=False), reads=[k('At'), k('V')], writes=[pk(5)])
                S.op('pe', lambda e: e.matmul(PS[5][:, 0:128], Qs[i][:], St[:, hh, :], start=False, stop=True), reads=[k('Qs'), ('S', hh)], writes=[pk(5)])
                S.op('pe', lambda e: e.matmul(PS[6][:, 0:128], Kd[i][:], V_[i][:], start=True, stop=True), reads=[k('Kd'), k('V')], writes=[pk(6)])
                S.op('dve', lambda e: e.scalar_tensor_tensor(out=St[:, hh, :], in0=St[:, hh, :], scalar=eblast, in1=PS[6][:, 0:128], op0=ALU.mult, op1=ALU.add),
                     reads=[('S', hh), k('sc'), pk(6)], writes=[('S', hh)])
                S.op('act', lambda e: e.activation(out=Jk[i][:], in_=PS[5][:, 0:128], func=AF.Square, accum_out=ssq), reads=[pk(5), k('sc')], writes=[k('Jk'), k('sc')])
                S.op('dve', lambda e: e.tensor_scalar(out=ssq, in0=ssq, scalar1=1.0 / 128, scalar2=EPS, op0=ALU.mult, op1=ALU.add), reads=[k('sc')], writes=[k('sc')])
                S.op('act', lambda e: e.activation(out=ssq, in_=ssq, func=AF.Ln), reads=[k('sc')], writes=[k('sc')])
                S.op('act', lambda e: e.activation(out=rstd, in_=ssq, func=AF.Exp, scale=-0.5), reads=[k('sc')], writes=[k('sc')])
                S.op('dve', lambda e: e.tensor_scalar(out=On[i][:], in0=PS[5][:, 0:128], scalar1=rstd, scalar2=None, op0=ALU.mult), reads=[pk(5), k('sc')], writes=[k('On')])
                S.op('pe', lambda e: e.transpose(PS[7][:, 0:128], On[i][:], ident[:]), reads=[k('On'), 'ident'], writes=[pk(7)])
                S.op('dve', lambda e: e.scalar_tensor_tensor(out=OT[:, hh, cs], in0=PS[7][:, 0:128], scalar=nw[:, 0:1], in1=OG[:, hh, cs], op0=ALU.mult, op1=ALU.mult),
                     reads=[pk(7), 'nw', ('OG', hh)], writes=[('OT', hh)])
        evs.append(S.dma('sp', ov[:, :, ts], OT[:], reads=[('OT', h) for h in range(HPC)]))
    S.finish(evs)
    return nc


def run_hgrn(hT_full, scale1, shift1, w_in, lb_logits, norm_w):
    T = hT_full.shape[1]
    HPC = 32 // NCORE
    nc = build_hgrn(T, HPC=HPC)
    vecs = np.ascontiguousarray(np.stack([vec_pk(scale1), vec_pk(shift1)], axis=1))
    nwp = np.ascontiguousarray(norm_w.reshape(128, 1).astype(np.float32))
    in_maps = []
    for c in range(NCORE):
        cols = []
        for hh in range(HPC):
            h = c * HPC + hh
            for typ in range(4):
                cols.append(w_in[:, typ * D + h * 128: typ * D + (h + 1) * 128])
        wc = np.ascontiguousarray(np.concatenate(cols, axis=1))
        ch = slice(c * HPC * 128, (c + 1) * HPC * 128)
        lbl = np.ascontiguousarray(np.stack([vec_pk(lb_logits[0, ch]), vec_pk(lb_logits[1, ch])], axis=1))
        in_maps.append({"hT": hT_full, "vecs": vecs, "w": wc, "lbl": lbl, "nw": nwp})
    res = _run(nc, in_maps)
    return np.concatenate([r["out"] for r in res], axis=0)


def build_gdn(T, HV=8, NT=256, stage=9):
    nc = bass.Bass("TRN2", target_bir_lowering=False)
    HQ = HV // 2
    NCONV = 2 * HQ + HV
    NCB = NCONV + HV
    hT = nc.dram_tensor("hT", [D, T], F32, kind="ExternalInput").ap()
    vecs = nc.dram_tensor("vecs", [128, 2, KC], F32, kind="ExternalInput").ap()
    w = nc.dram_tensor("w", [NCB, 128, KC * 128], F32, kind="ExternalInput").ap()
    wab = nc.dram_tensor("wab", [D, 2 * HV], F32, kind="ExternalInput").ap()
    cw_in = nc.dram_tensor("cw", [128, NCONV, 4], F32, kind="ExternalInput").ap()
    hp_in = nc.dram_tensor("hp", [128, 2, HV], F32, kind="ExternalInput").ap()
    nw_in = nc.dram_tensor("nw", [128, 1], F32, kind="ExternalInput").ap()
    out = nc.dram_tensor("out", [HV * 128, T], F32, kind="ExternalOutput").ap()
    S = Sched(nc)
    A_ = nc.alloc_sbuf_tensor
    vt = A_("vt", [128, 2, KC], F32); s1 = A_("s1", [128, KC], F32)
    cw = A_("cwt", [128, NCONV, 4], F32); hp = A_("hpt", [128, 2, HV], F32); nA = A_("nA", [128, HV], F32)
    nw = A_("nwt", [128, 1], F32)
    ones = A_("ones", [128, 128], F32); ident = A_("ident", [128, 128], F32)
    mU = A_("mU", [128, 128], F32); nSL = A_("nSL", [128, 128], F32); nSU = A_("nSU", [128, 128], F32); nU = A_("nU", [128, 128], F32)
    Hs = A_("Hs", [128, KC // 2, NT], F32); U = A_("U", [128, KC, NT], F32R)
    wb = [A_("wb%d" % i, [128, KC, 128], F32R) for i in range(2)]
    wabt = A_("wabt", [128, KC, 2 * HV], F32R)
    pre = [A_("pre%d" % i, [128, NT + 3], F32) for i in range(2)]
    halo = A_("halo", [128, NCONV, 3], F32)
    acc = [A_("acc%d" % i, [128, NT], F32) for i in range(2)]
    xs = [A_("xs%d" % i, [128, NT], F32) for i in range(2)]
    sq = acc
    QT = A_("QT", [128, HQ, NT], F32); KT = A_("KT", [128, HQ, NT], F32)
    VT = A_("VT", [128, HV, NT], F32); SZ = A_("SZ", [128, HV, NT], F32); OT = A_("OT", [128, HV, NT], F32)
    St = A_("St", [128, HV, 128], F32)
    GS = {n: A_("gs_" + n, [128, HV], F32) for n in ("emb", "den", "beta", "lnb", "tt", "g", "gc", "gl", "glb", "ngc", "egc", "bgc", "ekd", "egl")}
    NB = 2
    def mk(name, n=NB, w_=128):
        return [A_("%s%d" % (name, i), [128, w_], F32) for i in range(n)]
    KKs, KQs, Ktok = mk("KKs", HQ), mk("KQs", HQ), mk("Ktok", HQ)
    tA, tAT, tQ = mk("tA"), mk("tAT"), mk("tQ")
    Am, ATm, QKT = mk("Am", HV), mk("ATm", HV), mk("QKT", HV)
    Pm = [mk("Pm0", HV), mk("Pm1", HV)]; Qm = [mk("Qm0", HV), mk("Qm1", HV)]; Rm = [mk("Rm0", HV), mk("Rm1", HV)]
    kd, Us, WTs = mk("kd", HV), mk("Us", HV), mk("WTs", HV)
    vb, kbg, Os, On = mk("vb", HV), mk("kbg", HV), mk("Os", HV), mk("On", HV)
    vn, o1s = mk("vn"), mk("o1s")
    scs = A_("scs", [128, HV, 2], F32)
    sc = [A_("sc%d" % i, [128, 4], F32) for i in range(NB)]
    PS = [nc.alloc_psum_tensor("ps%d" % i, [128, 512], F32) for i in range(8)]
    pk = lambda i: ('ps', i)
    slots = [(b, 0) for b in range(3, 8)]
    sl_i = [0]
    def slot():
        s = slots[sl_i[0] % len(slots)]
        sl_i[0] += 1
        return s
    def sv(s):
        return PS[s[0]][:, s[1] * 128:(s[1] + 1) * 128]
    def sk(s):
        return ('ps', s[0])
    pair_i = [0]

    S.dma('sp', vt[:], vecs, writes=['vt'])
    S.dma('sp', cw[:], cw_in, writes=['cw'])
    S.dma('sp', hp[:], hp_in, writes=['hp'])
    S.dma('sp', nw[:], nw_in, writes=['nw'])
    S.dma('pool', wabt[:], wab.rearrange("(kc p) n -> p kc n", p=128), writes=['wabt'])
    S.op('dve', lambda e: e.memset(ones[:], 1.0), writes=['ones'])
    S.op('dve', lambda e: e.memset(St[:], 0.0), writes=[('S', h) for h in range(HV)])
    S.op('dve', lambda e: e.memset(halo[:], 0.0), writes=[('halo', b) for b in range(NCONV)])
    make_affine(S, ident[:], 'ident', ones[:], [[-1, 128]], 0, 1, ALU.is_equal)
    make_affine(S, mU[:], 'mU', ones[:], [[1, 128]], 0, -1, ALU.is_ge)
    make_affine(S, nU[:], 'nU', ones[:], [[1, 128]], 0, -1, ALU.is_ge)
    make_affine(S, nSU[:], 'nSU', ones[:], [[1, 128]], 0, -1, ALU.is_gt)
    make_affine(S, nSL[:], 'nSL', ones[:], [[-1, 128]], 0, 1, ALU.is_gt)
    for m_, k_ in ((nU, 'nU'), (nSU, 'nSU'), (nSL, 'nSL')):
        S.op('dve', lambda e: e.tensor_scalar(out=m_[:], in0=m_[:], scalar1=-1.0, scalar2=30000.0, op0=ALU.add, op1=ALU.mult), reads=[k_], writes=[k_])
    S.op('dve', lambda e: e.tensor_scalar(out=s1[:], in0=vt[:, 0, :], scalar1=1.0, scalar2=None, op0=ALU.add), reads=['vt'], writes=['s1'])
    S.op('act', lambda e: e.activation(out=nA[:], in_=hp[:, 0, :], func=AF.Exp), reads=['hp'], writes=['nA'])
    S.op('dve', lambda e: e.tensor_scalar(out=nA[:], in0=nA[:], scalar1=-1.0, scalar2=None, op0=ALU.mult), reads=['nA'], writes=['nA'])

    hv_ = hT.rearrange("(kc p) t -> p kc t", p=128)
    ov = out.rearrange("(h p) t -> p h t", p=128)
    evs = []
    cnt = [0]
    GK = ['gs']
    for t in range(T // NT):
        ts = slice(t * NT, (t + 1) * NT)
        for half in range(2):
            hb = half * (KC // 2)
            S.dma('sp', Hs[:], hv_[:, hb:hb + KC // 2, ts], writes=[('Hs', b) for b in range(KC // 2)])
            for b in range(KC // 2):
                blk = hb + b
                S.op('act', lambda e: e.activation(out=U[:, blk, :], in_=Hs[:, b, :], func=AF.Identity, scale=s1[:, blk:blk + 1], bias=vt[:, 1, blk:blk + 1]),
                     reads=[('Hs', b), 's1', 'vt'], writes=[('U', blk)])
        deferred = []
        for cb in range(NCB):
            wi = cb % 2
            S.dma('pool', wb[wi][:].rearrange("p k c -> p (k c)"), w[cb], writes=[('wb', wi)], max_dma_last_dim=8192)
            pp = PS[wi]
            for kc in range(KC):
                S.op('pe', lambda e: e.matmul(pp[:, 0:NT], wb[wi][:, kc, :], U[:, kc, :], start=(kc == 0), stop=(kc == KC - 1)),
                     reads=[('wb', wi), ('U', kc)], writes=[pk(wi)])
            while deferred:
                deferred.pop(0)()
            if cb >= NCONV:
                hv = cb - NCONV
                S.op('act', lambda e: e.activation(out=SZ[:, hv, :], in_=pp[:, 0:NT], func=AF.Silu), reads=[pk(wi)], writes=[('SZ', hv)])
                continue
            pr = pre[wi]; ac = acc[wi]
            S.op('dve', lambda e: e.tensor_copy(out=pr[:, 0:3], in_=halo[:, cb, :]), reads=[('halo', cb)], writes=[('pre', wi)])
            S.op('act', lambda e: e.activation(out=pr[:, 3:3 + NT], in_=pp[:, 0:NT], func=AF.Copy), reads=[pk(wi)], writes=[('pre', wi)])
            S.op('dve', lambda e: e.tensor_copy(out=halo[:, cb, :], in_=pr[:, NT:NT + 3]), reads=[('pre', wi)], writes=[('halo', cb)])
            S.op('dve', lambda e: e.tensor_scalar(out=ac[:], in0=pr[:, 0:NT], scalar1=cw[:, cb, 0:1], scalar2=None, op0=ALU.mult),
                 reads=[('pre', wi), 'cw'], writes=[('acc', wi)])
            for j in (1, 2, 3):
                S.op('dve', lambda e: e.scalar_tensor_tensor(out=ac[:], in0=pr[:, j:j + NT], scalar=cw[:, cb, j:j + 1], in1=ac[:], op0=ALU.mult, op1=ALU.add),
                     reads=[('pre', wi), 'cw', ('acc', wi)], writes=[('acc', wi)])
            if cb >= 2 * HQ:
                hv = cb - 2 * HQ
                S.op('act', lambda e: e.activation(out=VT[:, hv, :], in_=ac[:], func=AF.Silu), reads=[('acc', wi)], writes=[('VT', hv)])
                continue
            x = xs[wi]; s_ = sq[wi]
            S.op('act', lambda e: e.activation(out=x[:], in_=ac[:], func=AF.Silu), reads=[('acc', wi)], writes=[('xs', wi)])
            S.op('act', lambda e: e.activation(out=s_[:], in_=x[:], func=AF.Square), reads=[('xs', wi)], writes=[('acc', wi)])
            def l2tail(cb=cb, wi=wi, x=x, s_=s_):
                S.op('pe', lambda e: e.matmul(PS[2][:, 0:NT], ones[:], s_[:], start=True, stop=True), reads=[('acc', wi), 'ones'], writes=[pk(2)])
                S.op('dve', lambda e: e.tensor_scalar(out=s_[:], in0=PS[2][:, 0:NT], scalar1=EPS, scalar2=None, op0=ALU.add), reads=[pk(2)], writes=[('acc', wi)])
                S.op('act', lambda e: e.activation(out=s_[:], in_=s_[:], func=AF.Ln), reads=[('acc', wi)], writes=[('acc', wi)])
                S.op('act', lambda e: e.activation(out=s_[:], in_=s_[:], func=AF.Exp, scale=-0.5), reads=[('acc', wi)], writes=[('acc', wi)])
                if cb < HQ:
                    S.op('dve', lambda e: e.scalar_tensor_tensor(out=QT[:, cb, :], in0=x[:], scalar=128.0 ** -0.5, in1=s_[:], op0=ALU.mult, op1=ALU.mult),
                         reads=[('xs', wi), ('acc', wi)], writes=[('QT', cb)])
                else:
                    S.op('dve', lambda e: e.tensor_tensor(out=KT[:, cb - HQ, :], in0=x[:], in1=s_[:], op=ALU.mult),
                         reads=[('xs', wi), ('acc', wi)], writes=[('KT', cb - HQ)])
            deferred.append(l2tail)
        while deferred:
            deferred.pop(0)()
        for c in range(NT // 128 if stage >= 1 else 0):
            cs = slice(c * 128, (c + 1) * 128)
            for kc in range(KC):
                S.op('pe', lambda e: e.matmul(PS[2][:, 0:2 * HV], U[:, kc, cs], wabt[:, kc, :], start=(kc == 0), stop=(kc == KC - 1)),
                     reads=[('U', kc), 'wabt'], writes=[pk(2)])
            G = GS
            def dv(fn, extra=()):
                S.op('dve', fn, reads=GK + list(extra), writes=GK)
            def ac_(fn, extra=()):
                S.op('act', fn, reads=GK + list(extra), writes=GK)
            ac_(lambda e: e.activation(out=G['emb'][:], in_=PS[2][:, HV:2 * HV], func=AF.Exp, scale=-1.0), [pk(2)])
            dv(lambda e: e.tensor_scalar(out=G['den'][:], in0=G['emb'][:], scalar1=1.0, scalar2=None, op0=ALU.add))
            dv(lambda e: e.reciprocal(out=G['beta'][:], in_=G['den'][:]))
            ac_(lambda e: e.activation(out=G['lnb'][:], in_=G['den'][:], func=AF.Ln))
            dv(lambda e: e.tensor_scalar(out=G['lnb'][:], in0=G['lnb'][:], scalar1=-1.0, scalar2=None, op0=ALU.mult))
            dv(lambda e: e.tensor_tensor(out=G['tt'][:], in0=PS[2][:, 0:HV], in1=hp[:, 1, :], op=ALU.add), [pk(2), 'hp'])
            ac_(lambda e: e.activation(out=G['tt'][:], in_=G['tt'][:], func=AF.Exp))
            dv(lambda e: e.tensor_scalar(out=G['tt'][:], in0=G['tt'][:], scalar1=1.0, scalar2=None, op0=ALU.add))
            ac_(lambda e: e.activation(out=G['tt'][:], in_=G['tt'][:], func=AF.Ln))
            dv(lambda e: e.tensor_tensor(out=G['g'][:], in0=G['tt'][:], in1=nA[:], op=ALU.mult), ['nA'])
            S.op('pe', lambda e: e.matmul(PS[2][:, 32:32 + HV], mU[:], G['g'][:], start=True, stop=True), reads=GK + ['mU'], writes=[pk(2)])
            dv(lambda e: e.tensor_copy(out=G['gc'][:], in_=PS[2][:, 32:32 + HV]), [pk(2)])
            S.op('pe', lambda e: e.matmul(PS[2][:, 64:64 + HV], ones[:], G['g'][:], start=True, stop=True), reads=GK + ['ones'], writes=[pk(2)])
            dv(lambda e: e.tensor_copy(out=G['gl'][:], in_=PS[2][:, 64:64 + HV]), [pk(2)])
            dv(lambda e: e.tensor_tensor(out=G['glb'][:], in0=G['gc'][:], in1=G['lnb'][:], op=ALU.add))
            dv(lambda e: e.tensor_scalar(out=G['ngc'][:], in0=G['gc'][:], scalar1=-1.0, scalar2=None, op0=ALU.mult))
            ac_(lambda e: e.activation(out=G['egc'][:], in_=G['gc'][:], func=AF.Exp))
            dv(lambda e: e.tensor_tensor(out=G['bgc'][:], in0=G['egc'][:], in1=G['beta'][:], op=ALU.mult))
            dv(lambda e: e.tensor_tensor(out=G['ekd'][:], in0=G['gl'][:], in1=G['gc'][:], op=ALU.subtract))
            ac_(lambda e: e.activation(out=G['ekd'][:], in_=G['ekd'][:], func=AF.Exp))
            ac_(lambda e: e.activation(out=G['egl'][:], in_=G['gl'][:], func=AF.Exp))
            if stage < 2:
                continue
            col = lambda n, hv: GS[n][:, hv:hv + 1]
            for hq in range(HQ):
                kq = lambda n: (n, 'q', hq)
                s_kk, s_kq, s_kt = slot(), slot(), slot()
                S.op('pe', lambda e: e.matmul(sv(s_kk), KT[:, hq, cs], KT[:, hq, cs], start=True, stop=True), reads=[('KT', hq)], writes=[sk(s_kk)])
                S.op('pe', lambda e: e.matmul(sv(s_kq), KT[:, hq, cs], QT[:, hq, cs], start=True, stop=True), reads=[('KT', hq), ('QT', hq)], writes=[sk(s_kq)])
                S.op('pe', lambda e: e.transpose(sv(s_kt), KT[:, hq, cs], ident[:]), reads=[('KT', hq), 'ident'], writes=[sk(s_kt)])
                S.op('act', lambda e: e.activation(out=KKs[hq][:], in_=sv(s_kk), func=AF.Copy), reads=[sk(s_kk)], writes=[kq('KKs')])
                S.op('dve', lambda e: e.tensor_copy(out=KQs[hq][:], in_=sv(s_kq)), reads=[sk(s_kq)], writes=[kq('KQs')])
                S.op('act', lambda e: e.activation(out=Ktok[hq][:], in_=sv(s_kt), func=AF.Copy), reads=[sk(s_kt)], writes=[kq('Ktok')])
            for hv in range(HV):
                hq = hv // 2
                kq = lambda n: (n, 'q', hq)
                i = cnt[0] % NB
                cnt[0] += 1
                k = lambda n: (n, 'r', i)
                kh = lambda n: (n, 'h', hv)
                pb = pair_i[0] % 2
                pair_i[0] += 1
                bc1 = PS[2][:, 256 * pb:256 * pb + 128]
                bc2 = PS[2][:, 256 * pb + 128:256 * pb + 256]
                S.op('pe', lambda e: e.matmul(bc1, col('gc', hv).to_broadcast([128, 128]), ident[:], start=True, stop=True), reads=GK + ['ident'], writes=[pk(2)])
                S.op('pe', lambda e: e.matmul(bc2, col('glb', hv).to_broadcast([128, 128]), ident[:], start=True, stop=True), reads=GK + ['ident'], writes=[pk(2)])
                S.op('dve', lambda e: e.scalar_tensor_tensor(out=tA[i][:], in0=bc1, scalar=-1.0, in1=nSL[:], op0=ALU.mult, op1=ALU.add), reads=[pk(2), 'nSL'], writes=[k('tA')])
                S.op('act', lambda e: e.activation(out=tA[i][:], in_=tA[i][:], func=AF.Exp, bias=col('glb', hv)), reads=[k('tA')] + GK, writes=[k('tA')])
                S.op('dve', lambda e: e.tensor_tensor(out=tAT[i][:], in0=bc2, in1=nSU[:], op=ALU.add), reads=[pk(2), 'nSU'], writes=[k('tAT')])
                S.op('act', lambda e: e.activation(out=tAT[i][:], in_=tAT[i][:], func=AF.Exp, bias=col('ngc', hv)), reads=[k('tAT')] + GK, writes=[k('tAT')])
                S.op('dve', lambda e: e.tensor_tensor(out=tQ[i][:], in0=bc1, in1=nU[:], op=ALU.add), reads=[pk(2), 'nU'], writes=[k('tQ')])
                S.op('act', lambda e: e.activation(out=tQ[i][:], in_=tQ[i][:], func=AF.Exp, bias=col('ngc', hv)), reads=[k('tQ')] + GK, writes=[k('tQ')])
                S.op('pool', lambda e: e.tensor_tensor(out=Am[hv][:], in0=KKs[hq][:], in1=tA[i][:], op=ALU.mult), reads=[kq('KKs'), k('tA')], writes=[kh('Am')])
                S.op('pool', lambda e: e.tensor_tensor(out=ATm[hv][:], in0=KKs[hq][:], in1=tAT[i][:], op=ALU.mult), reads=[kq('KKs'), k('tAT')], writes=[kh('ATm')])
                S.op('pool', lambda e: e.tensor_tensor(out=QKT[hv][:], in0=KQs[hq][:], in1=tQ[i][:], op=ALU.mult), reads=[kq('KQs'), k('tQ')], writes=[kh('QKT')])
                S.op('pool', lambda e: e.tensor_tensor(out=Rm[0][hv][:], in0=ident[:], in1=ATm[hv][:], op=ALU.subtract), reads=['ident', kh('ATm')], writes=[kh('R0')])
            for hv in range(HV):
                hq = hv // 2
                kq = lambda n: (n, 'q', hq)
                kh = lambda n: (n, 'h', hv)
                s_v = slot()
                S.op('pe', lambda e: e.transpose(sv(s_v), VT[:, hv, cs], ident[:]), reads=[('VT', hv), 'ident'], writes=[sk(s_v)])
                S.op('act', lambda e: e.activation(out=vb[hv][:], in_=sv(s_v), func=AF.Identity, scale=col('beta', hv)), reads=[sk(s_v)] + GK, writes=[kh('vb')])
                S.op('act', lambda e: e.activation(out=kbg[hv][:], in_=Ktok[hq][:], func=AF.Identity, scale=col('bgc', hv)), reads=[kq('Ktok')] + GK, writes=[kh('kbg')])
                S.op('pool', lambda e: e.tensor_tensor(out=kd[hv][:], in0=Ktok[hq][:], in1=col('ekd', hv).to_broadcast([128, 128]), op=ALU.mult), reads=[kq('Ktok')] + GK, writes=[kh('kd')])
            if stage < 3:
                continue
            cur = {hv: (ATm[hv], ('ATm', 'h', hv), Am[hv], ('Am', 'h', hv), Rm[0][hv], ('R0', 'h', hv)) for hv in range(HV)}
            for m in range(1, 7):
                pi = m % 2
                nxt = {}
                for hv in range(HV):
                    kh = lambda n: (n, 'h', hv)
                    Pc, Pk, Qc, Qk, Rc, Rk = cur[hv]
                    s_q = slot()
                    S.op('pe', lambda e: e.matmul(sv(s_q), Pc[:], Qc[:], start=True, stop=True), reads=[Pk, Qk], writes=[sk(s_q)])
                    Qn = Qm[pi][hv]; Qnk = kh('Q%d' % pi)
                    S.op('act', lambda e: e.activation(out=Qn[:], in_=sv(s_q), func=AF.Copy), reads=[sk(s_q)], writes=[Qnk])
                    Pn, Pnk = Pc, Pk
                    if m < 6:
                        s_p = slot()
                        S.op('pe', lambda e: e.matmul(sv(s_p), Qc[:], Pc[:], start=True, stop=True), reads=[Pk, Qk], writes=[sk(s_p)])
                        Pn = Pm[pi][hv]; Pnk = kh('P%d' % pi)
                        S.op('dve', lambda e: e.tensor_copy(out=Pn[:], in_=sv(s_p)), reads=[sk(s_p)], writes=[Pnk])
                    nxt[hv] = (Pn, Pnk, Qn, Qnk)
                for hv in range(HV):
                    kh = lambda n: (n, 'h', hv)
                    Pn, Pnk, Qn, Qnk = nxt[hv]
                    Rc, Rk = cur[hv][4], cur[hv][5]
                    s_r = slot()
                    S.op('pe', lambda e: e.matmul(sv(s_r), Qn[:], Rc[:], start=True, stop=True), reads=[Qnk, Rk], writes=[sk(s_r)])
                    Rn = Rm[pi][hv]; Rnk = kh('R%d' % pi)
                    S.op('dve', lambda e: e.tensor_tensor(out=Rn[:], in0=sv(s_r), in1=Rc[:], op=ALU.add), reads=[sk(s_r), Rk], writes=[Rnk])
                    cur[hv] = (Pn, Pnk, Qn, Qnk, Rn, Rnk)
            if stage < 4:
                continue
            for hv in range(HV):
                kh = lambda n: (n, 'h', hv)
                Rc, Rk = cur[hv][4], cur[hv][5]
                s_u, s_w = slot(), slot()
                S.op('pe', lambda e: e.matmul(sv(s_u), Rc[:], vb[hv][:], start=True, stop=True), reads=[Rk, kh('vb')], writes=[sk(s_u)])
                S.op('pe', lambda e: e.matmul(sv(s_w), kbg[hv][:], Rc[:], start=True, stop=True), reads=[Rk, kh('kbg')], writes=[sk(s_w)])
                S.op('act', lambda e: e.activation(out=Us[hv][:], in_=sv(s_u), func=AF.Copy), reads=[sk(s_u)], writes=[kh('Us')])
                S.op('dve', lambda e: e.tensor_copy(out=WTs[hv][:], in_=sv(s_w)), reads=[sk(s_w)], writes=[kh('WTs')])
            if stage < 5:
                continue
            for hv in range(HV):
                hq = hv // 2
                kh = lambda n: (n, 'h', hv)
                i = cnt[0] % NB
                cnt[0] += 1
                k = lambda n: (n, 'r', i)
                s_vn, s_o1 = slot(), slot()
                S.op('pe', lambda e: e.matmul(sv(s_vn), WTs[hv][:], St[:, hv, :], start=True, stop=True), reads=[kh('WTs'), ('S', hv)], writes=[sk(s_vn)])
                S.op('pe', lambda e: e.matmul(sv(s_o1), QT[:, hq, cs], St[:, hv, :], start=True, stop=True), reads=[('QT', hq), ('S', hv)], writes=[sk(s_o1)])
                S.op('dve', lambda e: e.tensor_tensor(out=vn[i][:], in0=Us[hv][:], in1=sv(s_vn), op=ALU.subtract), reads=[kh('Us'), sk(s_vn)], writes=[k('vn')])
                S.op('act', lambda e: e.activation(out=o1s[i][:], in_=sv(s_o1), func=AF.Identity, scale=col('egc', hv)), reads=[sk(s_o1)] + GK, writes=[k('o1s')])
                s_o2, s_sn = slot(), slot()
                S.op('pe', lambda e: e.matmul(sv(s_o2), QKT[hv][:], vn[i][:], start=True, stop=True), reads=[kh('QKT'), k('vn')], writes=[sk(s_o2)])
                S.op('pe', lambda e: e.matmul(sv(s_sn), kd[hv][:], vn[i][:], start=True, stop=True), reads=[kh('kd'), k('vn')], writes=[sk(s_sn)])
                S.op('dve', lambda e: e.tensor_tensor(out=Os[hv][:], in0=sv(s_o2), in1=o1s[i][:], op=ALU.add), reads=[sk(s_o2), k('o1s')], writes=[kh('Os')])
                S.op('dve', lambda e: e.scalar_tensor_tensor(out=St[:, hv, :], in0=St[:, hv, :], scalar=col('egl', hv), in1=sv(s_sn), op0=ALU.mult, op1=ALU.add),
                     reads=[('S', hv), sk(s_sn)] + GK, writes=[('S', hv)])
            for hv in range(HV):
                kh = lambda n: (n, 'h', hv)
                ssq, rstd = scs[:, hv, 0:1], scs[:, hv, 1:2]
                S.op('act', lambda e: e.activation(out=On[hv][:], in_=Os[hv][:], func=AF.Square, accum_out=ssq), reads=[kh('Os'), kh('sc')], writes=[kh('On'), kh('sc')])
                S.op('dve', lambda e: e.tensor_scalar(out=ssq, in0=ssq, scalar1=1.0 / 128, scalar2=EPS, op0=ALU.mult, op1=ALU.add), reads=[kh('sc')], writes=[kh('sc')])
                S.op('act', lambda e: e.activation(out=ssq, in_=ssq, func=AF.Ln), reads=[kh('sc')], writes=[kh('sc')])
                S.op('act', lambda e: e.activation(out=rstd, in_=ssq, func=AF.Exp, scale=-0.5), reads=[kh('sc')], writes=[kh('sc')])
                S.op('dve', lambda e: e.tensor_scalar(out=On[hv][:], in0=Os[hv][:], scalar1=rstd, scalar2=None, op0=ALU.mult), reads=[kh('Os'), kh('sc')], writes=[kh('On')])
            for hv in range(HV):
                kh = lambda n: (n, 'h', hv)
                s_ot = slot()
                S.op('pe', lambda e: e.transpose(sv(s_ot), On[hv][:], ident[:]), reads=[kh('On'), 'ident'], writes=[sk(s_ot)])
                S.op('dve', lambda e: e.scalar_tensor_tensor(out=OT[:, hv, cs], in0=sv(s_ot), scalar=nw[:, 0:1], in1=SZ[:, hv, cs], op0=ALU.mult, op1=ALU.mult),
                     reads=[sk(s_ot), 'nw', ('SZ', hv)], writes=[('OT', hv)])
        if stage < 5:
            S.op('dve', lambda e: e.tensor_copy(out=OT[:], in_=VT[:]), reads=[('VT', h) for h in range(HV)] + [('OT', h) for h in range(HV)], writes=[('OT', h) for h in range(HV)])
        evs.append(S.dma('sp', ov[:, :, ts], OT[:], reads=[('OT', h) for h in range(HV)]))
    S.finish(evs)
    return nc


def run_gdn(hT_full, scale1, shift1, w_in, conv_w, a_log, dt_bias, norm_w, stage=9):
    T = hT_full.shape[1]
    HV = 64 // NCORE
    HQ = HV // 2
    nc = build_gdn(T, HV=HV, stage=stage)
    vecs = np.ascontiguousarray(np.stack([vec_pk(scale1), vec_pk(shift1)], axis=1))
    nwp = np.ascontiguousarray(norm_w.reshape(128, 1).astype(np.float32))
    KD = 4096; VDIM = 8192
    in_maps = []
    for c in range(NCORE):
        qcols = [slice((c * HQ + j) * 128, (c * HQ + j + 1) * 128) for j in range(HQ)]
        kcols = [slice(KD + (c * HQ + j) * 128, KD + (c * HQ + j + 1) * 128) for j in range(HQ)]
        vcols = [slice(2 * KD + (c * HV + j) * 128, 2 * KD + (c * HV + j + 1) * 128) for j in range(HV)]
        zcols = [slice(2 * KD + VDIM + (c * HV + j) * 128, 2 * KD + VDIM + (c * HV + j + 1) * 128) for j in range(HV)]
        wc = blk_cols(np.concatenate([w_in[:, s] for s in qcols + kcols + vcols + zcols], axis=1))
        a0 = 2 * KD + 2 * VDIM
        wab = np.ascontiguousarray(np.concatenate([w_in[:, a0 + c * HV: a0 + (c + 1) * HV], w_in[:, a0 + 64 + c * HV: a0 + 64 + (c + 1) * HV]], axis=1))
        cwc = np.concatenate([conv_w[:, s] for s in qcols + kcols + vcols], axis=1)
        cwp = np.ascontiguousarray(cwc.reshape(4, -1, 128).transpose(2, 1, 0))
        hp = np.ascontiguousarray(np.broadcast_to(np.stack([a_log[c * HV:(c + 1) * HV], dt_bias[c * HV:(c + 1) * HV]])[None], (128, 2, HV)).astype(np.float32))
        in_maps.append({"hT": hT_full, "vecs": vecs, "w": wc, "wab": wab, "cw": cwp, "hp": hp, "nw": nwp})
    res = _run(nc, in_maps)
    return np.concatenate([r["out"] for r in res], axis=0)


def build_route(TK, NT=256):
    nc = bass.Bass("TRN2", target_bir_lowering=False)
    NT = min(NT, TK)
    h1T = nc.dram_tensor("h1T", [D, TK], F32, kind="ExternalInput").ap()
    vecs = nc.dram_tensor("vecs", [128, 2, KC], F32, kind="ExternalInput").ap()
    wr = nc.dram_tensor("wr", [D, NG], F32, kind="ExternalInput").ap()
    out = nc.dram_tensor("out", [128, TK // 128], F32, kind="ExternalOutput").ap()
    S = Sched(nc)
    vt = nc.alloc_sbuf_tensor("vt", [128, 2, KC], F32)
    s2 = nc.alloc_sbuf_tensor("s2", [128, KC], F32)
    wrt = nc.alloc_sbuf_tensor("wrt", [128, KC, NG], F32)
    Hs = nc.alloc_sbuf_tensor("Hs", [128, KC, NT], F32)
    A = nc.alloc_sbuf_tensor("A", [128, KC, NT], F32R)
    Af = A[:].bitcast(F32)
    lgt = nc.alloc_sbuf_tensor("lgt", [128, NG], F32)
    ohg = nc.alloc_sbuf_tensor("ohg", [128, NG], F32)
    gm = nc.alloc_sbuf_tensor("gm", [128, 1], F32)
    ioti = nc.alloc_sbuf_tensor("ioti", [128, NG], I32)
    iotf = nc.alloc_sbuf_tensor("iotf", [128, NG], F32)
    gall = nc.alloc_sbuf_tensor("gall", [128, TK // 128], F32)
    ps = nc.alloc_psum_tensor("ps", [128, 512], F32)
    S.dma('sp', vt[:], vecs, writes=['vt'])
    S.dma('sp', wrt[:], wr.rearrange("(kc p) n -> p kc n", p=128), writes=['wrt'])
    S.op('pool', lambda e: e.iota(out=ioti[:], pattern=[[1, NG]], base=0, channel_multiplier=0), writes=['ioti'])
    S.op('dve', lambda e: e.tensor_copy(out=iotf[:], in_=ioti[:]), reads=['ioti'], writes=['iotf'])
    S.op('dve', lambda e: e.tensor_scalar(out=s2[:], in0=vt[:, 0, :], scalar1=1.0, scalar2=None, op0=ALU.add), reads=['vt'], writes=['s2'])
    hv = h1T.rearrange("(kc p) t -> p kc t", p=128)
    R = ['r']
    for t in range(TK // NT):
        ts = slice(t * NT, (t + 1) * NT)
        S.dma('sp', Hs[:], hv[:, :, ts], writes=[('Hs', b) for b in range(KC)])
        for blk in range(KC):
            S.op('act', lambda e: e.activation(out=A[:, blk, :], in_=Hs[:, blk, :], func=AF.Identity, scale=s2[:, blk:blk + 1], bias=vt[:, 1, blk:blk + 1]),
                 reads=[('Hs', blk), 's2', 'vt'], writes=[('A', blk)])
        for sub in range(NT // 128):
            ss = slice(sub * 128, (sub + 1) * 128)
            col = t * (NT // 128) + sub
            for kc in range(KC):
                S.op('pe', lambda e: e.matmul(ps[:, 0:NG], Af[:, kc, ss], wrt[:, kc, :], start=(kc == 0), stop=(kc == KC - 1)),
                     reads=[('A', kc), 'wrt'], writes=['ps'])
            S.op('act', lambda e: e.activation(out=lgt[:], in_=ps[:, 0:NG], func=AF.Copy), reads=['ps'], writes=R)
            S.op('dve', lambda e: e.tensor_reduce(out=gm[:], in_=lgt[:], axis=AX.X, op=ALU.max), reads=R, writes=R)
            S.op('dve', lambda e: e.tensor_scalar(out=ohg[:], in0=lgt[:], scalar1=gm[:, 0:1], scalar2=None, op0=ALU.is_equal), reads=R, writes=R)
            S.op('dve', lambda e: e.tensor_tensor(out=ohg[:], in0=ohg[:], in1=iotf[:], op=ALU.mult), reads=R + ['iotf'], writes=R)
            S.op('dve', lambda e: e.tensor_reduce(out=gall[:, col:col + 1], in_=ohg[:], axis=AX.X, op=ALU.add), reads=R, writes=R + ['gall'])
    ev = S.dma('sp', out, gall[:], reads=['gall'])
    S.finish([ev])
    return nc


def run_moe_grouped(h1T_full, scale2, shift2, gate2, lnw, lnb, w_group, w_expert, w_gate, w_up, w_down):
    T = h1T_full.shape[1]
    TK = T // NCORE
    nc = build_route(TK)
    vecs2 = np.ascontiguousarray(np.stack([vec_pk(scale2), vec_pk(shift2)], axis=1))
    in_maps = [{"h1T": np.ascontiguousarray(h1T_full[:, i * TK:(i + 1) * TK]), "vecs": vecs2, "wr": np.ascontiguousarray(w_group)}
               for i in range(NCORE)]
    res = _run(nc, in_maps)
    gidx = np.concatenate([np.ascontiguousarray(r["out"].T).reshape(-1) for r in res]).astype(np.int64)
    idxs = [np.nonzero(gidx == g)[0] for g in range(NG)]
    NP = max(256, -(-max(len(ix) for ix in idxs) // 256) * 256)
    nc2 = build_moe(NP, EPG=EPG, grouped=True)
    vecs = np.ascontiguousarray(np.stack([vec_pk(scale2), vec_pk(shift2), vec_pk(gate2), vec_pk(lnw), vec_pk(lnb)], axis=1))
    in_maps = []
    for g in range(NG):
        hg = np.zeros((D, NP), np.float32)
        hg[:, :len(idxs[g])] = h1T_full[:, idxs[g]]
        wr = np.ascontiguousarray(np.concatenate([w_group, w_expert[:, g * EPG:(g + 1) * EPG]], axis=1))
        in_maps.append({"h1T": hg, "vecs": vecs, "wr": wr,
                        "wg": np.stack([blk_cols(w_gate[e]) for e in range(g * EPG, (g + 1) * EPG)]),
                        "wu": np.stack([blk_cols(w_up[e]) for e in range(g * EPG, (g + 1) * EPG)]),
                        "wd": np.ascontiguousarray(w_down[g * EPG:(g + 1) * EPG])})
    res = _run(nc2, in_maps)
    outT = np.empty((D, T), np.float32)
    for g in range(NG):
        outT[:, idxs[g]] = res[g]["out"][:, :len(idxs[g])]
    return outT


def kernel(x, c, ada_w, ada_b, ln_w, ln_b, gdn_w_in, gdn_conv_w, gdn_a_log, gdn_dt_bias,
           gdn_norm_w, gdn_w_out, hgrn_w_in, hgrn_lb_logits, hgrn_norm_w, hgrn_w_out,
           moe_w_group, moe_w_expert, moe_w_gate, moe_w_up, moe_w_down):
    f = lambda a: np.asarray(a, np.float32)
    x = f(x); c = f(c)
    mod = run_mod(c, f(ada_w), f(ada_b))
    hT = np.ascontiguousarray(x[0].T)
    for layer in range(2):
        sh1, sc1, g1, sh2, sc2, g2 = [mod[layer, i * D:(i + 1) * D] for i in range(6)]
        if layer % 2 == 0:
            ogT = run_gdn(hT, sc1, sh1, f(gdn_w_in[0]), f(gdn_conv_w[0]), f(gdn_a_log[0]), f(gdn_dt_bias[0]), f(gdn_norm_w[0]))
            w_out = f(gdn_w_out[0])
        else:
            ogT = run_hgrn(hT, sc1, sh1, f(hgrn_w_in[0]), f(hgrn_lb_logits), f(hgrn_norm_w[0]))
            w_out = f(hgrn_w_out[0])
        h1T = run_outln(ogT, hT, w_out, g1, f(ln_w[layer, 0]), f(ln_b[layer, 0]))
        hT = run_moe_grouped(h1T, sc2, sh2, g2, f(ln_w[layer, 1]), f(ln_b[layer, 1]), f(moe_w_group[layer]), f(moe_w_expert[layer]),
                             f(moe_w_gate[layer]), f(moe_w_up[layer]), f(moe_w_down[layer]))
    return np.ascontiguousarray(hT.T)[None].astype(np.float32)
```

```python
import numpy as np
import concourse.bass as bass
import concourse.mybir as mybir
from concourse.bass_utils import run_bass_kernel_spmd

F32 = mybir.dt.float32; F32R = mybir.dt.float32r
I32 = mybir.dt.int32
AF = mybir.ActivationFunctionType; ALU = mybir.AluOpType
AX = mybir.AxisListType

D = 4096; SEQ = 16384; NCORE = 8; KC = D // 128
ALPHA = 4.0 ** 0.25
EPS = 1e-6
SIM_MODE = False


class Sched:
    def __init__(self, nc, n_dma_sems=32):
        self.nc = nc
        self.eng = {'pe': nc.tensor, 'dve': nc.vector, 'act': nc.scalar, 'pool': nc.gpsimd, 'sp': nc.sync}
        self.esem = {k: nc.alloc_semaphore('es_' + k) for k in ('pe', 'dve', 'act', 'pool')}
        self.ecnt = {k: 0 for k in self.esem}
        self.dsem = [nc.alloc_semaphore('ds%d' % i) for i in range(n_dma_sems)]
        self.dcnt = [0] * n_dma_sems
        self.dnext = 0
        self.waited = {}
        self.lastw = {}
        self.readers = {}
        self.nins = 0

    def _wait(self, e, ev):
        semkey, h, val, src = ev
        if src == e and e == 'pe':
            return
        k = (e, semkey)
        if self.waited.get(k, 0) >= val:
            return
        self.waited[k] = val
        self.eng[e].wait_ge(h, val)

    def _deps(self, e, reads, writes):
        for r in reads:
            ev = self.lastw.get(r)
            if ev is not None:
                self._wait(e, ev)
        for w in writes:
            ev = self.lastw.get(w)
            if ev is not None:
                self._wait(e, ev)
            for ev in self.readers.get(w, ()):
                self._wait(e, ev)

    def _commit(self, ev, reads, writes):
        for r in reads:
            lst = self.readers.setdefault(r, [])
            lst.append(ev)
            if len(lst) > 48:
                best = {}
                for x in lst:
                    if x[0] not in best or best[x[0]][2] < x[2]:
                        best[x[0]] = x
                self.readers[r] = list(best.values())
        for w in writes:
            self.lastw[w] = ev
            self.readers[w] = []

    def op(self, e, fn, reads=(), writes=()):
        self._deps(e, reads, writes)
        ins = fn(self.eng[e])
        self.ecnt[e] += 1
        ins.then_inc(self.esem[e], 1)
        ev = (e, self.esem[e], self.ecnt[e], e)
        self._commit(ev, reads, writes)
        self.nins += 1
        return ev

    def dma(self, q, out, in_, reads=(), writes=(), **kw):
        if q == 'pool' and SIM_MODE:
            q = 'sp'
            out = out.bitcast(F32)
        self._deps(q, reads, writes)
        i = self.dnext
        self.dnext = (self.dnext + 1) % len(self.dsem)
        if self.dcnt[i] > 0:
            self._wait(q, (('d', i), self.dsem[i], self.dcnt[i], None))
        self.dcnt[i] += 16
        self.eng[q].dma_start(out=out, in_=in_, **kw).then_inc(self.dsem[i], 16)
        ev = (('d', i), self.dsem[i], self.dcnt[i], None)
        self._commit(ev, reads, writes)
        self.nins += 1
        return ev

    def _dma_pool(self, out, in_, reads, writes, **kw):
        self._deps('pool', reads, writes)
        key = ('pd', writes[0])
        if not hasattr(self, 'psem'):
            self.psem = {}
        if key not in self.psem:
            self.psem[key] = self.nc.alloc_semaphore('pd%d' % len(self.psem))
        else:
            self.eng['pool'].wait_ge(self.psem[key], 16)
            self.eng['pool'].sem_clear(self.psem[key])
            for k in [k for k in self.waited if k[1] == key]:
                del self.waited[k]
        h = self.psem[key]
        self.eng['pool'].dma_start(out=out, in_=in_, **kw).then_inc(h, 16)
        ev = (key, h, 16, None)
        self._commit(ev, reads, writes)
        self.nins += 1
        return ev

    def finish(self, evs, q='sp'):
        for ev in evs:
            self._wait(q, ev)


def _run(nc, in_maps):
    res = run_bass_kernel_spmd(nc, in_maps, core_ids=list(range(NCORE)))
    return res.results


def blk_cols(w, kc=None):
    K, N = w.shape
    kc = K // 128
    nb = N // 128
    return np.ascontiguousarray(w.reshape(kc, 128, nb, 128).transpose(2, 1, 0, 3).reshape(nb, 128, kc * 128))


def vec_pk(v):
    v = np.asarray(v, np.float32).reshape(-1, 128)
    return np.ascontiguousarray(v.T)


def build_mod(ncols):
    nc = bass.Bass("TRN2", target_bir_lowering=False)
    nb = ncols // 128
    c_in = nc.dram_tensor("c", [128, KC], F32, kind="ExternalInput").ap()
    w_in = nc.dram_tensor("w", [D, ncols], F32, kind="ExternalInput").ap()
    b_in = nc.dram_tensor("b", [128, nb], F32, kind="ExternalInput").ap()
    out = nc.dram_tensor("out", [128, nb], F32, kind="ExternalOutput").ap()
    S = Sched(nc)
    ct = nc.alloc_sbuf_tensor("ct", [128, KC], F32)
    cond = nc.alloc_sbuf_tensor("cond", [128, KC], F32)
    sg = nc.alloc_sbuf_tensor("sg", [128, KC], F32)
    bt = nc.alloc_sbuf_tensor("bt", [128, nb], F32)
    ot = nc.alloc_sbuf_tensor("ot", [128, nb], F32)
    CW = 512
    wt = [nc.alloc_sbuf_tensor("wt%d" % i, [128, KC, CW], F32) for i in range(2)]
    pm = nc.alloc_psum_tensor("pm", [128, 512], F32)
    S.dma('sp', ct[:], c_in, writes=['ct'])
    S.dma('sp', bt[:], b_in, writes=['bt'])
    S.op('act', lambda e: e.activation(out=sg[:], in_=ct[:], func=AF.Sigmoid), reads=['ct'], writes=['sg'])
    S.op('dve', lambda e: e.tensor_tensor(out=cond[:], in0=ct[:], in1=sg[:], op=ALU.mult), reads=['ct', 'sg'], writes=['cond'])
    wv = w_in.rearrange("(kc p) n -> p kc n", p=128)
    for g in range(ncols // CW):
        buf = wt[g % 2]
        S.dma('sp', buf[:], wv[:, :, g * CW:(g + 1) * CW], writes=[('wt', g % 2)])
        for jj in range(CW // 128):
            j = g * (CW // 128) + jj
            for kc in range(KC):
                S.op('pe', lambda e: e.matmul(pm[:, j:j + 1], buf[:, kc, jj * 128:(jj + 1) * 128], cond[:, kc:kc + 1],
                                              start=(kc == 0), stop=(kc == KC - 1)),
                     reads=[('wt', g % 2), 'cond'], writes=['pm'])
    S.op('dve', lambda e: e.tensor_tensor(out=ot[:], in0=pm[:, 0:nb], in1=bt[:], op=ALU.add), reads=['pm', 'bt'], writes=['ot'])
    ev = S.dma('sp', out, ot[:], reads=['ot'])
    S.finish([ev])
    return nc


def run_mod(c, ada_w, ada_b):
    depth = ada_w.shape[0]
    ncols_all = depth * 6 * D
    ncols = ncols_all // NCORE
    nc = build_mod(ncols)
    cpk = vec_pk(c.reshape(-1))
    in_maps = []
    for i in range(NCORE):
        lo = i * ncols
        l = lo // (6 * D)
        off = lo % (6 * D)
        w = np.ascontiguousarray(ada_w[l][:, off:off + ncols])
        b = vec_pk(ada_b[l][off:off + ncols])
        in_maps.append({"c": cpk, "w": w, "b": b})
    res = _run(nc, in_maps)
    mod = np.concatenate([np.ascontiguousarray(r["out"].T).reshape(-1) for r in res]).reshape(depth, 6 * D)
    return mod


def emit_ln_tile(S, nc, T, zt, zkey, NT, eps, lnw, lnb, out_fn, ones, ps1, ps2, tmp, k1='ps1', k2='ps2'):
    sq, mean, msq, var, rstd, nmr, t1 = tmp
    for blk in range(KC):
        S.op('act', lambda e: e.activation(out=sq[blk % 2][:], in_=zt[:, blk, :], func=AF.Square),
             reads=[(zkey, blk)], writes=[('sq', blk % 2)])
        S.op('pe', lambda e: e.matmul(ps1[:, 0:NT], ones[:], zt[:, blk, :], start=(blk == 0), stop=(blk == KC - 1)),
             reads=[(zkey, blk), 'ones'], writes=[k1])
        S.op('pe', lambda e: e.matmul(ps2[:, 0:NT], ones[:], sq[blk % 2][:], start=(blk == 0), stop=(blk == KC - 1)),
             reads=[('sq', blk % 2), 'ones'], writes=[k2])
    S.op('act', lambda e: e.activation(out=mean[:], in_=ps1[:, 0:NT], func=AF.Copy, scale=1.0 / D), reads=[k1], writes=['mean'])
    S.op('dve', lambda e: e.tensor_tensor(out=msq[:], in0=mean[:], in1=mean[:], op=ALU.mult), reads=['mean'], writes=['msq'])
    S.op('dve', lambda e: e.scalar_tensor_tensor(out=var[:], in0=ps2[:, 0:NT], scalar=1.0 / D, in1=msq[:], op0=ALU.mult, op1=ALU.subtract),
         reads=[k2, 'msq'], writes=['var'])
    S.op('dve', lambda e: e.tensor_scalar(out=var[:], in0=var[:], scalar1=eps, scalar2=None, op0=ALU.add), reads=['var'], writes=['var'])
    S.op('act', lambda e: e.activation(out=var[:], in_=var[:], func=AF.Ln), reads=['var'], writes=['var'])
    S.op('act', lambda e: e.activation(out=rstd[:], in_=var[:], func=AF.Exp, scale=-0.5), reads=['var'], writes=['rstd'])
    S.op('dve', lambda e: e.scalar_tensor_tensor(out=nmr[:], in0=mean[:], scalar=-1.0, in1=rstd[:], op0=ALU.mult, op1=ALU.mult),
         reads=['mean', 'rstd'], writes=['nmr'])
    for blk in range(KC):
        tt = t1[blk % 2]
        S.op('pool', lambda e: e.tensor_tensor(out=tt[:], in0=zt[:, blk, :], in1=rstd[:], op=ALU.mult),
             reads=[(zkey, blk), 'rstd'], writes=[('t1', blk % 2)])
        S.op('dve', lambda e: e.tensor_tensor(out=tt[:], in0=tt[:], in1=nmr[:], op=ALU.add),
             reads=[('t1', blk % 2), 'nmr'], writes=[('t1', blk % 2)])
        out_fn(blk, tt, ('t1', blk % 2))


def alloc_ln_tmp(nc, NT):
    sq = [nc.alloc_sbuf_tensor("ln_sq%d" % i, [128, NT], F32) for i in range(2)]
    t1 = [nc.alloc_sbuf_tensor("ln_t1%d" % i, [128, NT], F32) for i in range(2)]
    names = ["mean", "msq", "var", "rstd", "nmr"]
    ts = [nc.alloc_sbuf_tensor("ln_" + n, [128, NT], F32) for n in names]
    return (sq, ts[0], ts[1], ts[2], ts[3], ts[4], t1)


def build_outln(VD, TK, NT=256):
    nc = bass.Bass("TRN2", target_bir_lowering=False)
    VC = VD // 128
    ogT = nc.dram_tensor("ogT", [VD, TK], F32, kind="ExternalInput").ap()
    hT = nc.dram_tensor("hT", [D, TK], F32, kind="ExternalInput").ap()
    wout = nc.dram_tensor("wout", [KC, 128, VD], F32, kind="ExternalInput").ap()
    vecs = nc.dram_tensor("vecs", [128, 3, KC], F32, kind="ExternalInput").ap()
    out = nc.dram_tensor("out", [D, TK], F32, kind="ExternalOutput").ap()
    S = Sched(nc)
    vt = nc.alloc_sbuf_tensor("vt", [128, 3, KC], F32)
    gs = nc.alloc_sbuf_tensor("gs", [128, KC], F32)
    ones = nc.alloc_sbuf_tensor("ones", [128, 128], F32)
    ogt = nc.alloc_sbuf_tensor("ogt", [128, VC, NT], F32R)
    zt = nc.alloc_sbuf_tensor("zt", [128, KC, NT], F32)
    ot = nc.alloc_sbuf_tensor("ot", [128, KC, NT], F32)
    wb = [nc.alloc_sbuf_tensor("wb%d" % i, [128, VC, 128], F32R) for i in range(2)]
    tmp = alloc_ln_tmp(nc, NT)
    py = [nc.alloc_psum_tensor("py%d" % i, [128, 512], F32) for i in range(2)]
    ps1 = nc.alloc_psum_tensor("ps1", [128, 512], F32)
    ps2 = nc.alloc_psum_tensor("ps2", [128, 512], F32)
    S.dma('sp', vt[:], vecs, writes=['vt'])
    S.op('dve', lambda e: e.memset(ones[:], 1.0), writes=['ones'])
    S.op('dve', lambda e: e.tensor_scalar(out=gs[:], in0=vt[:, 0, :], scalar1=1.0, scalar2=1.0 / ALPHA, op0=ALU.add, op1=ALU.mult),
         reads=['vt'], writes=['gs'])
    ogv = ogT.rearrange("(kc p) t -> p kc t", p=128)
    hv = hT.rearrange("(kc p) t -> p kc t", p=128)
    ov = out.rearrange("(kc p) t -> p kc t", p=128)
    evs = []
    for t in range(TK // NT):
        ts = slice(t * NT, (t + 1) * NT)
        S.dma('pool', ogt[:], ogv[:, :, ts], writes=['ogt'])
        S.dma('sp', zt[:], hv[:, :, ts], writes=[('zt', b) for b in range(KC)])
        for blk in range(KC):
            w = wb[blk % 2]
            S.dma('pool', w[:].rearrange("p k c -> p (k c)"), wout[blk], writes=[('wb', blk % 2)], max_dma_last_dim=8192)
            p = py[blk % 2]
            for kc in range(VC):
                S.op('pe', lambda e: e.matmul(p[:, 0:NT], w[:, kc, :], ogt[:, kc, :], start=(kc == 0), stop=(kc == VC - 1)),
                     reads=[('wb', blk % 2), 'ogt'], writes=[('py', blk % 2)])
            S.op('dve', lambda e: e.scalar_tensor_tensor(out=zt[:, blk, :], in0=p[:, 0:NT], scalar=gs[:, blk:blk + 1], in1=zt[:, blk, :],
                                                          op0=ALU.mult, op1=ALU.add),
                 reads=[('py', blk % 2), 'gs', ('zt', blk)], writes=[('zt', blk)])

        def out_fn(blk, tt, tkey):
            S.op('act', lambda e: e.activation(out=ot[:, blk, :], in_=tt[:], func=AF.Identity, scale=vt[:, 1, blk:blk + 1], bias=vt[:, 2, blk:blk + 1]),
                 reads=[tkey, 'vt'], writes=[('ot', blk)])
        emit_ln_tile(S, nc, t, zt, 'zt', NT, EPS / (ALPHA * ALPHA), None, None, out_fn, ones, ps1, ps2, tmp)
        evs.append(S.dma('sp', ov[:, :, ts], ot[:], reads=[('ot', b) for b in range(KC)]))
    S.finish(evs)
    return nc


def run_outln(ogT_full, hT_full, w_out, gate, lnw, lnb, TK=None):
    VD, T = ogT_full.shape
    TK = T // NCORE
    nc = build_outln(VD, TK)
    vecs = np.ascontiguousarray(np.stack([vec_pk(gate), vec_pk(lnw), vec_pk(lnb)], axis=1))
    w_blk = blk_cols(w_out)
    in_maps = []
    for i in range(NCORE):
        in_maps.append({"ogT": np.ascontiguousarray(ogT_full[:, i * TK:(i + 1) * TK]),
                        "hT": np.ascontiguousarray(hT_full[:, i * TK:(i + 1) * TK]),
                        "wout": w_blk, "vecs": vecs})
    res = _run(nc, in_maps)
    return np.concatenate([r["out"] for r in res], axis=1)


def make_affine(S, out_tile_ap, key, ones_ap, pattern, base, cm, cmp, fill=0.0):
    S.op('pool', lambda e: e.affine_select(out=out_tile_ap, in_=ones_ap, pattern=pattern, compare_op=cmp, fill=fill,
                                           base=base, channel_multiplier=cm),
         reads=['ones'], writes=[key])


NE = 64; NG = 8; EPG = 8; DFF = 256


def build_moe(TK, NT=256, EPG=8, grouped=False):
    NE = EPG if grouped else NG * EPG
    n_exp = NE
    NT = min(NT, TK)
    nc = bass.Bass("TRN2", target_bir_lowering=False)
    h1T = nc.dram_tensor("h1T", [D, TK], F32, kind="ExternalInput").ap()
    vecs = nc.dram_tensor("vecs", [128, 5, KC], F32, kind="ExternalInput").ap()
    wr = nc.dram_tensor("wr", [D, NG + NE], F32, kind="ExternalInput").ap()
    wg = nc.dram_tensor("wg", [NE, 2, 128, KC * 128], F32, kind="ExternalInput").ap()
    wu = nc.dram_tensor("wu", [NE, 2, 128, KC * 128], F32, kind="ExternalInput").ap()
    wd = nc.dram_tensor("wd", [NE, DFF, D], F32, kind="ExternalInput").ap()
    out = nc.dram_tensor("out", [D, TK], F32, kind="ExternalOutput").ap()
    S = Sched(nc)
    NR = NG + NE
    vt = nc.alloc_sbuf_tensor("vt", [128, 5, KC], F32)
    s2 = nc.alloc_sbuf_tensor("s2", [128, KC], F32)
    gs = nc.alloc_sbuf_tensor("gs", [128, KC], F32)
    ones = nc.alloc_sbuf_tensor("ones", [128, 128], F32)
    ident = nc.alloc_sbuf_tensor("ident", [128, 128], F32)
    wrt = nc.alloc_sbuf_tensor("wrt", [128, KC, NR], F32)
    A = nc.alloc_sbuf_tensor("A", [128, KC, NT], F32R)
    Af = A[:].bitcast(F32)
    Y = nc.alloc_sbuf_tensor("Y", [128, KC, NT], F32)
    wgb = [nc.alloc_sbuf_tensor("wgb%d" % i, [128, KC, 128], F32R) for i in range(2)]
    wub = [nc.alloc_sbuf_tensor("wub%d" % i, [128, KC, 128], F32R) for i in range(2)]
    wdb = [nc.alloc_sbuf_tensor("wdb%d" % i, [128, D], F32R) for i in range(2)]
    tmp = alloc_ln_tmp(nc, NT)
    osm = [nc.alloc_sbuf_tensor("osm%d" % i, [128, NT], F32) for i in range(2)]
    lgt = nc.alloc_sbuf_tensor("lgt", [128, NR], F32)
    sm = nc.alloc_sbuf_tensor("sm", [128, 16], F32)
    ohg = nc.alloc_sbuf_tensor("ohg", [128, NG], F32)
    eg = nc.alloc_sbuf_tensor("eg", [128, NG], F32)
    lm = nc.alloc_sbuf_tensor("lm", [128, NG, EPG], F32)
    lm2 = nc.alloc_sbuf_tensor("lm2", [128, NE], F32)
    oh1 = nc.alloc_sbuf_tensor("oh1", [128, NE], F32)
    oh2 = nc.alloc_sbuf_tensor("oh2", [128, NE], F32)
    wts = nc.alloc_sbuf_tensor("wts", [128, NE], F32)
    wtsT = nc.alloc_sbuf_tensor("wtsT", [NE, NT], F32)
    rw = [nc.alloc_sbuf_tensor("rw%d" % i, [NE, NT], F32) for i in range(2)]
    sgt = [nc.alloc_sbuf_tensor("sgt%d" % i, [128, NT], F32) for i in range(2)]
    tgt = [nc.alloc_sbuf_tensor("tgt%d" % i, [128, NT], F32) for i in range(2)]
    actT = [nc.alloc_sbuf_tensor("actT%d" % i, [128, NT], F32R) for i in range(4)]
    PS = [nc.alloc_psum_tensor("ps%d" % i, [128, 512], F32) for i in range(8)]

    def pk(i):
        return ('ps', i)

    S.dma('sp', vt[:], vecs, writes=['vt'])
    S.dma('sp', wrt[:], wr.rearrange("(kc p) n -> p kc n", p=128), writes=['wrt'])
    S.op('dve', lambda e: e.memset(ones[:], 1.0), writes=['ones'])
    make_affine(S, ident[:], 'ident', ones[:], [[-1, 128]], 0, 1, ALU.is_equal)
    S.op('dve', lambda e: e.tensor_scalar(out=s2[:], in0=vt[:, 0, :], scalar1=1.0, scalar2=None, op0=ALU.add), reads=['vt'], writes=['s2'])
    S.op('dve', lambda e: e.tensor_scalar(out=gs[:], in0=vt[:, 2, :], scalar1=1.0, scalar2=1.0 / ALPHA, op0=ALU.add, op1=ALU.mult),
         reads=['vt'], writes=['gs'])
    hv = h1T.rearrange("(kc p) t -> p kc t", p=128)
    ov = out.rearrange("(kc p) t -> p kc t", p=128)
    evs = []
    Akeys = [('A', b) for b in range(KC)]
    wcount = [0]
    for t in range(TK // NT):
        ts = slice(t * NT, (t + 1) * NT)
        S.dma('sp', Y[:], hv[:, :, ts], writes=[('Y', b) for b in range(KC)])
        for blk in range(KC):
            S.op('act', lambda e: e.activation(out=A[:, blk, :], in_=Y[:, blk, :], func=AF.Identity, scale=s2[:, blk:blk + 1], bias=vt[:, 1, blk:blk + 1]),
                 reads=[('Y', blk), 's2', 'vt'], writes=[('A', blk)])
        for sub in range(NT // 128):
            ss = slice(sub * 128, (sub + 1) * 128)
            for kc in range(KC):
                S.op('pe', lambda e: e.matmul(PS[4][:, 0:NR], Af[:, kc, ss], wrt[:, kc, :], start=(kc == 0), stop=(kc == KC - 1)),
                     reads=[('A', kc), 'wrt'], writes=[pk(4)])
            S.op('act', lambda e: e.activation(out=lgt[:], in_=PS[4][:, 0:NR], func=AF.Copy), reads=[pk(4)], writes=['lgt'])
            R = ['lgt', 'sm', 'ohg', 'eg', 'lm', 'lm2', 'oh1', 'oh2', 'wts']
            def dv(fn):
                S.op('dve', fn, reads=R, writes=R)
            def ac(fn):
                S.op('act', fn, reads=R, writes=R)
            gm, ngm, sge, pgrp, m1, m2, dl, e2, wA, wB = [sm[:, i:i + 1] for i in range(10)]
            dv(lambda e: e.tensor_reduce(out=gm, in_=lgt[:, 0:NG], axis=AX.X, op=ALU.max))
            dv(lambda e: e.tensor_scalar(out=ngm, in0=gm, scalar1=-1.0, scalar2=None, op0=ALU.mult))
            ac(lambda e: e.activation(out=eg[:], in_=lgt[:, 0:NG], func=AF.Exp, bias=ngm, accum_out=sge))
            dv(lambda e: e.reciprocal(out=pgrp, in_=sge))
            if grouped:
                lmf = lm[:, 0, :]
                dv(lambda e: e.tensor_copy(out=lmf, in_=lgt[:, NG:NR]))
            else:
                dv(lambda e: e.tensor_scalar(out=ohg[:], in0=lgt[:, 0:NG], scalar1=gm, scalar2=None, op0=ALU.is_equal))
                dv(lambda e: e.tensor_scalar(out=ohg[:], in0=ohg[:], scalar1=-1.0, scalar2=30000.0, op0=ALU.add, op1=ALU.mult))
                dv(lambda e: e.tensor_tensor(out=lm[:], in0=lgt[:, NG:NR].rearrange("p (g x) -> p g x", g=NG),
                                             in1=ohg[:].unsqueeze(2).to_broadcast([128, NG, EPG]), op=ALU.add))
                lmf = lm[:].rearrange("p g x -> p (g x)")
            dv(lambda e: e.tensor_reduce(out=m1, in_=lmf, axis=AX.X, op=ALU.max))
            dv(lambda e: e.tensor_scalar(out=oh1[:], in0=lmf, scalar1=m1, scalar2=None, op0=ALU.is_equal))
            dv(lambda e: e.scalar_tensor_tensor(out=lm2[:], in0=oh1[:], scalar=-30000.0, in1=lmf, op0=ALU.mult, op1=ALU.add))
            dv(lambda e: e.tensor_reduce(out=m2, in_=lm2[:], axis=AX.X, op=ALU.max))
            dv(lambda e: e.tensor_scalar(out=oh2[:], in0=lm2[:], scalar1=m2, scalar2=None, op0=ALU.is_equal))
            dv(lambda e: e.tensor_tensor(out=dl, in0=m2, in1=m1, op=ALU.subtract))
            ac(lambda e: e.activation(out=e2, in_=dl, func=AF.Exp))
            dv(lambda e: e.tensor_scalar(out=wA, in0=e2, scalar1=1.0, scalar2=None, op0=ALU.add))
            dv(lambda e: e.reciprocal(out=wA, in_=wA))
            dv(lambda e: e.tensor_tensor(out=wA, in0=wA, in1=pgrp, op=ALU.mult))
            dv(lambda e: e.tensor_tensor(out=wB, in0=wA, in1=e2, op=ALU.mult))
            dv(lambda e: e.tensor_scalar(out=wts[:], in0=oh1[:], scalar1=wA, scalar2=None, op0=ALU.mult))
            dv(lambda e: e.scalar_tensor_tensor(out=wts[:], in0=oh2[:], scalar=wB, in1=wts[:], op0=ALU.mult, op1=ALU.add))
            S.op('pe', lambda e: e.transpose(PS[4][0:NE, 0:128], wts[:], ident[:]), reads=R + ['ident'], writes=[pk(4)])
            S.op('act', lambda e: e.activation(out=wtsT[:, ss], in_=PS[4][0:NE, 0:128], func=AF.Copy), reads=[pk(4)], writes=['wtsT'])
        for ex in range(n_exp):
            r = rw[ex % 2]
            S.op('dve', lambda e: e.tensor_scalar(out=r[:], in0=wtsT[:], scalar1=ident[0:NE, ex:ex + 1], scalar2=None, op0=ALU.mult),
                 reads=['wtsT', 'ident'], writes=[('rw', ex % 2)])
            S.op('pe', lambda e: e.matmul(PS[4][:, 0:NT], ones[0:NE, :], r[:], start=True, stop=True),
                 reads=[('rw', ex % 2), 'ones'], writes=[pk(4)])
            for fb in range(2):
                wi = wcount[0] % 2
                wcount[0] += 1
                S.dma('pool', wgb[wi][:].rearrange("p k c -> p (k c)"), wg[ex, fb], writes=[('wgb', wi)], max_dma_last_dim=8192)
                S.dma('pool', wub[wi][:].rearrange("p k c -> p (k c)"), wu[ex, fb], writes=[('wub', wi)], max_dma_last_dim=8192)
                S.dma('pool', wdb[wi][:], wd[ex, fb * 128:(fb + 1) * 128, :], writes=[('wdb', wi)], max_dma_last_dim=8192)
                pg = PS[0 + wi]; pu = PS[2 + wi]
                for kc in range(KC):
                    S.op('pe', lambda e: e.matmul(pg[:, 0:NT], wgb[wi][:, kc, :], A[:, kc, :], start=(kc == 0), stop=(kc == KC - 1)),
                         reads=[('wgb', wi), ('A', kc)], writes=[pk(0 + wi)])
                for kc in range(KC):
                    S.op('pe', lambda e: e.matmul(pu[:, 0:NT], wub[wi][:, kc, :], A[:, kc, :], start=(kc == 0), stop=(kc == KC - 1)),
                         reads=[('wub', wi), ('A', kc)], writes=[pk(2 + wi)])
                S.op('act', lambda e: e.activation(out=sgt[wi][:], in_=pg[:, 0:NT], func=AF.Silu), reads=[pk(0 + wi)], writes=[('sgt', wi)])
                S.op('dve', lambda e: e.tensor_tensor(out=tgt[wi][:], in0=pu[:, 0:NT], in1=sgt[wi][:], op=ALU.mult),
                     reads=[pk(2 + wi), ('sgt', wi)], writes=[('tgt', wi)])
                ai = (ex % 2) * 2 + fb
                S.op('dve', lambda e: e.tensor_tensor(out=actT[ai][:], in0=PS[4][:, 0:NT], in1=tgt[wi][:], op=ALU.mult),
                     reads=[pk(4), ('tgt', wi)], writes=[('actT', ai)])
            for blk in range(KC):
                pyi = 5 + (blk % 2)
                for fb in range(2):
                    ai = (ex % 2) * 2 + fb
                    wi = (wcount[0] - 2 + fb) % 2
                    S.op('pe', lambda e: e.matmul(PS[pyi][:, 0:NT], wdb[wi][:, blk * 128:(blk + 1) * 128], actT[ai][:], start=(fb == 0), stop=(fb == 1)),
                         reads=[('wdb', wi), ('actT', ai)], writes=[pk(pyi)])
                if ex == 0:
                    S.op('dve', lambda e: e.tensor_copy(out=Y[:, blk, :], in_=PS[pyi][:, 0:NT]), reads=[pk(pyi)], writes=[('Y', blk)])
                else:
                    S.op('dve', lambda e: e.tensor_tensor(out=Y[:, blk, :], in0=PS[pyi][:, 0:NT], in1=Y[:, blk, :], op=ALU.add),
                         reads=[pk(pyi), ('Y', blk)], writes=[('Y', blk)])
        for blk in range(KC):
            hs = sgt[blk % 2]
            S.dma('sp', hs[:], hv[:, blk, ts], writes=[('sgt', blk % 2)])
            S.op('dve', lambda e: e.scalar_tensor_tensor(out=Y[:, blk, :], in0=Y[:, blk, :], scalar=gs[:, blk:blk + 1], in1=hs[:],
                                                          op0=ALU.mult, op1=ALU.add),
                 reads=[('Y', blk), ('sgt', blk % 2), 'gs'], writes=[('Y', blk)])

        def out_fn(blk, tt, tkey):
            o = osm[blk % 2]
            S.op('act', lambda e: e.activation(out=o[:], in_=tt[:], func=AF.Identity, scale=vt[:, 3, blk:blk + 1], bias=vt[:, 4, blk:blk + 1]),
                 reads=[tkey, 'vt'], writes=[('osm', blk % 2)])
            evs.append(S.dma('sp', ov[:, blk, ts], o[:], reads=[('osm', blk % 2)]))
        emit_ln_tile(S, nc, t, Y, 'Y', NT, EPS / (ALPHA * ALPHA), None, None, out_fn, ones, PS[0], PS[2], tmp, k1=('ps', 0), k2=('ps', 2))
    S.finish(evs)
    return nc


def run_moe(h1T_full, scale2, shift2, gate2, lnw, lnb, w_group, w_expert, w_gate, w_up, w_down):
    T = h1T_full.shape[1]
    TK = T // NCORE
    nc = build_moe(TK, EPG=w_expert.shape[1] // NG)
    vecs = np.ascontiguousarray(np.stack([vec_pk(scale2), vec_pk(shift2), vec_pk(gate2), vec_pk(lnw), vec_pk(lnb)], axis=1))
    wr = np.ascontiguousarray(np.concatenate([w_group, w_expert], axis=1))
    in_maps = []
    for i in range(NCORE):
        in_maps.append({"h1T": np.ascontiguousarray(h1T_full[:, i * TK:(i + 1) * TK]), "vecs": vecs, "wr": wr,
                        "wg": np.stack([blk_cols(w_gate[e]) for e in range(w_gate.shape[0])]),
                        "wu": np.stack([blk_cols(w_up[e]) for e in range(w_up.shape[0])]), "wd": w_down})
    res = _run(nc, in_maps)
    return np.concatenate([r["out"] for r in res], axis=1)


def build_hgrn(T, HPC=4, NT=512):
    nc = bass.Bass("TRN2", target_bir_lowering=False)
    NCB = 4 * HPC
    hT = nc.dram_tensor("hT", [D, T], F32, kind="ExternalInput").ap()
    vecs = nc.dram_tensor("vecs", [128, 2, KC], F32, kind="ExternalInput").ap()
    w = nc.dram_tensor("w", [D, NCB * 128], F32, kind="ExternalInput").ap()
    lbl = nc.dram_tensor("lbl", [128, 2, HPC], F32, kind="ExternalInput").ap()
    nw_in = nc.dram_tensor("nw", [128, 1], F32, kind="ExternalInput").ap()
    out = nc.dram_tensor("out", [HPC * 128, T], F32, kind="ExternalOutput").ap()
    S = Sched(nc)
    vt = nc.alloc_sbuf_tensor("vt", [128, 2, KC], F32)
    s1 = nc.alloc_sbuf_tensor("s1", [128, KC], F32)
    lbt = nc.alloc_sbuf_tensor("lbt", [128, 2, HPC], F32)
    lb = nc.alloc_sbuf_tensor("lb", [128, HPC], F32)
    oml = nc.alloc_sbuf_tensor("oml", [128, HPC], F32)
    nw = nc.alloc_sbuf_tensor("nwt", [128, 1], F32)
    ones = nc.alloc_sbuf_tensor("ones", [128, 128], F32)
    ident = nc.alloc_sbuf_tensor("ident", [128, 128], F32)
    maskU = nc.alloc_sbuf_tensor("maskU", [128, 128], F32)
    Hs = nc.alloc_sbuf_tensor("Hs", [128, KC // 2, NT], F32)
    U = nc.alloc_sbuf_tensor("U", [128, KC, NT], F32R)
    wb = [nc.alloc_sbuf_tensor("wb%d" % i, [128, KC, 128], F32R) for i in range(2)]
    QT = nc.alloc_sbuf_tensor("QT", [128, HPC, NT], F32)
    KT = nc.alloc_sbuf_tensor("KT", [128, HPC, NT], F32)
    LF = nc.alloc_sbuf_tensor("LF", [128, HPC, NT], F32)
    VT = nc.alloc_sbuf_tensor("VT", [128, HPC, NT], F32)
    OG = nc.alloc_sbuf_tensor("OG", [128, HPC, NT], F32)
    OT = nc.alloc_sbuf_tensor("OT", [128, HPC, NT], F32)
    St = nc.alloc_sbuf_tensor("St", [128, HPC, 128], F32)
    sgm = nc.alloc_sbuf_tensor("sgm", [128, NT], F32)
    NB = 2
    def mk(name):
        return [nc.alloc_sbuf_tensor("%s%d" % (name, i), [128, 128], F32) for i in range(NB)]
    B_, E1, Qt, Kt, Qs, KdT, At, V_, Kd, On, Jk = mk("B"), mk("E1"), mk("Qt"), mk("Kt"), mk("Qs"), mk("KdT"), mk("At"), mk("V"), mk("Kd"), mk("On"), mk("Jk")
    sc = [nc.alloc_sbuf_tensor("sc%d" % i, [128, 8], F32) for i in range(NB)]
    PS = [nc.alloc_psum_tensor("ps%d" % i, [128, 512], F32) for i in range(8)]
    pk = lambda i: ('ps', i)

    S.dma('sp', vt[:], vecs, writes=['vt'])
    S.dma('sp', lbt[:], lbl, writes=['lbt'])
    S.dma('sp', nw[:], nw_in, writes=['nw'])
    S.op('dve', lambda e: e.memset(ones[:], 1.0), writes=['ones'])
    S.op('dve', lambda e: e.memset(St[:], 0.0), writes=[('S', h) for h in range(HPC)])
    make_affine(S, ident[:], 'ident', ones[:], [[-1, 128]], 0, 1, ALU.is_equal)
    make_affine(S, maskU[:], 'maskU', ones[:], [[1, 128]], 0, -1, ALU.is_ge)
    S.op('dve', lambda e: e.tensor_scalar(out=s1[:], in0=vt[:, 0, :], scalar1=1.0, scalar2=None, op0=ALU.add), reads=['vt'], writes=['s1'])
    S.op('dve', lambda e: e.tensor_tensor(out=lb[:], in0=lbt[:, 0, :], in1=lbt[:, 1, :], op=ALU.subtract), reads=['lbt'], writes=['lb'])
    S.op('act', lambda e: e.activation(out=lb[:], in_=lb[:], func=AF.Exp), reads=['lb'], writes=['lb'])
    S.op('dve', lambda e: e.tensor_scalar(out=lb[:], in0=lb[:], scalar1=1.0, scalar2=None, op0=ALU.add), reads=['lb'], writes=['lb'])
    S.op('dve', lambda e: e.reciprocal(out=lb[:], in_=lb[:]), reads=['lb'], writes=['lb'])
    S.op('dve', lambda e: e.tensor_scalar(out=oml[:], in0=lb[:], scalar1=-1.0, scalar2=1.0, op0=ALU.mult, op1=ALU.add), reads=['lb'], writes=['oml'])

    hv = hT.rearrange("(kc p) t -> p kc t", p=128)
    wv = w.rearrange("(kc p) n -> p kc n", p=128)
    ov = out.rearrange("(h p) t -> p h t", p=128)
    evs = []
    cnt = [0]
    for t in range(T // NT):
        ts = slice(t * NT, (t + 1) * NT)
        for half in range(2):
            hb = half * (KC // 2)
            S.dma('sp', Hs[:], hv[:, hb:hb + KC // 2, ts], writes=[('Hs', b) for b in range(KC // 2)])
            for b in range(KC // 2):
                blk = hb + b
                S.op('act', lambda e: e.activation(out=U[:, blk, :], in_=Hs[:, b, :], func=AF.Identity, scale=s1[:, blk:blk + 1], bias=vt[:, 1, blk:blk + 1]),
                     reads=[('Hs', b), 's1', 'vt'], writes=[('U', blk)])
        for cb in range(NCB):
            hh, typ = cb // 4, cb % 4
            wi = cb % 2
            S.dma('pool', wb[wi][:], wv[:, :, cb * 128:(cb + 1) * 128], writes=[('wb', wi)])
            pp = PS[wi]
            for kc in range(KC):
                S.op('pe', lambda e: e.matmul(pp[:, 0:NT], wb[wi][:, kc, :], U[:, kc, :], start=(kc == 0), stop=(kc == KC - 1)),
                     reads=[('wb', wi), ('U', kc)], writes=[pk(wi)])
            if typ == 0:
                S.op('act', lambda e: e.activation(out=QT[:, hh, :], in_=pp[:, 0:NT], func=AF.Silu), reads=[pk(wi)], writes=[('QT', hh)])
            elif typ == 1:
                S.op('act', lambda e: e.activation(out=sgm[:], in_=pp[:, 0:NT], func=AF.Sigmoid), reads=[pk(wi)], writes=['sgm'])
                S.op('dve', lambda e: e.tensor_scalar(out=sgm[:], in0=sgm[:], scalar1=oml[:, hh:hh + 1], scalar2=lb[:, hh:hh + 1], op0=ALU.mult, op1=ALU.add),
                     reads=['sgm', 'oml', 'lb'], writes=['sgm'])
                S.op('act', lambda e: e.activation(out=LF[:, hh, :], in_=sgm[:], func=AF.Ln), reads=['sgm'], writes=[('LF', hh)])
                S.op('dve', lambda e: e.tensor_scalar(out=KT[:, hh, :], in0=sgm[:], scalar1=-1.0, scalar2=1.0, op0=ALU.mult, op1=ALU.add),
                     reads=['sgm'], writes=[('KT', hh)])
            elif typ == 2:
                S.op('act', lambda e: e.activation(out=VT[:, hh, :], in_=pp[:, 0:NT], func=AF.Copy), reads=[pk(wi)], writes=[('VT', hh)])
            else:
                S.op('act', lambda e: e.activation(out=OG[:, hh, :], in_=pp[:, 0:NT], func=AF.Silu), reads=[pk(wi)], writes=[('OG', hh)])
        for c in range(NT // 128):
            cs = slice(c * 128, (c + 1) * 128)
            for hh in range(HPC):
                i = cnt[0] % NB
                cnt[0] += 1
                k = lambda n: (n, i)
                bref, nbref, blast, eblast, ssq, rstd = [sc[i][:, j:j + 1] for j in range(6)]
                S.op('dve', lambda e: e.tensor_tensor_scan(out=B_[i][:], data0=ones[:], data1=LF[:, hh, cs], initial=0.0, op0=ALU.mult, op1=ALU.add),
                     reads=[('LF', hh), 'ones'], writes=[k('B')])
                S.op('dve', lambda e: e.tensor_copy(out=bref, in_=B_[i][:, 63:64]), reads=[k('B')], writes=[k('sc')])
                S.op('dve', lambda e: e.tensor_scalar(out=nbref, in0=B_[i][:, 63:64], scalar1=-1.0, scalar2=None, op0=ALU.mult), reads=[k('B'), k('sc')], writes=[k('sc')])
                S.op('dve', lambda e: e.tensor_copy(out=blast, in_=B_[i][:, 127:128]), reads=[k('B'), k('sc')], writes=[k('sc')])
                S.op('act', lambda e: e.activation(out=eblast, in_=blast, func=AF.Exp), reads=[k('sc')], writes=[k('sc')])
                S.op('act', lambda e: e.activation(out=E1[i][:], in_=B_[i][:], func=AF.Exp, bias=nbref), reads=[k('B'), k('sc')], writes=[k('E1')])
                S.op('dve', lambda e: e.tensor_tensor(out=Qt[i][:], in0=QT[:, hh, cs], in1=E1[i][:], op=ALU.mult), reads=[('QT', hh), k('E1')], writes=[k('Qt')])
                S.op('act', lambda e: e.activation(out=E1[i][:], in_=B_[i][:], func=AF.Exp, scale=-1.0, bias=bref), reads=[k('B'), k('sc'), k('Qt')], writes=[k('E1')])
                S.op('dve', lambda e: e.tensor_tensor(out=Kt[i][:], in0=KT[:, hh, cs], in1=E1[i][:], op=ALU.mult), reads=[('KT', hh), k('E1')], writes=[k('Kt')])
                S.op('act', lambda e: e.activation(out=E1[i][:], in_=B_[i][:], func=AF.Exp), reads=[k('B'), k('Kt')], writes=[k('E1')])
                S.op('dve', lambda e: e.tensor_tensor(out=Qs[i][:], in0=QT[:, hh, cs], in1=E1[i][:], op=ALU.mult), reads=[('QT', hh), k('E1')], writes=[k('Qs')])
                S.op('act', lambda e: e.activation(out=E1[i][:], in_=B_[i][:], func=AF.Exp, scale=-1.0, bias=blast), reads=[k('B'), k('sc'), k('Qs')], writes=[k('E1')])
                S.op('dve', lambda e: e.tensor_tensor(out=KdT[i][:], in0=KT[:, hh, cs], in1=E1[i][:], op=ALU.mult), reads=[('KT', hh), k('E1')], writes=[k('KdT')])
                S.op('pe', lambda e: e.matmul(PS[2][:, 0:128], Kt[i][:], Qt[i][:], start=True, stop=True), reads=[k('Kt'), k('Qt')], writes=[pk(2)])
                S.op('dve', lambda e: e.tensor_scalar(out=At[i][:], in0=PS[2][:, 0:128], scalar1=-1e30, scalar2=1e30, op0=ALU.max, op1=ALU.min),
                     reads=[pk(2)], writes=[k('At')])
                S.op('dve', lambda e: e.tensor_tensor(out=At[i][:], in0=At[i][:], in1=maskU[:], op=ALU.mult), reads=[k('At'), 'maskU'], writes=[k('At')])
                S.op('pe', lambda e: e.transpose(PS[3][:, 0:128], VT[:, hh, cs], ident[:]), reads=[('VT', hh), 'ident'], writes=[pk(3)])
                S.op('act', lambda e: e.activation(out=V_[i][:], in_=PS[3][:, 0:128], func=AF.Copy), reads=[pk(3)], writes=[k('V')])
                S.op('pe', lambda e: e.transpose(PS[4][:, 0:128], KdT[i][:], ident[:]), reads=[k('KdT'), 'ident'], writes=[pk(4)])
                S.op('dve', lambda e: e.tensor_copy(out=Kd[i][:], in_=PS[4][:, 0:128]), reads=[pk(4)], writes=[k('Kd')])
                S.op('pe', lambda e: e.matmul(PS[5][:, 0:128], At[i][:], V_[i][:], start=True, stop=False), reads=[k('At'), k('V')], writes=[pk(5)])
                S.op('pe', lambda e: e.matmul(PS[5][:, 0:128], Qs[i][:], St[:, hh, :], start=False, stop=True), reads=[k('Qs'), ('S', hh)], writes=[pk(5)])
                S.op('pe', lambda e: e.matmul(PS[6][:, 0:128], Kd[i][:], V_[i][:], start=True, stop=True), reads=[k('Kd'), k('V')], writes=[pk(6)])
                S.op('dve', lambda e: e.scalar_tensor_tensor(out=St[:, hh, :], in0=St[:, hh, :], scalar=eblast, in1=PS[6][:, 0:128], op0=ALU.mult, op1=ALU.add),
                     reads=[('S', hh), k('sc'), pk(6)], writes=[('S', hh)])
                S.op('act', lambda e: e.activation(out=Jk[i][:], in_=PS[5][:, 0:128], func=AF.Square, accum_out=ssq), reads=[pk(5), k('sc')], writes=[k('Jk'), k('sc')])
                S.op('dve', lambda e: e.tensor_scalar(out=ssq, in0=ssq, scalar1=1.0 / 128, scalar2=EPS, op0=ALU.mult, op1=ALU.add), reads=[k('sc')], writes=[k('sc')])
                S.op('act', lambda e: e.activation(out=ssq, in_=ssq, func=AF.Ln), reads=[k('sc')], writes=[k('sc')])
                S.op('act', lambda e: e.activation(out=rstd, in_=ssq, func=AF.Exp, scale=-0.5), reads=[k('sc')], writes=[k('sc')])
                S.op('dve', lambda e: e.tensor_scalar(out=On[i][:], in0=PS[5][:, 0:128], scalar1=rstd, scalar2=None, op0=ALU.mult), reads=[pk(5), k('sc')], writes=[k('On')])
                S.op('pe', lambda e: e.transpose(PS[7][:, 0:128], On[i][:], ident[:]), reads=[k('On'), 'ident'], writes=[pk(7)])
                S.op('dve', lambda e: e.scalar_tensor_tensor(out=OT[:, hh, cs], in0=PS[7][:, 0:128], scalar=nw[:, 0:1], in1=OG[:, hh, cs], op0=ALU.mult, op1=ALU.mult),
                     reads=[pk(7), 'nw', ('OG', hh)], writes=[('OT', hh)])
        evs.append(S.dma('sp', ov[:, :, ts], OT[:], reads=[('OT', h) for h in range(HPC)]))
    S.finish(evs)
    return nc


def run_hgrn(hT_full, scale1, shift1, w_in, lb_logits, norm_w):
    T = hT_full.shape[1]
    HPC = 32 // NCORE
    nc = build_hgrn(T, HPC=HPC)
    vecs = np.ascontiguousarray(np.stack([vec_pk(scale1), vec_pk(shift1)], axis=1))
    nwp = np.ascontiguousarray(norm_w.reshape(128, 1).astype(np.float32))
    in_maps = []
    for c in range(NCORE):
        cols = []
        for hh in range(HPC):
            h = c * HPC + hh
            for typ in range(4):
                cols.append(w_in[:, typ * D + h * 128: typ * D + (h + 1) * 128])
        wc = np.ascontiguousarray(np.concatenate(cols, axis=1))
        ch = slice(c * HPC * 128, (c + 1) * HPC * 128)
        lbl = np.ascontiguousarray(np.stack([vec_pk(lb_logits[0, ch]), vec_pk(lb_logits[1, ch])], axis=1))
        in_maps.append({"hT": hT_full, "vecs": vecs, "w": wc, "lbl": lbl, "nw": nwp})
    res = _run(nc, in_maps)
    return np.concatenate([r["out"] for r in res], axis=0)


def build_gdn(T, HV=8, NT=256, stage=9):
    nc = bass.Bass("TRN2", target_bir_lowering=False)
    HQ = HV // 2
    NCONV = 2 * HQ + HV
    NCB = NCONV + HV
    hT = nc.dram_tensor("hT", [D, T], F32, kind="ExternalInput").ap()
    vecs = nc.dram_tensor("vecs", [128, 2, KC], F32, kind="ExternalInput").ap()
    w = nc.dram_tensor("w", [NCB, 128, KC * 128], F32, kind="ExternalInput").ap()
    wab = nc.dram_tensor("wab", [D, 2 * HV], F32, kind="ExternalInput").ap()
    cw_in = nc.dram_tensor("cw", [128, NCONV, 4], F32, kind="ExternalInput").ap()
    hp_in = nc.dram_tensor("hp", [128, 2, HV], F32, kind="ExternalInput").ap()
    nw_in = nc.dram_tensor("nw", [128, 1], F32, kind="ExternalInput").ap()
    out = nc.dram_tensor("out", [HV * 128, T], F32, kind="ExternalOutput").ap()
    S = Sched(nc)
    A_ = nc.alloc_sbuf_tensor
    vt = A_("vt", [128, 2, KC], F32); s1 = A_("s1", [128, KC], F32)
    cw = A_("cwt", [128, NCONV, 4], F32); hp = A_("hpt", [128, 2, HV], F32); nA = A_("nA", [128, HV], F32)
    nw = A_("nwt", [128, 1], F32)
    ones = A_("ones", [128, 128], F32); ident = A_("ident", [128, 128], F32)
    mU = A_("mU", [128, 128], F32); nSL = A_("nSL", [128, 128], F32); nSU = A_("nSU", [128, 128], F32); nU = A_("nU", [128, 128], F32)
    Hs = A_("Hs", [128, KC // 2, NT], F32); U = A_("U", [128, KC, NT], F32R)
    wb = [A_("wb%d" % i, [128, KC, 128], F32R) for i in range(2)]
    wabt = A_("wabt", [128, KC, 2 * HV], F32R)
    pre = [A_("pre%d" % i, [128, NT + 3], F32) for i in range(2)]
    halo = A_("halo", [128, NCONV, 3], F32)
    acc = [A_("acc%d" % i, [128, NT], F32) for i in range(2)]
    xs = [A_("xs%d" % i, [128, NT], F32) for i in range(2)]
    sq = acc
    QT = A_("QT", [128, HQ, NT], F32); KT = A_("KT", [128, HQ, NT], F32)
    VT = A_("VT", [128, HV, NT], F32); SZ = A_("SZ", [128, HV, NT], F32); OT = A_("OT", [128, HV, NT], F32)
    St = A_("St", [128, HV, 128], F32)
    GS = {n: A_("gs_" + n, [128, HV], F32) for n in ("emb", "den", "beta", "lnb", "tt", "g", "gc", "gl", "glb", "ngc", "egc", "bgc", "ekd", "egl")}
    NB = 2
    def mk(name, n=NB, w_=128):
        return [A_("%s%d" % (name, i), [128, w_], F32) for i in range(n)]
    KKs, KQs, Ktok = mk("KKs", HQ), mk("KQs", HQ), mk("Ktok", HQ)
    tA, tAT, tQ = mk("tA"), mk("tAT"), mk("tQ")
    Am, ATm, QKT = mk("Am", HV), mk("ATm", HV), mk("QKT", HV)
    Pm = [mk("Pm0", HV), mk("Pm1", HV)]; Qm = [mk("Qm0", HV), mk("Qm1", HV)]; Rm = [mk("Rm0", HV), mk("Rm1", HV)]
    kd, Us, WTs = mk("kd", HV), mk("Us", HV), mk("WTs", HV)
    vb, kbg, Os, On = mk("vb", HV), mk("kbg", HV), mk("Os", HV), mk("On", HV)
    vn, o1s = mk("vn"), mk("o1s")
    scs = A_("scs", [128, HV, 2], F32)
    sc = [A_("sc%d" % i, [128, 4], F32) for i in range(NB)]
    PS = [nc.alloc_psum_tensor("ps%d" % i, [128, 512], F32) for i in range(8)]
    pk = lambda i: ('ps', i)
    slots = [(b, 0) for b in range(3, 8)]
    sl_i = [0]
    def slot():
        s = slots[sl_i[0] % len(slots)]
        sl_i[0] += 1
        return s
    def sv(s):
        return PS[s[0]][:, s[1] * 128:(s[1] + 1) * 128]
    def sk(s):
        return ('ps', s[0])
    pair_i = [0]

    S.dma('sp', vt[:], vecs, writes=['vt'])
    S.dma('sp', cw[:], cw_in, writes=['cw'])
    S.dma('sp', hp[:], hp_in, writes=['hp'])
    S.dma('sp', nw[:], nw_in, writes=['nw'])
    S.dma('pool', wabt[:], wab.rearrange("(kc p) n -> p kc n", p=128), writes=['wabt'])
    S.op('dve', lambda e: e.memset(ones[:], 1.0), writes=['ones'])
    S.op('dve', lambda e: e.memset(St[:], 0.0), writes=[('S', h) for h in range(HV)])
    S.op('dve', lambda e: e.memset(halo[:], 0.0), writes=[('halo', b) for b in range(NCONV)])
    make_affine(S, ident[:], 'ident', ones[:], [[-1, 128]], 0, 1, ALU.is_equal)
    make_affine(S, mU[:], 'mU', ones[:], [[1, 128]], 0, -1, ALU.is_ge)
    make_affine(S, nU[:], 'nU', ones[:], [[1, 128]], 0, -1, ALU.is_ge)
    make_affine(S, nSU[:], 'nSU', ones[:], [[1, 128]], 0, -1, ALU.is_gt)
    make_affine(S, nSL[:], 'nSL', ones[:], [[-1, 128]], 0, 1, ALU.is_gt)
    for m_, k_ in ((nU, 'nU'), (nSU, 'nSU'), (nSL, 'nSL')):
        S.op('dve', lambda e: e.tensor_scalar(out=m_[:], in0=m_[:], scalar1=-1.0, scalar2=30000.0, op0=ALU.add, op1=ALU.mult), reads=[k_], writes=[k_])
    S.op('dve', lambda e: e.tensor_scalar(out=s1[:], in0=vt[:, 0, :], scalar1=1.0, scalar2=None, op0=ALU.add), reads=['vt'], writes=['s1'])
    S.op('act', lambda e: e.activation(out=nA[:], in_=hp[:, 0, :], func=AF.Exp), reads=['hp'], writes=['nA'])
    S.op('dve', lambda e: e.tensor_scalar(out=nA[:], in0=nA[:], scalar1=-1.0, scalar2=None, op0=ALU.mult), reads=['nA'], writes=['nA'])

    hv_ = hT.rearrange("(kc p) t -> p kc t", p=128)
    ov = out.rearrange("(h p) t -> p h t", p=128)
    evs = []
    cnt = [0]
    GK = ['gs']
    for t in range(T // NT):
        ts = slice(t * NT, (t + 1) * NT)
        for half in range(2):
            hb = half * (KC // 2)
            S.dma('sp', Hs[:], hv_[:, hb:hb + KC // 2, ts], writes=[('Hs', b) for b in range(KC // 2)])
            for b in range(KC // 2):
                blk = hb + b
                S.op('act', lambda e: e.activation(out=U[:, blk, :], in_=Hs[:, b, :], func=AF.Identity, scale=s1[:, blk:blk + 1], bias=vt[:, 1, blk:blk + 1]),
                     reads=[('Hs', b), 's1', 'vt'], writes=[('U', blk)])
        deferred = []
        for cb in range(NCB):
            wi = cb % 2
            S.dma('pool', wb[wi][:].rearrange("p k c -> p (k c)"), w[cb], writes=[('wb', wi)], max_dma_last_dim=8192)
            pp = PS[wi]
            for kc in range(KC):
                S.op('pe', lambda e: e.matmul(pp[:, 0:NT], wb[wi][:, kc, :], U[:, kc, :], start=(kc == 0), stop=(kc == KC - 1)),
                     reads=[('wb', wi), ('U', kc)], writes=[pk(wi)])
            while deferred:
                deferred.pop(0)()
            if cb >= NCONV:
                hv = cb - NCONV
                S.op('act', lambda e: e.activation(out=SZ[:, hv, :], in_=pp[:, 0:NT], func=AF.Silu), reads=[pk(wi)], writes=[('SZ', hv)])
                continue
            pr = pre[wi]; ac = acc[wi]
            S.op('dve', lambda e: e.tensor_copy(out=pr[:, 0:3], in_=halo[:, cb, :]), reads=[('halo', cb)], writes=[('pre', wi)])
            S.op('act', lambda e: e.activation(out=pr[:, 3:3 + NT], in_=pp[:, 0:NT], func=AF.Copy), reads=[pk(wi)], writes=[('pre', wi)])
            S.op('dve', lambda e: e.tensor_copy(out=halo[:, cb, :], in_=pr[:, NT:NT + 3]), reads=[('pre', wi)], writes=[('halo', cb)])
            S.op('dve', lambda e: e.tensor_scalar(out=ac[:], in0=pr[:, 0:NT], scalar1=cw[:, cb, 0:1], scalar2=None, op0=ALU.mult),
                 reads=[('pre', wi), 'cw'], writes=[('acc', wi)])
            for j in (1, 2, 3):
                S.op('dve', lambda e: e.scalar_tensor_tensor(out=ac[:], in0=pr[:, j:j + NT], scalar=cw[:, cb, j:j + 1], in1=ac[:], op0=ALU.mult, op1=ALU.add),
                     reads=[('pre', wi), 'cw', ('acc', wi)], writes=[('acc', wi)])
            if cb >= 2 * HQ:
                hv = cb - 2 * HQ
                S.op('act', lambda e: e.activation(out=VT[:, hv, :], in_=ac[:], func=AF.Silu), reads=[('acc', wi)], writes=[('VT', hv)])
                continue
            x = xs[wi]; s_ = sq[wi]
            S.op('act', lambda e: e.activation(out=x[:], in_=ac[:], func=AF.Silu), reads=[('acc', wi)], writes=[('xs', wi)])
            S.op('act', lambda e: e.activation(out=s_[:], in_=x[:], func=AF.Square), reads=[('xs', wi)], writes=[('acc', wi)])
            def l2tail(cb=cb, wi=wi, x=x, s_=s_):
                S.op('pe', lambda e: e.matmul(PS[2][:, 0:NT], ones[:], s_[:], start=True, stop=True), reads=[('acc', wi), 'ones'], writes=[pk(2)])
                S.op('dve', lambda e: e.tensor_scalar(out=s_[:], in0=PS[2][:, 0:NT], scalar1=EPS, scalar2=None, op0=ALU.add), reads=[pk(2)], writes=[('acc', wi)])
                S.op('act', lambda e: e.activation(out=s_[:], in_=s_[:], func=AF.Ln), reads=[('acc', wi)], writes=[('acc', wi)])
                S.op('act', lambda e: e.activation(out=s_[:], in_=s_[:], func=AF.Exp, scale=-0.5), reads=[('acc', wi)], writes=[('acc', wi)])
                if cb < HQ:
                    S.op('dve', lambda e: e.scalar_tensor_tensor(out=QT[:, cb, :], in0=x[:], scalar=128.0 ** -0.5, in1=s_[:], op0=ALU.mult, op1=ALU.mult),
                         reads=[('xs', wi), ('acc', wi)], writes=[('QT', cb)])
                else:
                    S.op('dve', lambda e: e.tensor_tensor(out=KT[:, cb - HQ, :], in0=x[:], in1=s_[:], op=ALU.mult),
                         reads=[('xs', wi), ('acc', wi)], writes=[('KT', cb - HQ)])
            deferred.append(l2tail)
        while deferred:
            deferred.pop(0)()
        for c in range(NT // 128 if stage >= 1 else 0):
            cs = slice(c * 128, (c + 1) * 128)
            for kc in range(KC):
                S.op('pe', lambda e: e.matmul(PS[2][:, 0:2 * HV], U[:, kc, cs], wabt[:, kc, :], start=(kc == 0), stop=(kc == KC - 1)),
                     reads=[('U', kc), 'wabt'], writes=[pk(2)])
            G = GS
            def dv(fn, extra=()):
                S.op('dve', fn, reads=GK + list(extra), writes=GK)
            def ac_(fn, extra=()):
                S.op('act', fn, reads=GK + list(extra), writes=GK)
            ac_(lambda e: e.activation(out=G['emb'][:], in_=PS[2][:, HV:2 * HV], func=AF.Exp, scale=-1.0), [pk(2)])
            dv(lambda e: e.tensor_scalar(out=G['den'][:], in0=G['emb'][:], scalar1=1.0, scalar2=None, op0=ALU.add))
            dv(lambda e: e.reciprocal(out=G['beta'][:], in_=G['den'][:]))
            ac_(lambda e: e.activation(out=G['lnb'][:], in_=G['den'][:], func=AF.Ln))
            dv(lambda e: e.tensor_scalar(out=G['lnb'][:], in0=G['lnb'][:], scalar1=-1.0, scalar2=None, op0=ALU.mult))
            dv(lambda e: e.tensor_tensor(out=G['tt'][:], in0=PS[2][:, 0:HV], in1=hp[:, 1, :], op=ALU.add), [pk(2), 'hp'])
            ac_(lambda e: e.activation(out=G['tt'][:], in_=G['tt'][:], func=AF.Exp))
            dv(lambda e: e.tensor_scalar(out=G['tt'][:], in0=G['tt'][:], scalar1=1.0, scalar2=None, op0=ALU.add))
            ac_(lambda e: e.activation(out=G['tt'][:], in_=G['tt'][:], func=AF.Ln))
            dv(lambda e: e.tensor_tensor(out=G['g'][:], in0=G['tt'][:], in1=nA[:], op=ALU.mult), ['nA'])
            S.op('pe', lambda e: e.matmul(PS[2][:, 32:32 + HV], mU[:], G['g'][:], start=True, stop=True), reads=GK + ['mU'], writes=[pk(2)])
            dv(lambda e: e.tensor_copy(out=G['gc'][:], in_=PS[2][:, 32:32 + HV]), [pk(2)])
            S.op('pe', lambda e: e.matmul(PS[2][:, 64:64 + HV], ones[:], G['g'][:], start=True, stop=True), reads=GK + ['ones'], writes=[pk(2)])
            dv(lambda e: e.tensor_copy(out=G['gl'][:], in_=PS[2][:, 64:64 + HV]), [pk(2)])
            dv(lambda e: e.tensor_tensor(out=G['glb'][:], in0=G['gc'][:], in1=G['lnb'][:], op=ALU.add))
            dv(lambda e: e.tensor_scalar(out=G['ngc'][:], in0=G['gc'][:], scalar1=-1.0, scalar2=None, op0=ALU.mult))
            ac_(lambda e: e.activation(out=G['egc'][:], in_=G['gc'][:], func=AF.Exp))
            dv(lambda e: e.tensor_tensor(out=G['bgc'][:], in0=G['egc'][:], in1=G['beta'][:], op=ALU.mult))
            dv(lambda e: e.tensor_tensor(out=G['ekd'][:], in0=G['gl'][:], in1=G['gc'][:], op=ALU.subtract))
            ac_(lambda e: e.activation(out=G['ekd'][:], in_=G['ekd'][:], func=AF.Exp))
            ac_(lambda e: e.activation(out=G['egl'][:], in_=G['gl'][:], func=AF.Exp))
            if stage < 2:
                continue
            col = lambda n, hv: GS[n][:, hv:hv + 1]
            for hq in range(HQ):
                kq = lambda n: (n, 'q', hq)
                s_kk, s_kq, s_kt = slot(), slot(), slot()
                S.op('pe', lambda e: e.matmul(sv(s_kk), KT[:, hq, cs], KT[:, hq, cs], start=True, stop=True), reads=[('KT', hq)], writes=[sk(s_kk)])
                S.op('pe', lambda e: e.matmul(sv(s_kq), KT[:, hq, cs], QT[:, hq, cs], start=True, stop=True), reads=[('KT', hq), ('QT', hq)], writes=[sk(s_kq)])
                S.op('pe', lambda e: e.transpose(sv(s_kt), KT[:, hq, cs], ident[:]), reads=[('KT', hq), 'ident'], writes=[sk(s_kt)])
                S.op('act', lambda e: e.activation(out=KKs[hq][:], in_=sv(s_kk), func=AF.Copy), reads=[sk(s_kk)], writes=[kq('KKs')])
                S.op('dve', lambda e: e.tensor_copy(out=KQs[hq][:], in_=sv(s_kq)), reads=[sk(s_kq)], writes=[kq('KQs')])
                S.op('act', lambda e: e.activation(out=Ktok[hq][:], in_=sv(s_kt), func=AF.Copy), reads=[sk(s_kt)], writes=[kq('Ktok')])
            for hv in range(HV):
                hq = hv // 2
                kq = lambda n: (n, 'q', hq)
                i = cnt[0] % NB
                cnt[0] += 1
                k = lambda n: (n, 'r', i)
                kh = lambda n: (n, 'h', hv)
                pb = pair_i[0] % 2
                pair_i[0] += 1
                bc1 = PS[2][:, 256 * pb:256 * pb + 128]
                bc2 = PS[2][:, 256 * pb + 128:256 * pb + 256]
                S.op('pe', lambda e: e.matmul(bc1, col('gc', hv).to_broadcast([128, 128]), ident[:], start=True, stop=True), reads=GK + ['ident'], writes=[pk(2)])
                S.op('pe', lambda e: e.matmul(bc2, col('glb', hv).to_broadcast([128, 128]), ident[:], start=True, stop=True), reads=GK + ['ident'], writes=[pk(2)])
                S.op('dve', lambda e: e.scalar_tensor_tensor(out=tA[i][:], in0=bc1, scalar=-1.0, in1=nSL[:], op0=ALU.mult, op1=ALU.add), reads=[pk(2), 'nSL'], writes=[k('tA')])
                S.op('act', lambda e: e.activation(out=tA[i][:], in_=tA[i][:], func=AF.Exp, bias=col('glb', hv)), reads=[k('tA')] + GK, writes=[k('tA')])
                S.op('dve', lambda e: e.tensor_tensor(out=tAT[i][:], in0=bc2, in1=nSU[:], op=ALU.add), reads=[pk(2), 'nSU'], writes=[k('tAT')])
                S.op('act', lambda e: e.activation(out=tAT[i][:], in_=tAT[i][:], func=AF.Exp, bias=col('ngc', hv)), reads=[k('tAT')] + GK, writes=[k('tAT')])
                S.op('dve', lambda e: e.tensor_tensor(out=tQ[i][:], in0=bc1, in1=nU[:], op=ALU.add), reads=[pk(2), 'nU'], writes=[k('tQ')])
                S.op('act', lambda e: e.activation(out=tQ[i][:], in_=tQ[i][:], func=AF.Exp, bias=col('ngc', hv)), reads=[k('tQ')] + GK, writes=[k('tQ')])
                S.op('pool', lambda e: e.tensor_tensor(out=Am[hv][:], in0=KKs[hq][:], in1=tA[i][:], op=ALU.mult), reads=[kq('KKs'), k('tA')], writes=[kh('Am')])
                S.op('pool', lambda e: e.tensor_tensor(out=ATm[hv][:], in0=KKs[hq][:], in1=tAT[i][:], op=ALU.mult), reads=[kq('KKs'), k('tAT')], writes=[kh('ATm')])
                S.op('pool', lambda e: e.tensor_tensor(out=QKT[hv][:], in0=KQs[hq][:], in1=tQ[i][:], op=ALU.mult), reads=[kq('KQs'), k('tQ')], writes=[kh('QKT')])
                S.op('pool', lambda e: e.tensor_tensor(out=Rm[0][hv][:], in0=ident[:], in1=ATm[hv][:], op=ALU.subtract), reads=['ident', kh('ATm')], writes=[kh('R0')])
            for hv in range(HV):
                hq = hv // 2
                kq = lambda n: (n, 'q', hq)
                kh = lambda n: (n, 'h', hv)
                s_v = slot()
                S.op('pe', lambda e: e.transpose(sv(s_v), VT[:, hv, cs], ident[:]), reads=[('VT', hv), 'ident'], writes=[sk(s_v)])
                S.op('act', lambda e: e.activation(out=vb[hv][:], in_=sv(s_v), func=AF.Identity, scale=col('beta', hv)), reads=[sk(s_v)] + GK, writes=[kh('vb')])
                S.op('act', lambda e: e.activation(out=kbg[hv][:], in_=Ktok[hq][:], func=AF.Identity, scale=col('bgc', hv)), reads=[kq('Ktok')] + GK, writes=[kh('kbg')])
                S.op('pool', lambda e: e.tensor_tensor(out=kd[hv][:], in0=Ktok[hq][:], in1=col('ekd', hv).to_broadcast([128, 128]), op=ALU.mult), reads=[kq('Ktok')] + GK, writes=[kh('kd')])
            if stage < 3:
                continue
            cur = {hv: (ATm[hv], ('ATm', 'h', hv), Am[hv], ('Am', 'h', hv), Rm[0][hv], ('R0', 'h', hv)) for hv in range(HV)}
            for m in range(1, 7):
                pi = m % 2
                nxt = {}
                for hv in range(HV):
                    kh = lambda n: (n, 'h', hv)
                    Pc, Pk, Qc, Qk, Rc, Rk = cur[hv]
                    s_q = slot()
                    S.op('pe', lambda e: e.matmul(sv(s_q), Pc[:], Qc[:], start=True, stop=True), reads=[Pk, Qk], writes=[sk(s_q)])
                    Qn = Qm[pi][hv]; Qnk = kh('Q%d' % pi)
                    S.op('act', lambda e: e.activation(out=Qn[:], in_=sv(s_q), func=AF.Copy), reads=[sk(s_q)], writes=[Qnk])
                    Pn, Pnk = Pc, Pk
                    if m < 6:
                        s_p = slot()
                        S.op('pe', lambda e: e.matmul(sv(s_p), Qc[:], Pc[:], start=True, stop=True), reads=[Pk, Qk], writes=[sk(s_p)])
                        Pn = Pm[pi][hv]; Pnk = kh('P%d' % pi)
                        S.op('dve', lambda e: e.tensor_copy(out=Pn[:], in_=sv(s_p)), reads=[sk(s_p)], writes=[Pnk])
                    nxt[hv] = (Pn, Pnk, Qn, Qnk)
                for hv in range(HV):
                    kh = lambda n: (n, 'h', hv)
                    Pn, Pnk, Qn, Qnk = nxt[hv]
                    Rc, Rk = cur[hv][4], cur[hv][5]
                    s_r = slot()
                    S.op('pe', lambda e: e.matmul(sv(s_r), Qn[:], Rc[:], start=True, stop=True), reads=[Qnk, Rk], writes=[sk(s_r)])
                    Rn = Rm[pi][hv]; Rnk = kh('R%d' % pi)
                    S.op('dve', lambda e: e.tensor_tensor(out=Rn[:], in0=sv(s_r), in1=Rc[:], op=ALU.add), reads=[sk(s_r), Rk], writes=[Rnk])
                    cur[hv] = (Pn, Pnk, Qn, Qnk, Rn, Rnk)
            if stage < 4:
                continue
            for hv in range(HV):
                kh = lambda n: (n, 'h', hv)
                Rc, Rk = cur[hv][4], cur[hv][5]
                s_u, s_w = slot(), slot()
                S.op('pe', lambda e: e.matmul(sv(s_u), Rc[:], vb[hv][:], start=True, stop=True), reads=[Rk, kh('vb')], writes=[sk(s_u)])
                S.op('pe', lambda e: e.matmul(sv(s_w), kbg[hv][:], Rc[:], start=True, stop=True), reads=[Rk, kh('kbg')], writes=[sk(s_w)])
                S.op('act', lambda e: e.activation(out=Us[hv][:], in_=sv(s_u), func=AF.Copy), reads=[sk(s_u)], writes=[kh('Us')])
                S.op('dve', lambda e: e.tensor_copy(out=WTs[hv][:], in_=sv(s_w)), reads=[sk(s_w)], writes=[kh('WTs')])
            if stage < 5:
                continue
            for hv in range(HV):
                hq = hv // 2
                kh = lambda n: (n, 'h', hv)
                i = cnt[0] % NB
                cnt[0] += 1
                k = lambda n: (n, 'r', i)
                s_vn, s_o1 = slot(), slot()
                S.op('pe', lambda e: e.matmul(sv(s_vn), WTs[hv][:], St[:, hv, :], start=True, stop=True), reads=[kh('WTs'), ('S', hv)], writes=[sk(s_vn)])
                S.op('pe', lambda e: e.matmul(sv(s_o1), QT[:, hq, cs], St[:, hv, :], start=True, stop=True), reads=[('QT', hq), ('S', hv)], writes=[sk(s_o1)])
                S.op('dve', lambda e: e.tensor_tensor(out=vn[i][:], in0=Us[hv][:], in1=sv(s_vn), op=ALU.subtract), reads=[kh('Us'), sk(s_vn)], writes=[k('vn')])
                S.op('act', lambda e: e.activation(out=o1s[i][:], in_=sv(s_o1), func=AF.Identity, scale=col('egc', hv)), reads=[sk(s_o1)] + GK, writes=[k('o1s')])
                s_o2, s_sn = slot(), slot()
                S.op('pe', lambda e: e.matmul(sv(s_o2), QKT[hv][:], vn[i][:], start=True, stop=True), reads=[kh('QKT'), k('vn')], writes=[sk(s_o2)])
                S.op('pe', lambda e: e.matmul(sv(s_sn), kd[hv][:], vn[i][:], start=True, stop=True), reads=[kh('kd'), k('vn')], writes=[sk(s_sn)])
                S.op('dve', lambda e: e.tensor_tensor(out=Os[hv][:], in0=sv(s_o2), in1=o1s[i][:], op=ALU.add), reads=[sk(s_o2), k('o1s')], writes=[kh('Os')])
                S.op('dve', lambda e: e.scalar_tensor_tensor(out=St[:, hv, :], in0=St[:, hv, :], scalar=col('egl', hv), in1=sv(s_sn), op0=ALU.mult, op1=ALU.add),
                     reads=[('S', hv), sk(s_sn)] + GK, writes=[('S', hv)])
            for hv in range(HV):
                kh = lambda n: (n, 'h', hv)
                ssq, rstd = scs[:, hv, 0:1], scs[:, hv, 1:2]
                S.op('act', lambda e: e.activation(out=On[hv][:], in_=Os[hv][:], func=AF.Square, accum_out=ssq), reads=[kh('Os'), kh('sc')], writes=[kh('On'), kh('sc')])
                S.op('dve', lambda e: e.tensor_scalar(out=ssq, in0=ssq, scalar1=1.0 / 128, scalar2=EPS, op0=ALU.mult, op1=ALU.add), reads=[kh('sc')], writes=[kh('sc')])
                S.op('act', lambda e: e.activation(out=ssq, in_=ssq, func=AF.Ln), reads=[kh('sc')], writes=[kh('sc')])
                S.op('act', lambda e: e.activation(out=rstd, in_=ssq, func=AF.Exp, scale=-0.5), reads=[kh('sc')], writes=[kh('sc')])
                S.op('dve', lambda e: e.tensor_scalar(out=On[hv][:], in0=Os[hv][:], scalar1=rstd, scalar2=None, op0=ALU.mult), reads=[kh('Os'), kh('sc')], writes=[kh('On')])
            for hv in range(HV):
                kh = lambda n: (n, 'h', hv)
                s_ot = slot()
                S.op('pe', lambda e: e.transpose(sv(s_ot), On[hv][:], ident[:]), reads=[kh('On'), 'ident'], writes=[sk(s_ot)])
                S.op('dve', lambda e: e.scalar_tensor_tensor(out=OT[:, hv, cs], in0=sv(s_ot), scalar=nw[:, 0:1], in1=SZ[:, hv, cs], op0=ALU.mult, op1=ALU.mult),
                     reads=[sk(s_ot), 'nw', ('SZ', hv)], writes=[('OT', hv)])
        if stage < 5:
            S.op('dve', lambda e: e.tensor_copy(out=OT[:], in_=VT[:]), reads=[('VT', h) for h in range(HV)] + [('OT', h) for h in range(HV)], writes=[('OT', h) for h in range(HV)])
        evs.append(S.dma('sp', ov[:, :, ts], OT[:], reads=[('OT', h) for h in range(HV)]))
    S.finish(evs)
    return nc


def run_gdn(hT_full, scale1, shift1, w_in, conv_w, a_log, dt_bias, norm_w, stage=9):
    T = hT_full.shape[1]
    HV = 64 // NCORE
    HQ = HV // 2
    nc = build_gdn(T, HV=HV, stage=stage)
    vecs = np.ascontiguousarray(np.stack([vec_pk(scale1), vec_pk(shift1)], axis=1))
    nwp = np.ascontiguousarray(norm_w.reshape(128, 1).astype(np.float32))
    KD = 4096; VDIM = 8192
    in_maps = []
    for c in range(NCORE):
        qcols = [slice((c * HQ + j) * 128, (c * HQ + j + 1) * 128) for j in range(HQ)]
        kcols = [slice(KD + (c * HQ + j) * 128, KD + (c * HQ + j + 1) * 128) for j in range(HQ)]
        vcols = [slice(2 * KD + (c * HV + j) * 128, 2 * KD + (c * HV + j + 1) * 128) for j in range(HV)]
        zcols = [slice(2 * KD + VDIM + (c * HV + j) * 128, 2 * KD + VDIM + (c * HV + j + 1) * 128) for j in range(HV)]
        wc = blk_cols(np.concatenate([w_in[:, s] for s in qcols + kcols + vcols + zcols], axis=1))
        a0 = 2 * KD + 2 * VDIM
        wab = np.ascontiguousarray(np.concatenate([w_in[:, a0 + c * HV: a0 + (c + 1) * HV], w_in[:, a0 + 64 + c * HV: a0 + 64 + (c + 1) * HV]], axis=1))
        cwc = np.concatenate([conv_w[:, s] for s in qcols + kcols + vcols], axis=1)
        cwp = np.ascontiguousarray(cwc.reshape(4, -1, 128).transpose(2, 1, 0))
        hp = np.ascontiguousarray(np.broadcast_to(np.stack([a_log[c * HV:(c + 1) * HV], dt_bias[c * HV:(c + 1) * HV]])[None], (128, 2, HV)).astype(np.float32))
        in_maps.append({"hT": hT_full, "vecs": vecs, "w": wc, "wab": wab, "cw": cwp, "hp": hp, "nw": nwp})
    res = _run(nc, in_maps)
    return np.concatenate([r["out"] for r in res], axis=0)


def build_route(TK, NT=256):
    nc = bass.Bass("TRN2", target_bir_lowering=False)
    NT = min(NT, TK)
    h1T = nc.dram_tensor("h1T", [D, TK], F32, kind="ExternalInput").ap()
    vecs = nc.dram_tensor("vecs", [128, 2, KC], F32, kind="ExternalInput").ap()
    wr = nc.dram_tensor("wr", [D, NG], F32, kind="ExternalInput").ap()
    out = nc.dram_tensor("out", [128, TK // 128], F32, kind="ExternalOutput").ap()
    S = Sched(nc)
    vt = nc.alloc_sbuf_tensor("vt", [128, 2, KC], F32)
    s2 = nc.alloc_sbuf_tensor("s2", [128, KC], F32)
    wrt = nc.alloc_sbuf_tensor("wrt", [128, KC, NG], F32)
    Hs = nc.alloc_sbuf_tensor("Hs", [128, KC, NT], F32)
    A = nc.alloc_sbuf_tensor("A", [128, KC, NT], F32R)
    Af = A[:].bitcast(F32)
    lgt = nc.alloc_sbuf_tensor("lgt", [128, NG], F32)
    ohg = nc.alloc_sbuf_tensor("ohg", [128, NG], F32)
    gm = nc.alloc_sbuf_tensor("gm", [128, 1], F32)
    ioti = nc.alloc_sbuf_tensor("ioti", [128, NG], I32)
    iotf = nc.alloc_sbuf_tensor("iotf", [128, NG], F32)
    gall = nc.alloc_sbuf_tensor("gall", [128, TK // 128], F32)
    ps = nc.alloc_psum_tensor("ps", [128, 512], F32)
    S.dma('sp', vt[:], vecs, writes=['vt'])
    S.dma('sp', wrt[:], wr.rearrange("(kc p) n -> p kc n", p=128), writes=['wrt'])
    S.op('pool', lambda e: e.iota(out=ioti[:], pattern=[[1, NG]], base=0, channel_multiplier=0), writes=['ioti'])
    S.op('dve', lambda e: e.tensor_copy(out=iotf[:], in_=ioti[:]), reads=['ioti'], writes=['iotf'])
    S.op('dve', lambda e: e.tensor_scalar(out=s2[:], in0=vt[:, 0, :], scalar1=1.0, scalar2=None, op0=ALU.add), reads=['vt'], writes=['s2'])
    hv = h1T.rearrange("(kc p) t -> p kc t", p=128)
    R = ['r']
    for t in range(TK // NT):
        ts = slice(t * NT, (t + 1) * NT)
        S.dma('sp', Hs[:], hv[:, :, ts], writes=[('Hs', b) for b in range(KC)])
        for blk in range(KC):
            S.op('act', lambda e: e.activation(out=A[:, blk, :], in_=Hs[:, blk, :], func=AF.Identity, scale=s2[:, blk:blk + 1], bias=vt[:, 1, blk:blk + 1]),
                 reads=[('Hs', blk), 's2', 'vt'], writes=[('A', blk)])
        for sub in range(NT // 128):
            ss = slice(sub * 128, (sub + 1) * 128)
            col = t * (NT // 128) + sub
            for kc in range(KC):
                S.op('pe', lambda e: e.matmul(ps[:, 0:NG], Af[:, kc, ss], wrt[:, kc, :], start=(kc == 0), stop=(kc == KC - 1)),
                     reads=[('A', kc), 'wrt'], writes=['ps'])
            S.op('act', lambda e: e.activation(out=lgt[:], in_=ps[:, 0:NG], func=AF.Copy), reads=['ps'], writes=R)
            S.op('dve', lambda e: e.tensor_reduce(out=gm[:], in_=lgt[:], axis=AX.X, op=ALU.max), reads=R, writes=R)
            S.op('dve', lambda e: e.tensor_scalar(out=ohg[:], in0=lgt[:], scalar1=gm[:, 0:1], scalar2=None, op0=ALU.is_equal), reads=R, writes=R)
            S.op('dve', lambda e: e.tensor_tensor(out=ohg[:], in0=ohg[:], in1=iotf[:], op=ALU.mult), reads=R + ['iotf'], writes=R)
            S.op('dve', lambda e: e.tensor_reduce(out=gall[:, col:col + 1], in_=ohg[:], axis=AX.X, op=ALU.add), reads=R, writes=R + ['gall'])
    ev = S.dma('sp', out, gall[:], reads=['gall'])
    S.finish([ev])
    return nc


def run_moe_grouped(h1T_full, scale2, shift2, gate2, lnw, lnb, w_group, w_expert, w_gate, w_up, w_down):
    T = h1T_full.shape[1]
    TK = T // NCORE
    nc = build_route(TK)
    vecs2 = np.ascontiguousarray(np.stack([vec_pk(scale2), vec_pk(shift2)], axis=1))
    in_maps = [{"h1T": np.ascontiguousarray(h1T_full[:, i * TK:(i + 1) * TK]), "vecs": vecs2, "wr": np.ascontiguousarray(w_group)}
               for i in range(NCORE)]
    res = _run(nc, in_maps)
    gidx = np.concatenate([np.ascontiguousarray(r["out"].T).reshape(-1) for r in res]).astype(np.int64)
    idxs = [np.nonzero(gidx == g)[0] for g in range(NG)]
    NP = max(256, -(-max(len(ix) for ix in idxs) // 256) * 256)
    nc2 = build_moe(NP, EPG=EPG, grouped=True)
    vecs = np.ascontiguousarray(np.stack([vec_pk(scale2), vec_pk(shift2), vec_pk(gate2), vec_pk(lnw), vec_pk(lnb)], axis=1))
    in_maps = []
    for g in range(NG):
        hg = np.zeros((D, NP), np.float32)
        hg[:, :len(idxs[g])] = h1T_full[:, idxs[g]]
        wr = np.ascontiguousarray(np.concatenate([w_group, w_expert[:, g * EPG:(g + 1) * EPG]], axis=1))
        in_maps.append({"h1T": hg, "vecs": vecs, "wr": wr,
                        "wg": np.stack([blk_cols(w_gate[e]) for e in range(g * EPG, (g + 1) * EPG)]),
                        "wu": np.stack([blk_cols(w_up[e]) for e in range(g * EPG, (g + 1) * EPG)]),
                        "wd": np.ascontiguousarray(w_down[g * EPG:(g + 1) * EPG])})
    res = _run(nc2, in_maps)
    outT = np.empty((D, T), np.float32)
    for g in range(NG):
        outT[:, idxs[g]] = res[g]["out"][:, :len(idxs[g])]
    return outT


def kernel(x, c, ada_w, ada_b, ln_w, ln_b, gdn_w_in, gdn_conv_w, gdn_a_log, gdn_dt_bias,
           gdn_norm_w, gdn_w_out, hgrn_w_in, hgrn_lb_logits, hgrn_norm_w, hgrn_w_out,
           moe_w_group, moe_w_expert, moe_w_gate, moe_w_up, moe_w_down):
    f = lambda a: np.asarray(a, np.float32)
    x = f(x); c = f(c)
    mod = run_mod(c, f(ada_w), f(ada_b))
    hT = np.ascontiguousarray(x[0].T)
    for layer in range(2):
        sh1, sc1, g1, sh2, sc2, g2 = [mod[layer, i * D:(i + 1) * D] for i in range(6)]
        if layer % 2 == 0:
            ogT = run_gdn(hT, sc1, sh1, f(gdn_w_in[0]), f(gdn_conv_w[0]), f(gdn_a_log[0]), f(gdn_dt_bias[0]), f(gdn_norm_w[0]))
            w_out = f(gdn_w_out[0])
        else:
            ogT = run_hgrn(hT, sc1, sh1, f(hgrn_w_in[0]), f(hgrn_lb_logits), f(hgrn_norm_w[0]))
            w_out = f(hgrn_w_out[0])
        h1T = run_outln(ogT, hT, w_out, g1, f(ln_w[layer, 0]), f(ln_b[layer, 0]))
        hT = run_moe_grouped(h1T, sc2, sh2, g2, f(ln_w[layer, 1]), f(ln_b[layer, 1]), f(moe_w_group[layer]), f(moe_w_expert[layer]),
                             f(moe_w_gate[layer]), f(moe_w_up[layer]), f(moe_w_down[layer]))
    return np.ascontiguousarray(hT.T)[None].astype(np.float32)
```

```python
import numpy as np
import concourse.bass as bass
import concourse.mybir as mybir
from concourse.bass_utils import run_bass_kernel_spmd

F32 = mybir.dt.float32; F32R = mybir.dt.float32r
I32 = mybir.dt.int32
AF = mybir.ActivationFunctionType; ALU = mybir.AluOpType
AX = mybir.AxisListType

D = 4096; SEQ = 16384; NCORE = 8; KC = D // 128
ALPHA = 4.0 ** 0.25
EPS = 1e-6
SIM_MODE = False


class Sched:
    def __init__(self, nc, n_dma_sems=32):
        self.nc = nc
        self.eng = {'pe': nc.tensor, 'dve': nc.vector, 'act': nc.scalar, 'pool': nc.gpsimd, 'sp': nc.sync}
        self.esem = {k: nc.alloc_semaphore('es_' + k) for k in ('pe', 'dve', 'act', 'pool')}
        self.ecnt = {k: 0 for k in self.esem}
        self.dsem = [nc.alloc_semaphore('ds%d' % i) for i in range(n_dma_sems)]
        self.dcnt = [0] * n_dma_sems
        self.dnext = 0
        self.waited = {}
        self.lastw = {}
        self.readers = {}
        self.nins = 0

    def _wait(self, e, ev):
        semkey, h, val, src = ev
        if src == e and e == 'pe':
            return
        k = (e, semkey)
        if self.waited.get(k, 0) >= val:
            return
        self.waited[k] = val
        self.eng[e].wait_ge(h, val)

    def _deps(self, e, reads, writes):
        for r in reads:
            ev = self.lastw.get(r)
            if ev is not None:
                self._wait(e, ev)
        for w in writes:
            ev = self.lastw.get(w)
            if ev is not None:
                self._wait(e, ev)
            for ev in self.readers.get(w, ()):
                self._wait(e, ev)

    def _commit(self, ev, reads, writes):
        for r in reads:
            lst = self.readers.setdefault(r, [])
            lst.append(ev)
            if len(lst) > 48:
                best = {}
                for x in lst:
                    if x[0] not in best or best[x[0]][2] < x[2]:
                        best[x[0]] = x
                self.readers[r] = list(best.values())
        for w in writes:
            self.lastw[w] = ev
            self.readers[w] = []

    def op(self, e, fn, reads=(), writes=()):
        self._deps(e, reads, writes)
        ins = fn(self.eng[e])
        self.ecnt[e] += 1
        ins.then_inc(self.esem[e], 1)
        ev = (e, self.esem[e], self.ecnt[e], e)
        self._commit(ev, reads, writes)
        self.nins += 1
        return ev

    def dma(self, q, out, in_, reads=(), writes=(), **kw):
        if q == 'pool' and SIM_MODE:
            q = 'sp'
            out = out.bitcast(F32)
        self._deps(q, reads, writes)
        i = self.dnext
        self.dnext = (self.dnext + 1) % len(self.dsem)
        if self.dcnt[i] > 0:
            self._wait(q, (('d', i), self.dsem[i], self.dcnt[i], None))
        self.dcnt[i] += 16
        self.eng[q].dma_start(out=out, in_=in_, **kw).then_inc(self.dsem[i], 16)
        ev = (('d', i), self.dsem[i], self.dcnt[i], None)
        self._commit(ev, reads, writes)
        self.nins += 1
        return ev

    def _dma_pool(self, out, in_, reads, writes, **kw):
        self._deps('pool', reads, writes)
        key = ('pd', writes[0])
        if not hasattr(self, 'psem'):
            self.psem = {}
        if key not in self.psem:
            self.psem[key] = self.nc.alloc_semaphore('pd%d' % len(self.psem))
        else:
            self.eng['pool'].wait_ge(self.psem[key], 16)
            self.eng['pool'].sem_clear(self.psem[key])
            for k in [k for k in self.waited if k[1] == key]:
                del self.waited[k]
        h = self.psem[key]
        self.eng['pool'].dma_start(out=out, in_=in_, **kw).then_inc(h, 16)
        ev = (key, h, 16, None)
        self._commit(ev, reads, writes)
        self.nins += 1
        return ev

    def finish(self, evs, q='sp'):
        for ev in evs:
            self._wait(q, ev)


def _run(nc, in_maps):
    res = run_bass_kernel_spmd(nc, in_maps, core_ids=list(range(NCORE)))
    return res.results


def blk_cols(w, kc=None):
    K, N = w.shape
    kc = K // 128
    nb = N // 128
    return np.ascontiguousarray(w.reshape(kc, 128, nb, 128).transpose(2, 1, 0, 3).reshape(nb, 128, kc * 128))


def vec_pk(v):
    v = np.asarray(v, np.float32).reshape(-1, 128)
    return np.ascontiguousarray(v.T)


def build_mod(ncols):
    nc = bass.Bass("TRN2", target_bir_lowering=False)
    nb = ncols // 128
    c_in = nc.dram_tensor("c", [128, KC], F32, kind="ExternalInput").ap()
    w_in = nc.dram_tensor("w", [D, ncols], F32, kind="ExternalInput").ap()
    b_in = nc.dram_tensor("b", [128, nb], F32, kind="ExternalInput").ap()
    out = nc.dram_tensor("out", [128, nb], F32, kind="ExternalOutput").ap()
    S = Sched(nc)
    ct = nc.alloc_sbuf_tensor("ct", [128, KC], F32)
    cond = nc.alloc_sbuf_tensor("cond", [128, KC], F32)
    sg = nc.alloc_sbuf_tensor("sg", [128, KC], F32)
    bt = nc.alloc_sbuf_tensor("bt", [128, nb], F32)
    ot = nc.alloc_sbuf_tensor("ot", [128, nb], F32)
    CW = 512
    wt = [nc.alloc_sbuf_tensor("wt%d" % i, [128, KC, CW], F32) for i in range(2)]
    pm = nc.alloc_psum_tensor("pm", [128, 512], F32)
    S.dma('sp', ct[:], c_in, writes=['ct'])
    S.dma('sp', bt[:], b_in, writes=['bt'])
    S.op('act', lambda e: e.activation(out=sg[:], in_=ct[:], func=AF.Sigmoid), reads=['ct'], writes=['sg'])
    S.op('dve', lambda e: e.tensor_tensor(out=cond[:], in0=ct[:], in1=sg[:], op=ALU.mult), reads=['ct', 'sg'], writes=['cond'])
    wv = w_in.rearrange("(kc p) n -> p kc n", p=128)
    for g in range(ncols // CW):
        buf = wt[g % 2]
        S.dma('sp', buf[:], wv[:, :, g * CW:(g + 1) * CW], writes=[('wt', g % 2)])
        for jj in range(CW // 128):
            j = g * (CW // 128) + jj
            for kc in range(KC):
                S.op('pe', lambda e: e.matmul(pm[:, j:j + 1], buf[:, kc, jj * 128:(jj + 1) * 128], cond[:, kc:kc + 1],
                                              start=(kc == 0), stop=(kc == KC - 1)),
                     reads=[('wt', g % 2), 'cond'], writes=['pm'])
    S.op('dve', lambda e: e.tensor_tensor(out=ot[:], in0=pm[:, 0:nb], in1=bt[:], op=ALU.add), reads=['pm', 'bt'], writes=['ot'])
    ev = S.dma('sp', out, ot[:], reads=['ot'])
    S.finish([ev])
    return nc


def run_mod(c, ada_w, ada_b):
    depth = ada_w.shape[0]
    ncols_all = depth * 6 * D
    ncols = ncols_all // NCORE
    nc = build_mod(ncols)
    cpk = vec_pk(c.reshape(-1))
    in_maps = []
    for i in range(NCORE):
        lo = i * ncols
        l = lo // (6 * D)
        off = lo % (6 * D)
        w = np.ascontiguousarray(ada_w[l][:, off:off + ncols])
        b = vec_pk(ada_b[l][off:off + ncols])
        in_maps.append({"c": cpk, "w": w, "b": b})
    res = _run(nc, in_maps)
    mod = np.concatenate([np.ascontiguousarray(r["out"].T).reshape(-1) for r in res]).reshape(depth, 6 * D)
    return mod


def emit_ln_tile(S, nc, T, zt, zkey, NT, eps, lnw, lnb, out_fn, ones, ps1, ps2, tmp, k1='ps1', k2='ps2'):
    sq, mean, msq, var, rstd, nmr, t1 = tmp
    for blk in range(KC):
        S.op('act', lambda e: e.activation(out=sq[blk % 2][:], in_=zt[:, blk, :], func=AF.Square),
             reads=[(zkey, blk)], writes=[('sq', blk % 2)])
        S.op('pe', lambda e: e.matmul(ps1[:, 0:NT], ones[:], zt[:, blk, :], start=(blk == 0), stop=(blk == KC - 1)),
             reads=[(zkey, blk), 'ones'], writes=[k1])
        S.op('pe', lambda e: e.matmul(ps2[:, 0:NT], ones[:], sq[blk % 2][:], start=(blk == 0), stop=(blk == KC - 1)),
             reads=[('sq', blk % 2), 'ones'], writes=[k2])
    S.op('act', lambda e: e.activation(out=mean[:], in_=ps1[:, 0:NT], func=AF.Copy, scale=1.0 / D), reads=[k1], writes=['mean'])
    S.op('dve', lambda e: e.tensor_tensor(out=msq[:], in0=mean[:], in1=mean[:], op=ALU.mult), reads=['mean'], writes=['msq'])
    S.op('dve', lambda e: e.scalar_tensor_tensor(out=var[:], in0=ps2[:, 0:NT], scalar=1.0 / D, in1=msq[:], op0=ALU.mult, op1=ALU.subtract),
         reads=[k2, 'msq'], writes=['var'])
    S.op('dve', lambda e: e.tensor_scalar(out=var[:], in0=var[:], scalar1=eps, scalar2=None, op0=ALU.add), reads=['var'], writes=['var'])
    S.op('act', lambda e: e.activation(out=var[:], in_=var[:], func=AF.Ln), reads=['var'], writes=['var'])
    S.op('act', lambda e: e.activation(out=rstd[:], in_=var[:], func=AF.Exp, scale=-0.5), reads=['var'], writes=['rstd'])
    S.op('dve', lambda e: e.scalar_tensor_tensor(out=nmr[:], in0=mean[:], scalar=-1.0, in1=rstd[:], op0=ALU.mult, op1=ALU.mult),
         reads=['mean', 'rstd'], writes=['nmr'])
    for blk in range(KC):
        tt = t1[blk % 2]
        S.op('pool', lambda e: e.tensor_tensor(out=tt[:], in0=zt[:, blk, :], in1=rstd[:], op=ALU.mult),
             reads=[(zkey, blk), 'rstd'], writes=[('t1', blk % 2)])
        S.op('dve', lambda e: e.tensor_tensor(out=tt[:], in0=tt[:], in1=nmr[:], op=ALU.add),
             reads=[('t1', blk % 2), 'nmr'], writes=[('t1', blk % 2)])
        out_fn(blk, tt, ('t1', blk % 2))


def alloc_ln_tmp(nc, NT):
    sq = [nc.alloc_sbuf_tensor("ln_sq%d" % i, [128, NT], F32) for i in range(2)]
    t1 = [nc.alloc_sbuf_tensor("ln_t1%d" % i, [128, NT], F32) for i in range(2)]
    names = ["mean", "msq", "var", "rstd", "nmr"]
    ts = [nc.alloc_sbuf_tensor("ln_" + n, [128, NT], F32) for n in names]
    return (sq, ts[0], ts[1], ts[2], ts[3], ts[4], t1)


def build_outln(VD, TK, NT=None):
    nc = bass.Bass("TRN2", target_bir_lowering=False)
    if NT is None:
        NT = 512 if (VD <= 4096 and TK % 512 == 0) else 256
    VC = VD // 128
    ogT = nc.dram_tensor("ogT", [VD, TK], F32, kind="ExternalInput").ap()
    hT = nc.dram_tensor("hT", [D, TK], F32, kind="ExternalInput").ap()
    wout = nc.dram_tensor("wout", [KC, 128, VD], F32, kind="ExternalInput").ap()
    vecs = nc.dram_tensor("vecs", [128, 3, KC], F32, kind="ExternalInput").ap()
    out = nc.dram_tensor("out", [D, TK], F32, kind="ExternalOutput").ap()
    S = Sched(nc)
    vt = nc.alloc_sbuf_tensor("vt", [128, 3, KC], F32)
    gs = nc.alloc_sbuf_tensor("gs", [128, KC], F32)
    ones = nc.alloc_sbuf_tensor("ones", [128, 128], F32)
    ogt = nc.alloc_sbuf_tensor("ogt", [128, VC, NT], F32R)
    zt = nc.alloc_sbuf_tensor("zt", [128, KC, NT], F32)
    osm = [nc.alloc_sbuf_tensor("osm%d" % i, [128, NT], F32) for i in range(2)]
    wb = [nc.alloc_sbuf_tensor("wb%d" % i, [128, VC, 128], F32R) for i in range(2)]
    tmp = alloc_ln_tmp(nc, NT)
    py = [nc.alloc_psum_tensor("py%d" % i, [128, 512], F32) for i in range(2)]
    ps1 = nc.alloc_psum_tensor("ps1", [128, 512], F32)
    ps2 = nc.alloc_psum_tensor("ps2", [128, 512], F32)
    S.dma('sp', vt[:], vecs, writes=['vt'])
    S.op('dve', lambda e: e.memset(ones[:], 1.0), writes=['ones'])
    S.op('dve', lambda e: e.tensor_scalar(out=gs[:], in0=vt[:, 0, :], scalar1=1.0, scalar2=1.0 / ALPHA, op0=ALU.add, op1=ALU.mult),
         reads=['vt'], writes=['gs'])
    ogv = ogT.rearrange("(kc p) t -> p kc t", p=128)
    hv = hT.rearrange("(kc p) t -> p kc t", p=128)
    ov = out.rearrange("(kc p) t -> p kc t", p=128)
    evs = []
    for t in range(TK // NT):
        ts = slice(t * NT, (t + 1) * NT)
        S.dma('pool', ogt[:], ogv[:, :, ts], writes=['ogt'])
        S.dma('sp', zt[:], hv[:, :, ts], writes=[('zt', b) for b in range(KC)])
        for blk in range(KC):
            w = wb[blk % 2]
            S.dma('pool', w[:].rearrange("p k c -> p (k c)"), wout[blk], writes=[('wb', blk % 2)], max_dma_last_dim=8192)
            p = py[blk % 2]
            for kc in range(VC):
                S.op('pe', lambda e: e.matmul(p[:, 0:NT], w[:, kc, :], ogt[:, kc, :], start=(kc == 0), stop=(kc == VC - 1)),
                     reads=[('wb', blk % 2), 'ogt'], writes=[('py', blk % 2)])
            S.op('dve', lambda e: e.scalar_tensor_tensor(out=zt[:, blk, :], in0=p[:, 0:NT], scalar=gs[:, blk:blk + 1], in1=zt[:, blk, :],
                                                          op0=ALU.mult, op1=ALU.add),
                 reads=[('py', blk % 2), 'gs', ('zt', blk)], writes=[('zt', blk)])

        def out_fn(blk, tt, tkey):
            o = osm[blk % 2]
            S.op('act', lambda e: e.activation(out=o[:], in_=tt[:], func=AF.Identity, scale=vt[:, 1, blk:blk + 1], bias=vt[:, 2, blk:blk + 1]),
                 reads=[tkey, 'vt'], writes=[('osm', blk % 2)])
            evs.append(S.dma('sp', ov[:, blk, ts], o[:], reads=[('osm', blk % 2)]))
        emit_ln_tile(S, nc, t, zt, 'zt', NT, EPS / (ALPHA * ALPHA), None, None, out_fn, ones, ps1, ps2, tmp)
    S.finish(evs)
    return nc


def run_outln(ogT_full, hT_full, w_out, gate, lnw, lnb, TK=None):
    VD, T = ogT_full.shape
    TK = T // NCORE
    nc = build_outln(VD, TK)
    vecs = np.ascontiguousarray(np.stack([vec_pk(gate), vec_pk(lnw), vec_pk(lnb)], axis=1))
    w_blk = blk_cols(w_out)
    in_maps = []
    for i in range(NCORE):
        in_maps.append({"ogT": np.ascontiguousarray(ogT_full[:, i * TK:(i + 1) * TK]),
                        "hT": np.ascontiguousarray(hT_full[:, i * TK:(i + 1) * TK]),
                        "wout": w_blk, "vecs": vecs})
    res = _run(nc, in_maps)
    return np.concatenate([r["out"] for r in res], axis=1)


def make_affine(S, out_tile_ap, key, ones_ap, pattern, base, cm, cmp, fill=0.0):
    S.op('pool', lambda e: e.affine_select(out=out_tile_ap, in_=ones_ap, pattern=pattern, compare_op=cmp, fill=fill,
                                           base=base, channel_multiplier=cm),
         reads=['ones'], writes=[key])


NE = 64; NG = 8; EPG = 8; DFF = 256


def build_moe(TK, NT=256, EPG=8, grouped=False):
    NE = EPG if grouped else NG * EPG
    n_exp = NE
    NT = min(NT, TK)
    nc = bass.Bass("TRN2", target_bir_lowering=False)
    h1T = nc.dram_tensor("h1T", [D, TK], F32, kind="ExternalInput").ap()
    vecs = nc.dram_tensor("vecs", [128, 5, KC], F32, kind="ExternalInput").ap()
    wr = nc.dram_tensor("wr", [D, NG + NE], F32, kind="ExternalInput").ap()
    wg = nc.dram_tensor("wg", [NE, 2, 128, KC * 128], F32, kind="ExternalInput").ap()
    wu = nc.dram_tensor("wu", [NE, 2, 128, KC * 128], F32, kind="ExternalInput").ap()
    wd = nc.dram_tensor("wd", [NE, DFF, D], F32, kind="ExternalInput").ap()
    out = nc.dram_tensor("out", [D, TK], F32, kind="ExternalOutput").ap()
    S = Sched(nc)
    NR = NG + NE
    vt = nc.alloc_sbuf_tensor("vt", [128, 5, KC], F32)
    s2 = nc.alloc_sbuf_tensor("s2", [128, KC], F32)
    gs = nc.alloc_sbuf_tensor("gs", [128, KC], F32)
    ones = nc.alloc_sbuf_tensor("ones", [128, 128], F32)
    ident = nc.alloc_sbuf_tensor("ident", [128, 128], F32)
    wrt = nc.alloc_sbuf_tensor("wrt", [128, KC, NR], F32)
    A = nc.alloc_sbuf_tensor("A", [128, KC, NT], F32R)
    Af = A[:].bitcast(F32)
    Y = nc.alloc_sbuf_tensor("Y", [128, KC, NT], F32)
    wgb = [nc.alloc_sbuf_tensor("wgb%d" % i, [128, KC, 128], F32R) for i in range(2)]
    wub = [nc.alloc_sbuf_tensor("wub%d" % i, [128, KC, 128], F32R) for i in range(2)]
    wdb = [nc.alloc_sbuf_tensor("wdb%d" % i, [128, D], F32R) for i in range(2)]
    tmp = alloc_ln_tmp(nc, NT)
    osm = [nc.alloc_sbuf_tensor("osm%d" % i, [128, NT], F32) for i in range(2)]
    lgt = nc.alloc_sbuf_tensor("lgt", [128, NR], F32)
    sm = nc.alloc_sbuf_tensor("sm", [128, 16], F32)
    ohg = nc.alloc_sbuf_tensor("ohg", [128, NG], F32)
    eg = nc.alloc_sbuf_tensor("eg", [128, NG], F32)
    lm = nc.alloc_sbuf_tensor("lm", [128, NG, EPG], F32)
    lm2 = nc.alloc_sbuf_tensor("lm2", [128, NE], F32)
    oh1 = nc.alloc_sbuf_tensor("oh1", [128, NE], F32)
    oh2 = nc.alloc_sbuf_tensor("oh2", [128, NE], F32)
    wts = nc.alloc_sbuf_tensor("wts", [128, NE], F32)
    wtsT = nc.alloc_sbuf_tensor("wtsT", [NE, NT], F32)
    rw = [nc.alloc_sbuf_tensor("rw%d" % i, [NE, NT], F32) for i in range(2)]
    sgt = [nc.alloc_sbuf_tensor("sgt%d" % i, [128, NT], F32) for i in range(2)]
    tgt = [nc.alloc_sbuf_tensor("tgt%d" % i, [128, NT], F32) for i in range(2)]
    actT = [nc.alloc_sbuf_tensor("actT%d" % i, [128, NT], F32R) for i in range(4)]
    PS = [nc.alloc_psum_tensor("ps%d" % i, [128, 512], F32) for i in range(8)]

    def pk(i):
        return ('ps', i)

    S.dma('sp', vt[:], vecs, writes=['vt'])
    S.dma('sp', wrt[:], wr.rearrange("(kc p) n -> p kc n", p=128), writes=['wrt'])
    S.op('dve', lambda e: e.memset(ones[:], 1.0), writes=['ones'])
    make_affine(S, ident[:], 'ident', ones[:], [[-1, 128]], 0, 1, ALU.is_equal)
    S.op('dve', lambda e: e.tensor_scalar(out=s2[:], in0=vt[:, 0, :], scalar1=1.0, scalar2=None, op0=ALU.add), reads=['vt'], writes=['s2'])
    S.op('dve', lambda e: e.tensor_scalar(out=gs[:], in0=vt[:, 2, :], scalar1=1.0, scalar2=1.0 / ALPHA, op0=ALU.add, op1=ALU.mult),
         reads=['vt'], writes=['gs'])
    hv = h1T.rearrange("(kc p) t -> p kc t", p=128)
    ov = out.rearrange("(kc p) t -> p kc t", p=128)
    evs = []
    Akeys = [('A', b) for b in range(KC)]
    wcount = [0]
    for t in range(TK // NT):
        ts = slice(t * NT, (t + 1) * NT)
        S.dma('sp', Y[:], hv[:, :, ts], writes=[('Y', b) for b in range(KC)])
        for blk in range(KC):
            S.op('act', lambda e: e.activation(out=A[:, blk, :], in_=Y[:, blk, :], func=AF.Identity, scale=s2[:, blk:blk + 1], bias=vt[:, 1, blk:blk + 1]),
                 reads=[('Y', blk), 's2', 'vt'], writes=[('A', blk)])
        for sub in range(NT // 128):
            ss = slice(sub * 128, (sub + 1) * 128)
            for kc in range(KC):
                S.op('pe', lambda e: e.matmul(PS[4][:, 0:NR], Af[:, kc, ss], wrt[:, kc, :], start=(kc == 0), stop=(kc == KC - 1)),
                     reads=[('A', kc), 'wrt'], writes=[pk(4)])
            S.op('act', lambda e: e.activation(out=lgt[:], in_=PS[4][:, 0:NR], func=AF.Copy), reads=[pk(4)], writes=['lgt'])
            R = ['lgt', 'sm', 'ohg', 'eg', 'lm', 'lm2', 'oh1', 'oh2', 'wts']
            def dv(fn):
                S.op('dve', fn, reads=R, writes=R)
            def ac(fn):
                S.op('act', fn, reads=R, writes=R)
            gm, ngm, sge, pgrp, m1, m2, dl, e2, wA, wB = [sm[:, i:i + 1] for i in range(10)]
            dv(lambda e: e.tensor_reduce(out=gm, in_=lgt[:, 0:NG], axis=AX.X, op=ALU.max))
            dv(lambda e: e.tensor_scalar(out=ngm, in0=gm, scalar1=-1.0, scalar2=None, op0=ALU.mult))
            ac(lambda e: e.activation(out=eg[:], in_=lgt[:, 0:NG], func=AF.Exp, bias=ngm, accum_out=sge))
            dv(lambda e: e.reciprocal(out=pgrp, in_=sge))
            if grouped:
                lmf = lm[:, 0, :]
                dv(lambda e: e.tensor_copy(out=lmf, in_=lgt[:, NG:NR]))
            else:
                dv(lambda e: e.tensor_scalar(out=ohg[:], in0=lgt[:, 0:NG], scalar1=gm, scalar2=None, op0=ALU.is_equal))
                dv(lambda e: e.tensor_scalar(out=ohg[:], in0=ohg[:], scalar1=-1.0, scalar2=30000.0, op0=ALU.add, op1=ALU.mult))
                dv(lambda e: e.tensor_tensor(out=lm[:], in0=lgt[:, NG:NR].rearrange("p (g x) -> p g x", g=NG),
                                             in1=ohg[:].unsqueeze(2).to_broadcast([128, NG, EPG]), op=ALU.add))
                lmf = lm[:].rearrange("p g x -> p (g x)")
            dv(lambda e: e.tensor_reduce(out=m1, in_=lmf, axis=AX.X, op=ALU.max))
            dv(lambda e: e.tensor_scalar(out=oh1[:], in0=lmf, scalar1=m1, scalar2=None, op0=ALU.is_equal))
            dv(lambda e: e.scalar_tensor_tensor(out=lm2[:], in0=oh1[:], scalar=-30000.0, in1=lmf, op0=ALU.mult, op1=ALU.add))
            dv(lambda e: e.tensor_reduce(out=m2, in_=lm2[:], axis=AX.X, op=ALU.max))
            dv(lambda e: e.tensor_scalar(out=oh2[:], in0=lm2[:], scalar1=m2, scalar2=None, op0=ALU.is_equal))
            dv(lambda e: e.tensor_tensor(out=dl, in0=m2, in1=m1, op=ALU.subtract))
            ac(lambda e: e.activation(out=e2, in_=dl, func=AF.Exp))
            dv(lambda e: e.tensor_scalar(out=wA, in0=e2, scalar1=1.0, scalar2=None, op0=ALU.add))
            dv(lambda e: e.reciprocal(out=wA, in_=wA))
            dv(lambda e: e.tensor_tensor(out=wA, in0=wA, in1=pgrp, op=ALU.mult))
            dv(lambda e: e.tensor_tensor(out=wB, in0=wA, in1=e2, op=ALU.mult))
            dv(lambda e: e.tensor_scalar(out=wts[:], in0=oh1[:], scalar1=wA, scalar2=None, op0=ALU.mult))
            dv(lambda e: e.scalar_tensor_tensor(out=wts[:], in0=oh2[:], scalar=wB, in1=wts[:], op0=ALU.mult, op1=ALU.add))
            S.op('pe', lambda e: e.transpose(PS[4][0:NE, 0:128], wts[:], ident[:]), reads=R + ['ident'], writes=[pk(4)])
            S.op('act', lambda e: e.activation(out=wtsT[:, ss], in_=PS[4][0:NE, 0:128], func=AF.Copy), reads=[pk(4)], writes=['wtsT'])
        for ex in range(n_exp):
            r = rw[ex % 2]
            S.op('dve', lambda e: e.tensor_scalar(out=r[:], in0=wtsT[:], scalar1=ident[0:NE, ex:ex + 1], scalar2=None, op0=ALU.mult),
                 reads=['wtsT', 'ident'], writes=[('rw', ex % 2)])
            S.op('pe', lambda e: e.matmul(PS[4][:, 0:NT], ones[0:NE, :], r[:], start=True, stop=True),
                 reads=[('rw', ex % 2), 'ones'], writes=[pk(4)])
            for fb in range(2):
                wi = wcount[0] % 2
                wcount[0] += 1
                S.dma('pool', wgb[wi][:].rearrange("p k c -> p (k c)"), wg[ex, fb], writes=[('wgb', wi)], max_dma_last_dim=8192)
                S.dma('pool', wub[wi][:].rearrange("p k c -> p (k c)"), wu[ex, fb], writes=[('wub', wi)], max_dma_last_dim=8192)
                S.dma('pool', wdb[wi][:], wd[ex, fb * 128:(fb + 1) * 128, :], writes=[('wdb', wi)], max_dma_last_dim=8192)
                pg = PS[0 + wi]; pu = PS[2 + wi]
                for kc in range(KC):
                    S.op('pe', lambda e: e.matmul(pg[:, 0:NT], wgb[wi][:, kc, :], A[:, kc, :], start=(kc == 0), stop=(kc == KC - 1)),
                         reads=[('wgb', wi), ('A', kc)], writes=[pk(0 + wi)])
                for kc in range(KC):
                    S.op('pe', lambda e: e.matmul(pu[:, 0:NT], wub[wi][:, kc, :], A[:, kc, :], start=(kc == 0), stop=(kc == KC - 1)),
                         reads=[('wub', wi), ('A', kc)], writes=[pk(2 + wi)])
                S.op('act', lambda e: e.activation(out=sgt[wi][:], in_=pg[:, 0:NT], func=AF.Silu), reads=[pk(0 + wi)], writes=[('sgt', wi)])
                S.op('dve', lambda e: e.tensor_tensor(out=tgt[wi][:], in0=pu[:, 0:NT], in1=sgt[wi][:], op=ALU.mult),
                     reads=[pk(2 + wi), ('sgt', wi)], writes=[('tgt', wi)])
                ai = (ex % 2) * 2 + fb
                S.op('dve', lambda e: e.tensor_tensor(out=actT[ai][:], in0=PS[4][:, 0:NT], in1=tgt[wi][:], op=ALU.mult),
                     reads=[pk(4), ('tgt', wi)], writes=[('actT', ai)])
            for blk in range(KC):
                pyi = 5 + (blk % 2)
                for fb in range(2):
                    ai = (ex % 2) * 2 + fb
                    wi = (wcount[0] - 2 + fb) % 2
                    S.op('pe', lambda e: e.matmul(PS[pyi][:, 0:NT], wdb[wi][:, blk * 128:(blk + 1) * 128], actT[ai][:], start=(fb == 0), stop=(fb == 1)),
                         reads=[('wdb', wi), ('actT', ai)], writes=[pk(pyi)])
                if ex == 0:
                    S.op('dve', lambda e: e.tensor_copy(out=Y[:, blk, :], in_=PS[pyi][:, 0:NT]), reads=[pk(pyi)], writes=[('Y', blk)])
                else:
                    S.op('dve', lambda e: e.tensor_tensor(out=Y[:, blk, :], in0=PS[pyi][:, 0:NT], in1=Y[:, blk, :], op=ALU.add),
                         reads=[pk(pyi), ('Y', blk)], writes=[('Y', blk)])
        for blk in range(KC):
            hs = sgt[blk % 2]
            S.dma('sp', hs[:], hv[:, blk, ts], writes=[('sgt', blk % 2)])
            S.op('dve', lambda e: e.scalar_tensor_tensor(out=Y[:, blk, :], in0=Y[:, blk, :], scalar=gs[:, blk:blk + 1], in1=hs[:],
                                                          op0=ALU.mult, op1=ALU.add),
                 reads=[('Y', blk), ('sgt', blk % 2), 'gs'], writes=[('Y', blk)])

        def out_fn(blk, tt, tkey):
            o = osm[blk % 2]
            S.op('act', lambda e: e.activation(out=o[:], in_=tt[:], func=AF.Identity, scale=vt[:, 3, blk:blk + 1], bias=vt[:, 4, blk:blk + 1]),
                 reads=[tkey, 'vt'], writes=[('osm', blk % 2)])
            evs.append(S.dma('sp', ov[:, blk, ts], o[:], reads=[('osm', blk % 2)]))
        emit_ln_tile(S, nc, t, Y, 'Y', NT, EPS / (ALPHA * ALPHA), None, None, out_fn, ones, PS[0], PS[2], tmp, k1=('ps', 0), k2=('ps', 2))
    S.finish(evs)
    return nc


def run_moe(h1T_full, scale2, shift2, gate2, lnw, lnb, w_group, w_expert, w_gate, w_up, w_down):
    T = h1T_full.shape[1]
    TK = T // NCORE
    nc = build_moe(TK, EPG=w_expert.shape[1] // NG)
    vecs = np.ascontiguousarray(np.stack([vec_pk(scale2), vec_pk(shift2), vec_pk(gate2), vec_pk(lnw), vec_pk(lnb)], axis=1))
    wr = np.ascontiguousarray(np.concatenate([w_group, w_expert], axis=1))
    in_maps = []
    for i in range(NCORE):
        in_maps.append({"h1T": np.ascontiguousarray(h1T_full[:, i * TK:(i + 1) * TK]), "vecs": vecs, "wr": wr,
                        "wg": np.stack([blk_cols(w_gate[e]) for e in range(w_gate.shape[0])]),
                        "wu": np.stack([blk_cols(w_up[e]) for e in range(w_up.shape[0])]), "wd": w_down})
    res = _run(nc, in_maps)
    return np.concatenate([r["out"] for r in res], axis=1)


def build_hgrn(T, HPC=4, NT=512):
    nc = bass.Bass("TRN2", target_bir_lowering=False)
    NCB = 4 * HPC
    hT = nc.dram_tensor("hT", [D, T], F32, kind="ExternalInput").ap()
    vecs = nc.dram_tensor("vecs", [128, 2, KC], F32, kind="ExternalInput").ap()
    w = nc.dram_tensor("w", [D, NCB * 128], F32, kind="ExternalInput").ap()
    lbl = nc.dram_tensor("lbl", [128, 2, HPC], F32, kind="ExternalInput").ap()
    nw_in = nc.dram_tensor("nw", [128, 1], F32, kind="ExternalInput").ap()
    out = nc.dram_tensor("out", [HPC * 128, T], F32, kind="ExternalOutput").ap()
    S = Sched(nc)
    vt = nc.alloc_sbuf_tensor("vt", [128, 2, KC], F32)
    s1 = nc.alloc_sbuf_tensor("s1", [128, KC], F32)
    lbt = nc.alloc_sbuf_tensor("lbt", [128, 2, HPC], F32)
    lb = nc.alloc_sbuf_tensor("lb", [128, HPC], F32)
    oml = nc.alloc_sbuf_tensor("oml", [128, HPC], F32)
    nw = nc.alloc_sbuf_tensor("nwt", [128, 1], F32)
    ones = nc.alloc_sbuf_tensor("ones", [128, 128], F32)
    ident = nc.alloc_sbuf_tensor("ident", [128, 128], F32)
    maskU = nc.alloc_sbuf_tensor("maskU", [128, 128], F32)
    Hs = nc.alloc_sbuf_tensor("Hs", [128, KC // 2, NT], F32)
    U = nc.alloc_sbuf_tensor("U", [128, KC, NT], F32R)
    wb = [nc.alloc_sbuf_tensor("wb%d" % i, [128, KC, 128], F32R) for i in range(2)]
    QT = nc.alloc_sbuf_tensor("QT", [128, HPC, NT], F32)
    KT = nc.alloc_sbuf_tensor("KT", [128, HPC, NT], F32)
    LF = nc.alloc_sbuf_tensor("LF", [128, HPC, NT], F32)
    VT = nc.alloc_sbuf_tensor("VT", [128, HPC, NT], F32)
    OG = nc.alloc_sbuf_tensor("OG", [128, HPC, NT], F32)
    OT = nc.alloc_sbuf_tensor("OT", [128, HPC, NT], F32)
    St = nc.alloc_sbuf_tensor("St", [128, HPC, 128], F32)
    sgm = nc.alloc_sbuf_tensor("sgm", [128, NT], F32)
    NB = 2
    def mk(name):
        return [nc.alloc_sbuf_tensor("%s%d" % (name, i), [128, 128], F32) for i in range(NB)]
    B_, E1, Qt, Kt, Qs, KdT, At, V_, Kd, On, Jk = mk("B"), mk("E1"), mk("Qt"), mk("Kt"), mk("Qs"), mk("KdT"), mk("At"), mk("V"), mk("Kd"), mk("On"), mk("Jk")
    sc = [nc.alloc_sbuf_tensor("sc%d" % i, [128, 8], F32) for i in range(NB)]
    PS = [nc.alloc_psum_tensor("ps%d" % i, [128, 512], F32) for i in range(8)]
    pk = lambda i: ('ps', i)

    S.dma('sp', vt[:], vecs, writes=['vt'])
    S.dma('sp', lbt[:], lbl, writes=['lbt'])
    S.dma('sp', nw[:], nw_in, writes=['nw'])
    S.op('dve', lambda e: e.memset(ones[:], 1.0), writes=['ones'])
    S.op('dve', lambda e: e.memset(St[:], 0.0), writes=[('S', h) for h in range(HPC)])
    make_affine(S, ident[:], 'ident', ones[:], [[-1, 128]], 0, 1, ALU.is_equal)
    make_affine(S, maskU[:], 'maskU', ones[:], [[1, 128]], 0, -1, ALU.is_ge)
    S.op('dve', lambda e: e.tensor_scalar(out=s1[:], in0=vt[:, 0, :], scalar1=1.0, scalar2=None, op0=ALU.add), reads=['vt'], writes=['s1'])
    S.op('dve', lambda e: e.tensor_tensor(out=lb[:], in0=lbt[:, 0, :], in1=lbt[:, 1, :], op=ALU.subtract), reads=['lbt'], writes=['lb'])
    S.op('act', lambda e: e.activation(out=lb[:], in_=lb[:], func=AF.Exp), reads=['lb'], writes=['lb'])
    S.op('dve', lambda e: e.tensor_scalar(out=lb[:], in0=lb[:], scalar1=1.0, scalar2=None, op0=ALU.add), reads=['lb'], writes=['lb'])
    S.op('dve', lambda e: e.reciprocal(out=lb[:], in_=lb[:]), reads=['lb'], writes=['lb'])
    S.op('dve', lambda e: e.tensor_scalar(out=oml[:], in0=lb[:], scalar1=-1.0, scalar2=1.0, op0=ALU.mult, op1=ALU.add), reads=['lb'], writes=['oml'])

    hv = hT.rearrange("(kc p) t -> p kc t", p=128)
    wv = w.rearrange("(kc p) n -> p kc n", p=128)
    ov = out.rearrange("(h p) t -> p h t", p=128)
    evs = []
    cnt = [0]
    for t in range(T // NT):
        ts = slice(t * NT, (t + 1) * NT)
        for half in range(2):
            hb = half * (KC // 2)
            S.dma('sp', Hs[:], hv[:, hb:hb + KC // 2, ts], writes=[('Hs', b) for b in range(KC // 2)])
            for b in range(KC // 2):
                blk = hb + b
                S.op('act', lambda e: e.activation(out=U[:, blk, :], in_=Hs[:, b, :], func=AF.Identity, scale=s1[:, blk:blk + 1], bias=vt[:, 1, blk:blk + 1]),
                     reads=[('Hs', b), 's1', 'vt'], writes=[('U', blk)])
        for cb in range(NCB):
            hh, typ = cb // 4, cb % 4
            wi = cb % 2
            S.dma('pool', wb[wi][:], wv[:, :, cb * 128:(cb + 1) * 128], writes=[('wb', wi)])
            pp = PS[wi]
            for kc in range(KC):
                S.op('pe', lambda e: e.matmul(pp[:, 0:NT], wb[wi][:, kc, :], U[:, kc, :], start=(kc == 0), stop=(kc == KC - 1)),
                     reads=[('wb', wi), ('U', kc)], writes=[pk(wi)])
            if typ == 0:
                S.op('act', lambda e: e.activation(out=QT[:, hh, :], in_=pp[:, 0:NT], func=AF.Silu), reads=[pk(wi)], writes=[('QT', hh)])
            elif typ == 1:
                S.op('act', lambda e: e.activation(out=sgm[:], in_=pp[:, 0:NT], func=AF.Sigmoid), reads=[pk(wi)], writes=['sgm'])
                S.op('dve', lambda e: e.tensor_scalar(out=sgm[:], in0=sgm[:], scalar1=oml[:, hh:hh + 1], scalar2=lb[:, hh:hh + 1], op0=ALU.mult, op1=ALU.add),
                     reads=['sgm', 'oml', 'lb'], writes=['sgm'])
                S.op('act', lambda e: e.activation(out=LF[:, hh, :], in_=sgm[:], func=AF.Ln), reads=['sgm'], writes=[('LF', hh)])
                S.op('dve', lambda e: e.tensor_scalar(out=KT[:, hh, :], in0=sgm[:], scalar1=-1.0, scalar2=1.0, op0=ALU.mult, op1=ALU.add),
                     reads=['sgm'], writes=[('KT', hh)])
            elif typ == 2:
                S.op('act', lambda e: e.activation(out=VT[:, hh, :], in_=pp[:, 0:NT], func=AF.Copy), reads=[pk(wi)], writes=[('VT', hh)])
            else:
                S.op('act', lambda e: e.activation(out=OG[:, hh, :], in_=pp[:, 0:NT], func=AF.Silu), reads=[pk(wi)], writes=[('OG', hh)])
        for c in range(NT // 128):
            cs = slice(c * 128, (c + 1) * 128)
            for hh in range(HPC):
                i = cnt[0] % NB
                cnt[0] += 1
                k = lambda n: (n, i)
                bref, nbref, blast, eblast, ssq, rstd = [sc[i][:, j:j + 1] for j in range(6)]
                S.op('dve', lambda e: e.tensor_tensor_scan(out=B_[i][:], data0=ones[:], data1=LF[:, hh, cs], initial=0.0, op0=ALU.mult, op1=ALU.add),
                     reads=[('LF', hh), 'ones'], writes=[k('B')])
                S.op('dve', lambda e: e.tensor_copy(out=bref, in_=B_[i][:, 63:64]), reads=[k('B')], writes=[k('sc')])
                S.op('dve', lambda e: e.tensor_scalar(out=nbref, in0=B_[i][:, 63:64], scalar1=-1.0, scalar2=None, op0=ALU.mult), reads=[k('B'), k('sc')], writes=[k('sc')])
                S.op('dve', lambda e: e.tensor_copy(out=blast, in_=B_[i][:, 127:128]), reads=[k('B'), k('sc')], writes=[k('sc')])
                S.op('act', lambda e: e.activation(out=eblast, in_=blast, func=AF.Exp), reads=[k('sc')], writes=[k('sc')])
                S.op('act', lambda e: e.activation(out=E1[i][:], in_=B_[i][:], func=AF.Exp, bias=nbref), reads=[k('B'), k('sc')], writes=[k('E1')])
                S.op('dve', lambda e: e.tensor_tensor(out=Qt[i][:], in0=QT[:, hh, cs], in1=E1[i][:], op=ALU.mult), reads=[('QT', hh), k('E1')], writes=[k('Qt')])
                S.op('act', lambda e: e.activation(out=E1[i][:], in_=B_[i][:], func=AF.Exp, scale=-1.0, bias=bref), reads=[k('B'), k('sc'), k('Qt')], writes=[k('E1')])
                S.op('dve', lambda e: e.tensor_tensor(out=Kt[i][:], in0=KT[:, hh, cs], in1=E1[i][:], op=ALU.mult), reads=[('KT', hh), k('E1')], writes=[k('Kt')])
                S.op('act', lambda e: e.activation(out=E1[i][:], in_=B_[i][:], func=AF.Exp), reads=[k('B'), k('Kt')], writes=[k('E1')])
                S.op('dve', lambda e: e.tensor_tensor(out=Qs[i][:], in0=QT[:, hh, cs], in1=E1[i][:], op=ALU.mult), reads=[('QT', hh), k('E1')], writes=[k('Qs')])
                S.op('act', lambda e: e.activation(out=E1[i][:], in_=B_[i][:], func=AF.Exp, scale=-1.0, bias=blast), reads=[k('B'), k('sc'), k('Qs')], writes=[k('E1')])
                S.op('dve', lambda e: e.tensor_tensor(out=KdT[i][:], in0=KT[:, hh, cs], in1=E1[i][:], op=ALU.mult), reads=[('KT', hh), k('E1')], writes=[k('KdT')])
                S.op('pe', lambda e: e.matmul(PS[2][:, 0:128], Kt[i][:], Qt[i][:], start=True, stop=True), reads=[k('Kt'), k('Qt')], writes=[pk(2)])
                S.op('dve', lambda e: e.tensor_scalar(out=At[i][:], in0=PS[2][:, 0:128], scalar1=-1e30, scalar2=1e30, op0=ALU.max, op1=ALU.min),
                     reads=[pk(2)], writes=[k('At')])
                S.op('dve', lambda e: e.tensor_tensor(out=At[i][:], in0=At[i][:], in1=maskU[:], op=ALU.mult), reads=[k('At'), 'maskU'], writes=[k('At')])
                S.op('pe', lambda e: e.transpose(PS[3][:, 0:128], VT[:, hh, cs], ident[:]), reads=[('VT', hh), 'ident'], writes=[pk(3)])
                S.op('act', lambda e: e.activation(out=V_[i][:], in_=PS[3][:, 0:128], func=AF.Copy), reads=[pk(3)], writes=[k('V')])
                S.op('pe', lambda e: e.transpose(PS[4][:, 0:128], KdT[i][:], ident[:]), reads=[k('KdT'), 'ident'], writes=[pk(4)])
                S.op('dve', lambda e: e.tensor_copy(out=Kd[i][:], in_=PS[4][:, 0:128]), reads=[pk(4)], writes=[k('Kd')])
                S.op('pe', lambda e: e.matmul(PS[5][:, 0:128], At[i][:], V_[i][:], start=True, stop=False), reads=[k('At'), k('V')], writes=[pk(5)])
                S.op('pe', lambda e: e.matmul(PS[5][:, 0:128], Qs[i][:], St[:, hh, :], start=False, stop=True), reads=[k('Qs'), ('S', hh)], writes=[pk(5)])
                S.op('pe', lambda e: e.matmul(PS[6][:, 0:128], Kd[i][:], V_[i][:], start=True, stop=True), reads=[k('Kd'), k('V')], writes=[pk(6)])
                S.op('dve', lambda e: e.scalar_tensor_tensor(out=St[:, hh, :], in0=St[:, hh, :], scalar=eblast, in1=PS[6][:, 0:128], op0=ALU.mult, op1=ALU.add),
                     reads=[('S', hh), k('sc'), pk(6)], writes=[('S', hh)])
                S.op('act', lambda e: e.activation(out=Jk[i][:], in_=PS[5][:, 0:128], func=AF.Square, accum_out=ssq), reads=[pk(5), k('sc')], writes=[k('Jk'), k('sc')])
                S.op('dve', lambda e: e.tensor_scalar(out=ssq, in0=ssq, scalar1=1.0 / 128, scalar2=EPS, op0=ALU.mult, op1=ALU.add), reads=[k('sc')], writes=[k('sc')])
                S.op('act', lambda e: e.activation(out=ssq, in_=ssq, func=AF.Ln), reads=[k('sc')], writes=[k('sc')])
                S.op('act', lambda e: e.activation(out=rstd, in_=ssq, func=AF.Exp, scale=-0.5), reads=[k('sc')], writes=[k('sc')])
                S.op('dve', lambda e: e.tensor_scalar(out=On[i][:], in0=PS[5][:, 0:128], scalar1=rstd, scalar2=None, op0=ALU.mult), reads=[pk(5), k('sc')], writes=[k('On')])
                S.op('pe', lambda e: e.transpose(PS[7][:, 0:128], On[i][:], ident[:]), reads=[k('On'), 'ident'], writes=[pk(7)])
                S.op('dve', lambda e: e.scalar_tensor_tensor(out=OT[:, hh, cs], in0=PS[7][:, 0:128], scalar=nw[:, 0:1], in1=OG[:, hh, cs], op0=ALU.mult, op1=ALU.mult),
                     reads=[pk(7), 'nw', ('OG', hh)], writes=[('OT', hh)])
        evs.append(S.dma('sp', ov[:, :, ts], OT[:], reads=[('OT', h) for h in range(HPC)]))
    S.finish(evs)
    return nc


def run_hgrn(hT_full, scale1, shift1, w_in, lb_logits, norm_w):
    T = hT_full.shape[1]
    HPC = 32 // NCORE
    nc = build_hgrn(T, HPC=HPC)
    vecs = np.ascontiguousarray(np.stack([vec_pk(scale1), vec_pk(shift1)], axis=1))
    nwp = np.ascontiguousarray(norm_w.reshape(128, 1).astype(np.float32))
    in_maps = []
    for c in range(NCORE):
        cols = []
        for hh in range(HPC):
            h = c * HPC + hh
            for typ in range(4):
                cols.append(w_in[:, typ * D + h * 128: typ * D + (h + 1) * 128])
        wc = np.ascontiguousarray(np.concatenate(cols, axis=1))
        ch = slice(c * HPC * 128, (c + 1) * HPC * 128)
        lbl = np.ascontiguousarray(np.stack([vec_pk(lb_logits[0, ch]), vec_pk(lb_logits[1, ch])], axis=1))
        in_maps.append({"hT": hT_full, "vecs": vecs, "w": wc, "lbl": lbl, "nw": nwp})
    res = _run(nc, in_maps)
    return np.concatenate([r["out"] for r in res], axis=0)


def build_gdn(T, HV=8, NT=256, stage=9):
    nc = bass.Bass("TRN2", target_bir_lowering=False)
    HQ = HV // 2
    NCONV = 2 * HQ + HV
    NCB = NCONV + HV
    hT = nc.dram_tensor("hT", [D, T], F32, kind="ExternalInput").ap()
    vecs = nc.dram_tensor("vecs", [128, 2, KC], F32, kind="ExternalInput").ap()
    w = nc.dram_tensor("w", [NCB, 128, KC * 128], F32, kind="ExternalInput").ap()
    wab = nc.dram_tensor("wab", [D, 2 * HV], F32, kind="ExternalInput").ap()
    cw_in = nc.dram_tensor("cw", [128, NCONV, 4], F32, kind="ExternalInput").ap()
    hp_in = nc.dram_tensor("hp", [128, 2, HV], F32, kind="ExternalInput").ap()
    nw_in = nc.dram_tensor("nw", [128, 1], F32, kind="ExternalInput").ap()
    out = nc.dram_tensor("out", [HV * 128, T], F32, kind="ExternalOutput").ap()
    S = Sched(nc)
    A_ = nc.alloc_sbuf_tensor
    vt = A_("vt", [128, 2, KC], F32); s1 = A_("s1", [128, KC], F32)
    cw = A_("cwt", [128, NCONV, 4], F32); hp = A_("hpt", [128, 2, HV], F32); nA = A_("nA", [128, HV], F32)
    nw = A_("nwt", [128, 1], F32)
    ones = A_("ones", [128, 128], F32); ident = A_("ident", [128, 128], F32)
    mU = A_("mU", [128, 128], F32); nSL = A_("nSL", [128, 128], F32); nSU = A_("nSU", [128, 128], F32); nU = A_("nU", [128, 128], F32)
    Hs = A_("Hs", [128, KC // 2, NT], F32); U = A_("U", [128, KC, NT], F32R)
    wb = [A_("wb%d" % i, [128, KC, 128], F32R) for i in range(2)]
    wabt = A_("wabt", [128, KC, 2 * HV], F32R)
    pre = [A_("pre%d" % i, [128, NT + 3], F32) for i in range(2)]
    halo = A_("halo", [128, NCONV, 3], F32)
    acc = [A_("acc%d" % i, [128, NT], F32) for i in range(2)]
    xs = [A_("xs%d" % i, [128, NT], F32) for i in range(2)]
    sq = acc
    QT = A_("QT", [128, HQ, NT], F32); KT = A_("KT", [128, HQ, NT], F32)
    VT = A_("VT", [128, HV, NT], F32); SZ = A_("SZ", [128, HV, NT], F32); OT = A_("OT", [128, HV, NT], F32)
    St = A_("St", [128, HV, 128], F32)
    GS = {n: A_("gs_" + n, [128, HV], F32) for n in ("emb", "den", "beta", "lnb", "tt", "g", "gc", "gl", "glb", "ngc", "egc", "bgc", "ekd", "egl")}
    NB = 2
    def mk(name, n=NB, w_=128):
        return [A_("%s%d" % (name, i), [128, w_], F32) for i in range(n)]
    KKs, KQs, Ktok = mk("KKs", HQ), mk("KQs", HQ), mk("Ktok", HQ)
    tA, tAT, tQ = mk("tA"), mk("tAT"), mk("tQ")
    Am, ATm, QKT = mk("Am", HV), mk("ATm", HV), mk("QKT", HV)
    Pm = [mk("Pm0", HV), mk("Pm1", HV)]; Qm = [mk("Qm0", HV), mk("Qm1", HV)]; Rm = [mk("Rm0", HV), mk("Rm1", HV)]
    kd, Us, WTs = mk("kd", HV), mk("Us", HV), mk("WTs", HV)
    vb, kbg, Os, On = mk("vb", HV), mk("kbg", HV), mk("Os", HV), mk("On", HV)
    vn, o1s = mk("vn"), mk("o1s")
    scs = A_("scs", [128, HV, 2], F32)
    sc = [A_("sc%d" % i, [128, 4], F32) for i in range(NB)]
    PS = [nc.alloc_psum_tensor("ps%d" % i, [128, 512], F32) for i in range(8)]
    pk = lambda i: ('ps', i)
    slots = [(b, 0) for b in range(3, 8)]
    sl_i = [0]
    def slot():
        s = slots[sl_i[0] % len(slots)]
        sl_i[0] += 1
        return s
    def sv(s):
        return PS[s[0]][:, s[1] * 128:(s[1] + 1) * 128]
    def sk(s):
        return ('ps', s[0])
    pair_i = [0]

    S.dma('sp', vt[:], vecs, writes=['vt'])
    S.dma('sp', cw[:], cw_in, writes=['cw'])
    S.dma('sp', hp[:], hp_in, writes=['hp'])
    S.dma('sp', nw[:], nw_in, writes=['nw'])
    S.dma('pool', wabt[:], wab.rearrange("(kc p) n -> p kc n", p=128), writes=['wabt'])
    S.op('dve', lambda e: e.memset(ones[:], 1.0), writes=['ones'])
    S.op('dve', lambda e: e.memset(St[:], 0.0), writes=[('S', h) for h in range(HV)])
    S.op('dve', lambda e: e.memset(halo[:], 0.0), writes=[('halo', b) for b in range(NCONV)])
    make_affine(S, ident[:], 'ident', ones[:], [[-1, 128]], 0, 1, ALU.is_equal)
    make_affine(S, mU[:], 'mU', ones[:], [[1, 128]], 0, -1, ALU.is_ge)
    make_affine(S, nU[:], 'nU', ones[:], [[1, 128]], 0, -1, ALU.is_ge)
    make_affine(S, nSU[:], 'nSU', ones[:], [[1, 128]], 0, -1, ALU.is_gt)
    make_affine(S, nSL[:], 'nSL', ones[:], [[-1, 128]], 0, 1, ALU.is_gt)
    for m_, k_ in ((nU, 'nU'), (nSU, 'nSU'), (nSL, 'nSL')):
        S.op('dve', lambda e: e.tensor_scalar(out=m_[:], in0=m_[:], scalar1=-1.0, scalar2=30000.0, op0=ALU.add, op1=ALU.mult), reads=[k_], writes=[k_])
    S.op('dve', lambda e: e.tensor_scalar(out=s1[:], in0=vt[:, 0, :], scalar1=1.0, scalar2=None, op0=ALU.add), reads=['vt'], writes=['s1'])
    S.op('act', lambda e: e.activation(out=nA[:], in_=hp[:, 0, :], func=AF.Exp), reads=['hp'], writes=['nA'])
    S.op('dve', lambda e: e.tensor_scalar(out=nA[:], in0=nA[:], scalar1=-1.0, scalar2=None, op0=ALU.mult), reads=['nA'], writes=['nA'])

    hv_ = hT.rearrange("(kc p) t -> p kc t", p=128)
    ov = out.rearrange("(h p) t -> p h t", p=128)
    evs = []
    cnt = [0]
    GK = ['gs']
    for t in range(T // NT):
        ts = slice(t * NT, (t + 1) * NT)
        for half in range(2):
            hb = half * (KC // 2)
            S.dma('sp', Hs[:], hv_[:, hb:hb + KC // 2, ts], writes=[('Hs', b) for b in range(KC // 2)])
            for b in range(KC // 2):
                blk = hb + b
                S.op('act', lambda e: e.activation(out=U[:, blk, :], in_=Hs[:, b, :], func=AF.Identity, scale=s1[:, blk:blk + 1], bias=vt[:, 1, blk:blk + 1]),
                     reads=[('Hs', b), 's1', 'vt'], writes=[('U', blk)])
        deferred = []
        for cb in range(NCB):
            wi = cb % 2
            S.dma('pool', wb[wi][:].rearrange("p k c -> p (k c)"), w[cb], writes=[('wb', wi)], max_dma_last_dim=8192)
            pp = PS[wi]
            for kc in range(KC):
                S.op('pe', lambda e: e.matmul(pp[:, 0:NT], wb[wi][:, kc, :], U[:, kc, :], start=(kc == 0), stop=(kc == KC - 1)),
                     reads=[('wb', wi), ('U', kc)], writes=[pk(wi)])
            while deferred:
                deferred.pop(0)()
            if cb >= NCONV:
                hv = cb - NCONV
                S.op('act', lambda e: e.activation(out=SZ[:, hv, :], in_=pp[:, 0:NT], func=AF.Silu), reads=[pk(wi)], writes=[('SZ', hv)])
                continue
            pr = pre[wi]; ac = acc[wi]
            S.op('dve', lambda e: e.tensor_copy(out=pr[:, 0:3], in_=halo[:, cb, :]), reads=[('halo', cb)], writes=[('pre', wi)])
            S.op('act', lambda e: e.activation(out=pr[:, 3:3 + NT], in_=pp[:, 0:NT], func=AF.Copy), reads=[pk(wi)], writes=[('pre', wi)])
            S.op('dve', lambda e: e.tensor_copy(out=halo[:, cb, :], in_=pr[:, NT:NT + 3]), reads=[('pre', wi)], writes=[('halo', cb)])
            S.op('dve', lambda e: e.tensor_scalar(out=ac[:], in0=pr[:, 0:NT], scalar1=cw[:, cb, 0:1], scalar2=None, op0=ALU.mult),
                 reads=[('pre', wi), 'cw'], writes=[('acc', wi)])
            for j in (1, 2, 3):
                S.op('dve', lambda e: e.scalar_tensor_tensor(out=ac[:], in0=pr[:, j:j + NT], scalar=cw[:, cb, j:j + 1], in1=ac[:], op0=ALU.mult, op1=ALU.add),
                     reads=[('pre', wi), 'cw', ('acc', wi)], writes=[('acc', wi)])
            if cb >= 2 * HQ:
                hv = cb - 2 * HQ
                S.op('act', lambda e: e.activation(out=VT[:, hv, :], in_=ac[:], func=AF.Silu), reads=[('acc', wi)], writes=[('VT', hv)])
                continue
            x = xs[wi]; s_ = sq[wi]
            S.op('act', lambda e: e.activation(out=x[:], in_=ac[:], func=AF.Silu), reads=[('acc', wi)], writes=[('xs', wi)])
            S.op('act', lambda e: e.activation(out=s_[:], in_=x[:], func=AF.Square), reads=[('xs', wi)], writes=[('acc', wi)])
            def l2tail(cb=cb, wi=wi, x=x, s_=s_):
                S.op('pe', lambda e: e.matmul(PS[2][:, 0:NT], ones[:], s_[:], start=True, stop=True), reads=[('acc', wi), 'ones'], writes=[pk(2)])
                S.op('dve', lambda e: e.tensor_scalar(out=s_[:], in0=PS[2][:, 0:NT], scalar1=EPS, scalar2=None, op0=ALU.add), reads=[pk(2)], writes=[('acc', wi)])
                S.op('act', lambda e: e.activation(out=s_[:], in_=s_[:], func=AF.Ln), reads=[('acc', wi)], writes=[('acc', wi)])
                S.op('act', lambda e: e.activation(out=s_[:], in_=s_[:], func=AF.Exp, scale=-0.5), reads=[('acc', wi)], writes=[('acc', wi)])
                if cb < HQ:
                    S.op('dve', lambda e: e.scalar_tensor_tensor(out=QT[:, cb, :], in0=x[:], scalar=128.0 ** -0.5, in1=s_[:], op0=ALU.mult, op1=ALU.mult),
                         reads=[('xs', wi), ('acc', wi)], writes=[('QT', cb)])
                else:
                    S.op('dve', lambda e: e.tensor_tensor(out=KT[:, cb - HQ, :], in0=x[:], in1=s_[:], op=ALU.mult),
                         reads=[('xs', wi), ('acc', wi)], writes=[('KT', cb - HQ)])
            deferred.append(l2tail)
        while deferred:
            deferred.pop(0)()
        for c in range(NT // 128 if stage >= 1 else 0):
            cs = slice(c * 128, (c + 1) * 128)
            for kc in range(KC):
                S.op('pe', lambda e: e.matmul(PS[2][:, 0:2 * HV], U[:, kc, cs], wabt[:, kc, :], start=(kc == 0), stop=(kc == KC - 1)),
                     reads=[('U', kc), 'wabt'], writes=[pk(2)])
            G = GS
            def dv(fn, extra=()):
                S.op('dve', fn, reads=GK + list(extra), writes=GK)
            def ac_(fn, extra=()):
                S.op('act', fn, reads=GK + list(extra), writes=GK)
            ac_(lambda e: e.activation(out=G['emb'][:], in_=PS[2][:, HV:2 * HV], func=AF.Exp, scale=-1.0), [pk(2)])
            dv(lambda e: e.tensor_scalar(out=G['den'][:], in0=G['emb'][:], scalar1=1.0, scalar2=None, op0=ALU.add))
            dv(lambda e: e.reciprocal(out=G['beta'][:], in_=G['den'][:]))
            ac_(lambda e: e.activation(out=G['lnb'][:], in_=G['den'][:], func=AF.Ln))
            dv(lambda e: e.tensor_scalar(out=G['lnb'][:], in0=G['lnb'][:], scalar1=-1.0, scalar2=None, op0=ALU.mult))
            dv(lambda e: e.tensor_tensor(out=G['tt'][:], in0=PS[2][:, 0:HV], in1=hp[:, 1, :], op=ALU.add), [pk(2), 'hp'])
            ac_(lambda e: e.activation(out=G['tt'][:], in_=G['tt'][:], func=AF.Exp))
            dv(lambda e: e.tensor_scalar(out=G['tt'][:], in0=G['tt'][:], scalar1=1.0, scalar2=None, op0=ALU.add))
            ac_(lambda e: e.activation(out=G['tt'][:], in_=G['tt'][:], func=AF.Ln))
            dv(lambda e: e.tensor_tensor(out=G['g'][:], in0=G['tt'][:], in1=nA[:], op=ALU.mult), ['nA'])
            S.op('pe', lambda e: e.matmul(PS[2][:, 32:32 + HV], mU[:], G['g'][:], start=True, stop=True), reads=GK + ['mU'], writes=[pk(2)])
            dv(lambda e: e.tensor_copy(out=G['gc'][:], in_=PS[2][:, 32:32 + HV]), [pk(2)])
            S.op('pe', lambda e: e.matmul(PS[2][:, 64:64 + HV], ones[:], G['g'][:], start=True, stop=True), reads=GK + ['ones'], writes=[pk(2)])
            dv(lambda e: e.tensor_copy(out=G['gl'][:], in_=PS[2][:, 64:64 + HV]), [pk(2)])
            dv(lambda e: e.tensor_tensor(out=G['glb'][:], in0=G['gc'][:], in1=G['lnb'][:], op=ALU.add))
            dv(lambda e: e.tensor_scalar(out=G['ngc'][:], in0=G['gc'][:], scalar1=-1.0, scalar2=None, op0=ALU.mult))
            ac_(lambda e: e.activation(out=G['egc'][:], in_=G['gc'][:], func=AF.Exp))
            dv(lambda e: e.tensor_tensor(out=G['bgc'][:], in0=G['egc'][:], in1=G['beta'][:], op=ALU.mult))
            dv(lambda e: e.tensor_tensor(out=G['ekd'][:], in0=G['gl'][:], in1=G['gc'][:], op=ALU.subtract))
            ac_(lambda e: e.activation(out=G['ekd'][:], in_=G['ekd'][:], func=AF.Exp))
            ac_(lambda e: e.activation(out=G['egl'][:], in_=G['gl'][:], func=AF.Exp))
            if stage < 2:
                continue
            col = lambda n, hv: GS[n][:, hv:hv + 1]
            for hq in range(HQ):
                kq = lambda n: (n, 'q', hq)
                s_kk, s_kq, s_kt = slot(), slot(), slot()
                S.op('pe', lambda e: e.matmul(sv(s_kk), KT[:, hq, cs], KT[:, hq, cs], start=True, stop=True), reads=[('KT', hq)], writes=[sk(s_kk)])
                S.op('pe', lambda e: e.matmul(sv(s_kq), KT[:, hq, cs], QT[:, hq, cs], start=True, stop=True), reads=[('KT', hq), ('QT', hq)], writes=[sk(s_kq)])
                S.op('pe', lambda e: e.transpose(sv(s_kt), KT[:, hq, cs], ident[:]), reads=[('KT', hq), 'ident'], writes=[sk(s_kt)])
                S.op('act', lambda e: e.activation(out=KKs[hq][:], in_=sv(s_kk), func=AF.Copy), reads=[sk(s_kk)], writes=[kq('KKs')])
                S.op('dve', lambda e: e.tensor_copy(out=KQs[hq][:], in_=sv(s_kq)), reads=[sk(s_kq)], writes=[kq('KQs')])
                S.op('act', lambda e: e.activation(out=Ktok[hq][:], in_=sv(s_kt), func=AF.Copy), reads=[sk(s_kt)], writes=[kq('Ktok')])
            for hv in range(HV):
                hq = hv // 2
                kq = lambda n: (n, 'q', hq)
                i = cnt[0] % NB
                cnt[0] += 1
                k = lambda n: (n, 'r', i)
                kh = lambda n: (n, 'h', hv)
                pb = pair_i[0] % 2
                pair_i[0] += 1
                bc1 = PS[2][:, 256 * pb:256 * pb + 128]
                bc2 = PS[2][:, 256 * pb + 128:256 * pb + 256]
                S.op('pe', lambda e: e.matmul(bc1, col('gc', hv).to_broadcast([128, 128]), ident[:], start=True, stop=True), reads=GK + ['ident'], writes=[pk(2)])
                S.op('pe', lambda e: e.matmul(bc2, col('glb', hv).to_broadcast([128, 128]), ident[:], start=True, stop=True), reads=GK + ['ident'], writes=[pk(2)])
                S.op('dve', lambda e: e.scalar_tensor_tensor(out=tA[i][:], in0=bc1, scalar=-1.0, in1=nSL[:], op0=ALU.mult, op1=ALU.add), reads=[pk(2), 'nSL'], writes=[k('tA')])
                S.op('act', lambda e: e.activation(out=tA[i][:], in_=tA[i][:], func=AF.Exp, bias=col('glb', hv)), reads=[k('tA')] + GK, writes=[k('tA')])
                S.op('dve', lambda e: e.tensor_tensor(out=tAT[i][:], in0=bc2, in1=nSU[:], op=ALU.add), reads=[pk(2), 'nSU'], writes=[k('tAT')])
                S.op('act', lambda e: e.activation(out=tAT[i][:], in_=tAT[i][:], func=AF.Exp, bias=col('ngc', hv)), reads=[k('tAT')] + GK, writes=[k('tAT')])
                S.op('dve', lambda e: e.tensor_tensor(out=tQ[i][:], in0=bc1, in1=nU[:], op=ALU.add), reads=[pk(2), 'nU'], writes=[k('tQ')])
                S.op('act', lambda e: e.activation(out=tQ[i][:], in_=tQ[i][:], func=AF.Exp, bias=col('ngc', hv)), reads=[k('tQ')] + GK, writes=[k('tQ')])
                S.op('pool', lambda e: e.tensor_tensor(out=Am[hv][:], in0=KKs[hq][:], in1=tA[i][:], op=ALU.mult), reads=[kq('KKs'), k('tA')], writes=[kh('Am')])
                S.op('pool', lambda e: e.tensor_tensor(out=ATm[hv][:], in0=KKs[hq][:], in1=tAT[i][:], op=ALU.mult), reads=[kq('KKs'), k('tAT')], writes=[kh('ATm')])
                S.op('pool', lambda e: e.tensor_tensor(out=QKT[hv][:], in0=KQs[hq][:], in1=tQ[i][:], op=ALU.mult), reads=[kq('KQs'), k('tQ')], writes=[kh('QKT')])
                S.op('pool', lambda e: e.tensor_tensor(out=Rm[0][hv][:], in0=ident[:], in1=ATm[hv][:], op=ALU.subtract), reads=['ident', kh('ATm')], writes=[kh('R0')])
            for hv in range(HV):
                hq = hv // 2
                kq = lambda n: (n, 'q', hq)
                kh = lambda n: (n, 'h', hv)
                s_v = slot()
                S.op('pe', lambda e: e.transpose(sv(s_v), VT[:, hv, cs], ident[:]), reads=[('VT', hv), 'ident'], writes=[sk(s_v)])
                S.op('act', lambda e: e.activation(out=vb[hv][:], in_=sv(s_v), func=AF.Identity, scale=col('beta', hv)), reads=[sk(s_v)] + GK, writes=[kh('vb')])
                S.op('act', lambda e: e.activation(out=kbg[hv][:], in_=Ktok[hq][:], func=AF.Identity, scale=col('bgc', hv)), reads=[kq('Ktok')] + GK, writes=[kh('kbg')])
                S.op('pool', lambda e: e.tensor_tensor(out=kd[hv][:], in0=Ktok[hq][:], in1=col('ekd', hv).to_broadcast([128, 128]), op=ALU.mult), reads=[kq('Ktok')] + GK, writes=[kh('kd')])
            if stage < 3:
                continue
            cur = {hv: (ATm[hv], ('ATm', 'h', hv), Am[hv], ('Am', 'h', hv), Rm[0][hv], ('R0', 'h', hv)) for hv in range(HV)}
            for m in range(1, 7):
                pi = m % 2
                nxt = {}
                for hv in range(HV):
                    kh = lambda n: (n, 'h', hv)
                    Pc, Pk, Qc, Qk, Rc, Rk = cur[hv]
                    s_q = slot()
                    S.op('pe', lambda e: e.matmul(sv(s_q), Pc[:], Qc[:], start=True, stop=True), reads=[Pk, Qk], writes=[sk(s_q)])
                    Qn = Qm[pi][hv]; Qnk = kh('Q%d' % pi)
                    S.op('act', lambda e: e.activation(out=Qn[:], in_=sv(s_q), func=AF.Copy), reads=[sk(s_q)], writes=[Qnk])
                    Pn, Pnk = Pc, Pk
                    if m < 6:
                        s_p = slot()
                        S.op('pe', lambda e: e.matmul(sv(s_p), Qc[:], Pc[:], start=True, stop=True), reads=[Pk, Qk], writes=[sk(s_p)])
                        Pn = Pm[pi][hv]; Pnk = kh('P%d' % pi)
                        S.op('dve', lambda e: e.tensor_copy(out=Pn[:], in_=sv(s_p)), reads=[sk(s_p)], writes=[Pnk])
                    nxt[hv] = (Pn, Pnk, Qn, Qnk)
                for hv in range(HV):
                    kh = lambda n: (n, 'h', hv)
                    Pn, Pnk, Qn, Qnk = nxt[hv]
                    Rc, Rk = cur[hv][4], cur[hv][5]
                    s_r = slot()
                    S.op('pe', lambda e: e.matmul(sv(s_r), Qn[:], Rc[:], start=True, stop=True), reads=[Qnk, Rk], writes=[sk(s_r)])
                    Rn = Rm[pi][hv]; Rnk = kh('R%d' % pi)
                    S.op('dve', lambda e: e.tensor_tensor(out=Rn[:], in0=sv(s_r), in1=Rc[:], op=ALU.add), reads=[sk(s_r), Rk], writes=[Rnk])
                    cur[hv] = (Pn, Pnk, Qn, Qnk, Rn, Rnk)
            if stage < 4:
                continue
            for hv in range(HV):
                kh = lambda n: (n, 'h', hv)
                Rc, Rk = cur[hv][4], cur[hv][5]
                s_u, s_w = slot(), slot()
                S.op('pe', lambda e: e.matmul(sv(s_u), Rc[:], vb[hv][:], start=True, stop=True), reads=[Rk, kh('vb')], writes=[sk(s_u)])
                S.op('pe', lambda e: e.matmul(sv(s_w), kbg[hv][:], Rc[:], start=True, stop=True), reads=[Rk, kh('kbg')], writes=[sk(s_w)])
                S.op('act', lambda e: e.activation(out=Us[hv][:], in_=sv(s_u), func=AF.Copy), reads=[sk(s_u)], writes=[kh('Us')])
                S.op('dve', lambda e: e.tensor_copy(out=WTs[hv][:], in_=sv(s_w)), reads=[sk(s_w)], writes=[kh('WTs')])
            if stage < 5:
                continue
            for hv in range(HV):
                hq = hv // 2
                kh = lambda n: (n, 'h', hv)
                i = cnt[0] % NB
                cnt[0] += 1
                k = lambda n: (n, 'r', i)
                s_vn, s_o1 = slot(), slot()
                S.op('pe', lambda e: e.matmul(sv(s_vn), WTs[hv][:], St[:, hv, :], start=True, stop=True), reads=[kh('WTs'), ('S', hv)], writes=[sk(s_vn)])
                S.op('pe', lambda e: e.matmul(sv(s_o1), QT[:, hq, cs], St[:, hv, :], start=True, stop=True), reads=[('QT', hq), ('S', hv)], writes=[sk(s_o1)])
                S.op('dve', lambda e: e.tensor_tensor(out=vn[i][:], in0=Us[hv][:], in1=sv(s_vn), op=ALU.subtract), reads=[kh('Us'), sk(s_vn)], writes=[k('vn')])
                S.op('act', lambda e: e.activation(out=o1s[i][:], in_=sv(s_o1), func=AF.Identity, scale=col('egc', hv)), reads=[sk(s_o1)] + GK, writes=[k('o1s')])
                s_o2, s_sn = slot(), slot()
                S.op('pe', lambda e: e.matmul(sv(s_o2), QKT[hv][:], vn[i][:], start=True, stop=True), reads=[kh('QKT'), k('vn')], writes=[sk(s_o2)])
                S.op('pe', lambda e: e.matmul(sv(s_sn), kd[hv][:], vn[i][:], start=True, stop=True), reads=[kh('kd'), k('vn')], writes=[sk(s_sn)])
                S.op('dve', lambda e: e.tensor_tensor(out=Os[hv][:], in0=sv(s_o2), in1=o1s[i][:], op=ALU.add), reads=[sk(s_o2), k('o1s')], writes=[kh('Os')])
                S.op('dve', lambda e: e.scalar_tensor_tensor(out=St[:, hv, :], in0=St[:, hv, :], scalar=col('egl', hv), in1=sv(s_sn), op0=ALU.mult, op1=ALU.add),
                     reads=[('S', hv), sk(s_sn)] + GK, writes=[('S', hv)])
            for hv in range(HV):
                kh = lambda n: (n, 'h', hv)
                ssq, rstd = scs[:, hv, 0:1], scs[:, hv, 1:2]
                S.op('act', lambda e: e.activation(out=On[hv][:], in_=Os[hv][:], func=AF.Square, accum_out=ssq), reads=[kh('Os'), kh('sc')], writes=[kh('On'), kh('sc')])
                S.op('dve', lambda e: e.tensor_scalar(out=ssq, in0=ssq, scalar1=1.0 / 128, scalar2=EPS, op0=ALU.mult, op1=ALU.add), reads=[kh('sc')], writes=[kh('sc')])
                S.op('act', lambda e: e.activation(out=ssq, in_=ssq, func=AF.Ln), reads=[kh('sc')], writes=[kh('sc')])
                S.op('act', lambda e: e.activation(out=rstd, in_=ssq, func=AF.Exp, scale=-0.5), reads=[kh('sc')], writes=[kh('sc')])
                S.op('dve', lambda e: e.tensor_scalar(out=On[hv][:], in0=Os[hv][:], scalar1=rstd, scalar2=None, op0=ALU.mult), reads=[kh('Os'), kh('sc')], writes=[kh('On')])
            for hv in range(HV):
                kh = lambda n: (n, 'h', hv)
                s_ot = slot()
                S.op('pe', lambda e: e.transpose(sv(s_ot), On[hv][:], ident[:]), reads=[kh('On'), 'ident'], writes=[sk(s_ot)])
                S.op('dve', lambda e: e.scalar_tensor_tensor(out=OT[:, hv, cs], in0=sv(s_ot), scalar=nw[:, 0:1], in1=SZ[:, hv, cs], op0=ALU.mult, op1=ALU.mult),
                     reads=[sk(s_ot), 'nw', ('SZ', hv)], writes=[('OT', hv)])
        if stage < 5:
            S.op('dve', lambda e: e.tensor_copy(out=OT[:], in_=VT[:]), reads=[('VT', h) for h in range(HV)] + [('OT', h) for h in range(HV)], writes=[('OT', h) for h in range(HV)])
        evs.append(S.dma('sp', ov[:, :, ts], OT[:], reads=[('OT', h) for h in range(HV)]))
    S.finish(evs)
    return nc


def run_gdn(hT_full, scale1, shift1, w_in, conv_w, a_log, dt_bias, norm_w, stage=9):
    T = hT_full.shape[1]
    HV = 64 // NCORE
    HQ = HV // 2
    nc = build_gdn(T, HV=HV, stage=stage)
    vecs = np.ascontiguousarray(np.stack([vec_pk(scale1), vec_pk(shift1)], axis=1))
    nwp = np.ascontiguousarray(norm_w.reshape(128, 1).astype(np.float32))
    KD = 4096; VDIM = 8192
    in_maps = []
    for c in range(NCORE):
        qcols = [slice((c * HQ + j) * 128, (c * HQ + j + 1) * 128) for j in range(HQ)]
        kcols = [slice(KD + (c * HQ + j) * 128, KD + (c * HQ + j + 1) * 128) for j in range(HQ)]
        vcols = [slice(2 * KD + (c * HV + j) * 128, 2 * KD + (c * HV + j + 1) * 128) for j in range(HV)]
        zcols = [slice(2 * KD + VDIM + (c * HV + j) * 128, 2 * KD + VDIM + (c * HV + j + 1) * 128) for j in range(HV)]
        wc = blk_cols(np.concatenate([w_in[:, s] for s in qcols + kcols + vcols + zcols], axis=1))
        a0 = 2 * KD + 2 * VDIM
        wab = np.ascontiguousarray(np.concatenate([w_in[:, a0 + c * HV: a0 + (c + 1) * HV], w_in[:, a0 + 64 + c * HV: a0 + 64 + (c + 1) * HV]], axis=1))
        cwc = np.concatenate([conv_w[:, s] for s in qcols + kcols + vcols], axis=1)
        cwp = np.ascontiguousarray(cwc.reshape(4, -1, 128).transpose(2, 1, 0))
        hp = np.ascontiguousarray(np.broadcast_to(np.stack([a_log[c * HV:(c + 1) * HV], dt_bias[c * HV:(c + 1) * HV]])[None], (128, 2, HV)).astype(np.float32))
        in_maps.append({"hT": hT_full, "vecs": vecs, "w": wc, "wab": wab, "cw": cwp, "hp": hp, "nw": nwp})
    res = _run(nc, in_maps)
    return np.concatenate([r["out"] for r in res], axis=0)


def build_route(TK, NT=256):
    nc = bass.Bass("TRN2", target_bir_lowering=False)
    NT = min(NT, TK)
    h1T = nc.dram_tensor("h1T", [D, TK], F32, kind="ExternalInput").ap()
    vecs = nc.dram_tensor("vecs", [128, 2, KC], F32, kind="ExternalInput").ap()
    wr = nc.dram_tensor("wr", [D, NG], F32, kind="ExternalInput").ap()
    out = nc.dram_tensor("out", [128, TK // 128], F32, kind="ExternalOutput").ap()
    S = Sched(nc)
    vt = nc.alloc_sbuf_tensor("vt", [128, 2, KC], F32)
    s2 = nc.alloc_sbuf_tensor("s2", [128, KC], F32)
    wrt = nc.alloc_sbuf_tensor("wrt", [128, KC, NG], F32)
    Hs = nc.alloc_sbuf_tensor("Hs", [128, KC, NT], F32)
    A = nc.alloc_sbuf_tensor("A", [128, KC, NT], F32R)
    Af = A[:].bitcast(F32)
    lgt = nc.alloc_sbuf_tensor("lgt", [128, NG], F32)
    ohg = nc.alloc_sbuf_tensor("ohg", [128, NG], F32)
    gm = nc.alloc_sbuf_tensor("gm", [128, 1], F32)
    ioti = nc.alloc_sbuf_tensor("ioti", [128, NG], I32)
    iotf = nc.alloc_sbuf_tensor("iotf", [128, NG], F32)
    gall = nc.alloc_sbuf_tensor("gall", [128, TK // 128], F32)
    ps = nc.alloc_psum_tensor("ps", [128, 512], F32)
    S.dma('sp', vt[:], vecs, writes=['vt'])
    S.dma('sp', wrt[:], wr.rearrange("(kc p) n -> p kc n", p=128), writes=['wrt'])
    S.op('pool', lambda e: e.iota(out=ioti[:], pattern=[[1, NG]], base=0, channel_multiplier=0), writes=['ioti'])
    S.op('dve', lambda e: e.tensor_copy(out=iotf[:], in_=ioti[:]), reads=['ioti'], writes=['iotf'])
    S.op('dve', lambda e: e.tensor_scalar(out=s2[:], in0=vt[:, 0, :], scalar1=1.0, scalar2=None, op0=ALU.add), reads=['vt'], writes=['s2'])
    hv = h1T.rearrange("(kc p) t -> p kc t", p=128)
    R = ['r']
    for t in range(TK // NT):
        ts = slice(t * NT, (t + 1) * NT)
        S.dma('sp', Hs[:], hv[:, :, ts], writes=[('Hs', b) for b in range(KC)])
        for blk in range(KC):
            S.op('act', lambda e: e.activation(out=A[:, blk, :], in_=Hs[:, blk, :], func=AF.Identity, scale=s2[:, blk:blk + 1], bias=vt[:, 1, blk:blk + 1]),
                 reads=[('Hs', blk), 's2', 'vt'], writes=[('A', blk)])
        for sub in range(NT // 128):
            ss = slice(sub * 128, (sub + 1) * 128)
            col = t * (NT // 128) + sub
            for kc in range(KC):
                S.op('pe', lambda e: e.matmul(ps[:, 0:NG], Af[:, kc, ss], wrt[:, kc, :], start=(kc == 0), stop=(kc == KC - 1)),
                     reads=[('A', kc), 'wrt'], writes=['ps'])
            S.op('act', lambda e: e.activation(out=lgt[:], in_=ps[:, 0:NG], func=AF.Copy), reads=['ps'], writes=R)
            S.op('dve', lambda e: e.tensor_reduce(out=gm[:], in_=lgt[:], axis=AX.X, op=ALU.max), reads=R, writes=R)
            S.op('dve', lambda e: e.tensor_scalar(out=ohg[:], in0=lgt[:], scalar1=gm[:, 0:1], scalar2=None, op0=ALU.is_equal), reads=R, writes=R)
            S.op('dve', lambda e: e.tensor_tensor(out=ohg[:], in0=ohg[:], in1=iotf[:], op=ALU.mult), reads=R + ['iotf'], writes=R)
            S.op('dve', lambda e: e.tensor_reduce(out=gall[:, col:col + 1], in_=ohg[:], axis=AX.X, op=ALU.add), reads=R, writes=R + ['gall'])
    ev = S.dma('sp', out, gall[:], reads=['gall'])
    S.finish([ev])
    return nc


def run_moe_grouped(h1T_full, scale2, shift2, gate2, lnw, lnb, w_group, w_expert, w_gate, w_up, w_down):
    T = h1T_full.shape[1]
    TK = T // NCORE
    nc = build_route(TK)
    vecs2 = np.ascontiguousarray(np.stack([vec_pk(scale2), vec_pk(shift2)], axis=1))
    in_maps = [{"h1T": np.ascontiguousarray(h1T_full[:, i * TK:(i + 1) * TK]), "vecs": vecs2, "wr": np.ascontiguousarray(w_group)}
               for i in range(NCORE)]
    res = _run(nc, in_maps)
    gidx = np.concatenate([np.ascontiguousarray(r["out"].T).reshape(-1) for r in res]).astype(np.int64)
    idxs = [np.nonzero(gidx == g)[0] for g in range(NG)]
    NP = max(256, -(-max(len(ix) for ix in idxs) // 256) * 256)
    nc2 = build_moe(NP, EPG=EPG, grouped=True)
    vecs = np.ascontiguousarray(np.stack([vec_pk(scale2), vec_pk(shift2), vec_pk(gate2), vec_pk(lnw), vec_pk(lnb)], axis=1))
    in_maps = []
    for g in range(NG):
        hg = np.zeros((D, NP), np.float32)
        hg[:, :len(idxs[g])] = h1T_full[:, idxs[g]]
        wr = np.ascontiguousarray(np.concatenate([w_group, w_expert[:, g * EPG:(g + 1) * EPG]], axis=1))
        in_maps.append({"h1T": hg, "vecs": vecs, "wr": wr,
                        "wg": np.stack([blk_cols(w_gate[e]) for e in range(g * EPG, (g + 1) * EPG)]),
                        "wu": np.stack([blk_cols(w_up[e]) for e in range(g * EPG, (g + 1) * EPG)]),
                        "wd": np.ascontiguousarray(w_down[g * EPG:(g + 1) * EPG])})
    res = _run(nc2, in_maps)
    outT = np.empty((D, T), np.float32)
    for g in range(NG):
        outT[:, idxs[g]] = res[g]["out"][:, :len(idxs[g])]
    return outT


def kernel(x, c, ada_w, ada_b, ln_w, ln_b, gdn_w_in, gdn_conv_w, gdn_a_log, gdn_dt_bias,
           gdn_norm_w, gdn_w_out, hgrn_w_in, hgrn_lb_logits, hgrn_norm_w, hgrn_w_out,
           moe_w_group, moe_w_expert, moe_w_gate, moe_w_up, moe_w_down):
    f = lambda a: np.asarray(a, np.float32)
    x = f(x); c = f(c)
    mod = run_mod(c, f(ada_w), f(ada_b))
    hT = np.ascontiguousarray(x[0].T)
    for layer in range(2):
        sh1, sc1, g1, sh2, sc2, g2 = [mod[layer, i * D:(i + 1) * D] for i in range(6)]
        if layer % 2 == 0:
            ogT = run_gdn(hT, sc1, sh1, f(gdn_w_in[0]), f(gdn_conv_w[0]), f(gdn_a_log[0]), f(gdn_dt_bias[0]), f(gdn_norm_w[0]))
            w_out = f(gdn_w_out[0])
        else:
            ogT = run_hgrn(hT, sc1, sh1, f(hgrn_w_in[0]), f(hgrn_lb_logits), f(hgrn_norm_w[0]))
            w_out = f(hgrn_w_out[0])
        h1T = run_outln(ogT, hT, w_out, g1, f(ln_w[layer, 0]), f(ln_b[layer, 0]))
        hT = run_moe_grouped(h1T, sc2, sh2, g2, f(ln_w[layer, 1]), f(ln_b[layer, 1]), f(moe_w_group[layer]), f(moe_w_expert[layer]),
                             f(moe_w_gate[layer]), f(moe_w_up[layer]), f(moe_w_down[layer]))
    return np.ascontiguousarray(hT.T)[None].astype(np.float32)
```
